# Optimizing a Trainium2 kernel written in Bass

```python
import math
import jax, jax.numpy as jnp
from jax import lax
import numpy as np

D_MODEL = 2048
BATCH = 4
SEQ = 4096
DEPTH = 2

N_HEADS = 32
N_KV_HEADS = 4
HEAD_DIM = D_MODEL // N_HEADS
GROUP = N_HEADS // N_KV_HEADS
Q_DIM = N_HEADS * HEAD_DIM
KV_DIM = N_KV_HEADS * HEAD_DIM
D_FF = 4 * D_MODEL
WINDOW = 128
MOBA_BLOCK = 256
MOBA_TOPK = 3
MOBA_Q_CHUNK = 16
N_A_LAYERS = (DEPTH + 1) // 2
N_B_LAYERS = DEPTH - N_A_LAYERS
RMS_EPS = 1e-6
NEG_INF = -1e30

kernel_name = "yoco_swa_sink_moba_hybrid"


def rmsnorm(x, g):
    xf = x.astype(jnp.float32)
    y = xf * lax.rsqrt(jnp.mean(xf * xf, axis=-1, keepdims=True) + RMS_EPS)
    return (y * g.astype(jnp.float32)).astype(x.dtype)


def alibi_slopes(n):
    return jnp.exp2(-8.0 * jnp.arange(1, n + 1, dtype=jnp.float32) / n)


def sq_relu_mlp(x, w_up, w_down):
    h = jax.nn.relu(x @ w_up)
    return (h * h) @ w_down


def sliding_window_sink_attention(q, k, v, sinks, slopes):
    B, T, H, dh = q.shape
    nb = T // WINDOW
    qb = q.reshape(B, nb, WINDOW, N_KV_HEADS, GROUP, dh)
    kb = k.reshape(B, nb, WINDOW, N_KV_HEADS, dh)
    vb = v.reshape(B, nb, WINDOW, N_KV_HEADS, dh)

    def with_prev(a):
        prev = jnp.concatenate([jnp.zeros_like(a[:, :1]), a[:, :-1]], axis=1)
        return jnp.concatenate([prev, a], axis=2)

    kk, vv = with_prev(kb), with_prev(vb)
    s = jnp.einsum('bnqkgd,bnskd->bkgnqs', qb, kk).astype(jnp.float32) * (1.0 / math.sqrt(dh))
    qi = jnp.arange(WINDOW)[:, None] + WINDOW
    si = jnp.arange(2 * WINDOW)[None, :]
    dist = qi - si
    band = (dist >= 0) & (dist < WINDOW)
    n_idx = jnp.arange(nb)[:, None, None]
    mask = band[None] & ((n_idx > 0) | (si[None] >= WINDOW))
    bias = -slopes[:, :, None, None, None] * dist.astype(jnp.float32)[None, None, None]
    s = jnp.where(mask, s + bias, NEG_INF)
    sink = jnp.broadcast_to(sinks.astype(jnp.float32).reshape(N_KV_HEADS, GROUP, 1, 1, 1),
                            s.shape[:-1] + (1,))
    p = jax.nn.softmax(jnp.concatenate([s, sink], axis=-1), axis=-1)[..., :-1]
    o = jnp.einsum('bkgnqs,bnskd->bnqkgd', p.astype(v.dtype), vv)
    return o.reshape(B, T, H * dh)


def moba_shared_kv_side(x, kv_norm, w_kv_shared):
    B, T, _ = x.shape
    kv = rmsnorm(x, kv_norm) @ w_kv_shared
    k, v = jnp.split(kv, [KV_DIM], axis=-1)
    nblk = -(-T // MOBA_BLOCK)
    pad = nblk * MOBA_BLOCK - T

    def blocks(a):
        a = jnp.pad(a.reshape(B, T, N_KV_HEADS, HEAD_DIM), ((0, 0), (0, pad), (0, 0), (0, 0)))
        return a.reshape(B, nblk, MOBA_BLOCK, N_KV_HEADS, HEAD_DIM).transpose(0, 3, 1, 2, 4)

    kblk, vblk = blocks(k), blocks(v)
    counts = jnp.clip(T - jnp.arange(nblk) * MOBA_BLOCK, 1, MOBA_BLOCK).astype(jnp.float32)
    kmean = (kblk.astype(jnp.float32).sum(axis=3) / counts[:, None]).astype(k.dtype)
    return kblk, vblk, kmean


def moba_attention(q, kblk, vblk, kmean, slopes):
    B, T, H, dh = q.shape
    nblk = kblk.shape[2]
    ks = min(MOBA_TOPK, nblk)
    scale = 1.0 / math.sqrt(dh)
    q5 = q.reshape(B, T, N_KV_HEADS, GROUP, dh)
    gate = jnp.einsum('btkgd,bknd->bkgtn', q5, kmean).astype(jnp.float32)
    qblock = jnp.arange(T) // MOBA_BLOCK
    past = jnp.arange(nblk)[None, :] < qblock[:, None]
    gate = jnp.where(past, gate, NEG_INF)
    _, idx = lax.top_k(gate, ks)
    valid = idx < qblock[:, None]

    nc = T // MOBA_Q_CHUNK
    q_c = q5.reshape(B, nc, MOBA_Q_CHUNK, N_KV_HEADS, GROUP, dh).transpose(1, 0, 3, 4, 2, 5)
    idx_c = jnp.moveaxis(idx.reshape(B, N_KV_HEADS, GROUP, nc, MOBA_Q_CHUNK, ks), 3, 0)
    val_c = jnp.moveaxis(valid.reshape(B, N_KV_HEADS, GROUP, nc, MOBA_Q_CHUNK, ks), 3, 0)
    t0s = jnp.arange(nc, dtype=jnp.int32) * MOBA_Q_CHUNK
    b_ix = jnp.arange(B)[:, None, None, None, None]
    kv_ix = jnp.arange(N_KV_HEADS)[None, :, None, None, None]
    slopes4 = slopes[:, :, None, None]
    slopes5 = slopes[:, :, None, None, None]
    blk_pos = jnp.arange(MOBA_BLOCK)

    def step(args):
        qc, ic, vc, t0 = args
        pos_t = t0 + jnp.arange(MOBA_Q_CHUNK)
        k_sel = kblk[b_ix, kv_ix, ic]
        v_sel = vblk[b_ix, kv_ix, ic]
        s_sel = jnp.einsum('bkgqd,bkgqjsd->bkgqjs', qc, k_sel).astype(jnp.float32) * scale
        dist_sel = (pos_t[:, None, None] - (ic[..., None] * MOBA_BLOCK + blk_pos)).astype(jnp.float32)
        s_sel = jnp.where(vc[..., None], s_sel - slopes5 * dist_sel, NEG_INF)
        s_sel = s_sel.reshape(B, N_KV_HEADS, GROUP, MOBA_Q_CHUNK, ks * MOBA_BLOCK)
        j0 = t0 // MOBA_BLOCK
        k_own = lax.dynamic_index_in_dim(kblk, j0, axis=2, keepdims=False)
        v_own = lax.dynamic_index_in_dim(vblk, j0, axis=2, keepdims=False)
        dist_own = pos_t[:, None] - (j0 * MOBA_BLOCK + blk_pos)[None, :]
        s_own = jnp.einsum('bkgqd,bksd->bkgqs', qc, k_own).astype(jnp.float32) * scale
        s_own = jnp.where(dist_own >= 0, s_own - slopes4 * dist_own.astype(jnp.float32), NEG_INF)
        p = jax.nn.softmax(jnp.concatenate([s_sel, s_own], axis=-1), axis=-1).astype(vblk.dtype)
        p_sel = p[..., :ks * MOBA_BLOCK].reshape(B, N_KV_HEADS, GROUP, MOBA_Q_CHUNK, ks, MOBA_BLOCK)
        p_own = p[..., ks * MOBA_BLOCK:]
        return (jnp.einsum('bkgqjs,bkgqjsd->bkgqd', p_sel, v_sel)
                + jnp.einsum('bkgqs,bksd->bkgqd', p_own, v_own))

    o = lax.map(step, (q_c, idx_c, val_c, t0s))
    return o.transpose(1, 0, 4, 2, 3, 5).reshape(B, T, H * dh)


def setup_inputs(seed: int = 0) -> dict:
    key = jax.random.key(seed)
    ks = jax.random.split(key, 16)
    f32 = jnp.float32

    def w(k, shape, fan_in):
        return jax.random.normal(k, shape, f32) * (fan_in ** -0.5)

    def gain(k, shape):
        return 1.0 + 0.05 * jax.random.normal(k, shape, f32)

    return {
        "x": jax.random.normal(ks[0], (BATCH, SEQ, D_MODEL), f32),
        "w_qkv_a": w(ks[1], (N_A_LAYERS, D_MODEL, Q_DIM + 2 * KV_DIM), D_MODEL),
        "sinks_a": 0.5 * jax.random.normal(ks[2], (N_A_LAYERS, N_HEADS), f32),
        "w_o_a": w(ks[3], (N_A_LAYERS, Q_DIM, D_MODEL), Q_DIM),
        "kv_norm": gain(ks[4], (D_MODEL,)),
        "w_kv_shared": w(ks[5], (D_MODEL, 2 * KV_DIM), D_MODEL),
        "w_q_b": w(ks[6], (N_B_LAYERS, D_MODEL, Q_DIM), D_MODEL),
        "w_o_b": w(ks[7], (N_B_LAYERS, Q_DIM, D_MODEL), Q_DIM),
        "norm_attn_pre": gain(ks[8], (DEPTH, D_MODEL)),
        "norm_attn_post": gain(ks[9], (DEPTH, D_MODEL)),
        "norm_mlp_pre": gain(ks[10], (DEPTH, D_MODEL)),
        "norm_mlp_post": gain(ks[11], (DEPTH, D_MODEL)),
        "w_up": w(ks[12], (DEPTH, D_MODEL, D_FF), D_MODEL),
        "w_down": w(ks[13], (DEPTH, D_FF, D_MODEL), D_FF),
    }


def reference(x, w_qkv_a, sinks_a, w_o_a, kv_norm, w_kv_shared, w_q_b, w_o_b,
              norm_attn_pre, norm_attn_post, norm_mlp_pre, norm_mlp_post, w_up, w_down):
    B, T, _ = x.shape
    slopes = alibi_slopes(N_HEADS).reshape(N_KV_HEADS, GROUP)
    shared = None
    for l in range(DEPTH):
        h = rmsnorm(x, norm_attn_pre[l])
        if l < N_A_LAYERS:
            qkv = h @ w_qkv_a[l]
            q, k, v = jnp.split(qkv, [Q_DIM, Q_DIM + KV_DIM], axis=-1)
            mix = sliding_window_sink_attention(
                q.reshape(B, T, N_HEADS, HEAD_DIM),
                k.reshape(B, T, N_KV_HEADS, HEAD_DIM),
                v.reshape(B, T, N_KV_HEADS, HEAD_DIM),
                sinks_a[l], slopes) @ w_o_a[l]
        else:
            j = l - N_A_LAYERS
            q = (h @ w_q_b[j]).reshape(B, T, N_HEADS, HEAD_DIM)
            kblk, vblk, kmean = shared
            mix = moba_attention(q, kblk, vblk, kmean, slopes) @ w_o_b[j]
        x = x + rmsnorm(mix, norm_attn_post[l])
        h = rmsnorm(x, norm_mlp_pre[l])
        x = x + rmsnorm(sq_relu_mlp(h, w_up[l], w_down[l]), norm_mlp_post[l])
        if l == N_A_LAYERS - 1:
            shared = moba_shared_kv_side(x, kv_norm, w_kv_shared)
    return x
```

```python
import numpy as np
from contextlib import ExitStack
import concourse.bass as bass
import concourse.mybir as mybir
from concourse.bass_utils import run_bass_kernel_spmd

F32 = mybir.dt.float32
BF16 = mybir.dt.bfloat16
AF = mybir.ActivationFunctionType
ALU = mybir.AluOpType
AX = mybir.AxisListType

D = 2048
DFF = 8192
NT = 16
NG = 2
GT = 8
GTOK = GT * 128
NH = 32
EPS = 1e-6
RS = 1.0 / float(np.sqrt(D))
NEG = -1.0e30
VW = 72


class Res:
    __slots__ = ("name", "writers", "readers", "excl")

    def __init__(self, name, excl=False):
        self.name = name
        self.writers = []
        self.readers = []
        self.excl = excl


class Prog:
    def __init__(self, nc, n_dma_sems=8):
        self.nc = nc
        self.eng = {"pe": nc.tensor, "act": nc.scalar, "dve": nc.vector, "pool": nc.gpsimd, "sp": nc.sync}
        self.sem = {}
        self.cnt = {}
        for e in ("pe", "act", "dve", "pool"):
            self.sem[e] = nc.alloc_semaphore(name="s_" + e)
            self.cnt[e] = 0
        self.known = {}
        self.dma_pool = {}
        for q in ("sp", "pool"):
            self.dma_pool[q] = [[nc.alloc_semaphore(name=f"d_{q}{i}"), 0] for i in range(n_dma_sems)]
        self.dma_rr = {"sp": 0, "pool": 0}

    def _wait(self, ename, ev):
        s, v = ev
        key = (ename, s.name)
        if self.known.get(key, 0) >= v:
            return
        self.known[key] = v
        self.eng[ename].wait_ge(s, v)

    def _deps(self, ename, reads, writes):
        evs = {}

        def add(ev):
            s, v = ev
            if s.name not in evs or evs[s.name][1] < v:
                evs[s.name] = ev
        for r in reads:
            for ev in r.writers:
                add(ev)
        for w in writes:
            for ev in w.writers:
                add(ev)
            for ev in w.readers:
                add(ev)
        for ev in evs.values():
            if ename == "pe" and ev[0] is self.sem["pe"]:
                continue
            self._wait(ename, ev)

    def _commit(self, ev, reads, writes):
        for r in reads:
            r.readers.append(ev)
            if len(r.readers) > 48:
                best = {}
                for s, v in r.readers:
                    if s.name not in best or best[s.name][1] < v:
                        best[s.name] = (s, v)
                r.readers = list(best.values())
        for w in writes:
            w.writers = [ev]
            w.readers = []

    def op(self, ename, fn, reads=(), writes=(), inc=True):
        if ename != "pe":
            ex = [r for r in reads if r.excl]
            if ex:
                writes = list(writes) + ex
                reads = [r for r in reads if not r.excl]
        self._deps(ename, reads, writes)
        ins = fn()
        seq = self.cnt[ename] + 1
        if inc:
            ins.then_inc(self.sem[ename], 1)
            self.cnt[ename] = seq
        self._commit((self.sem[ename], seq), reads, writes)
        return ins

    def dma(self, q, out, in_, reads=(), writes=()):
        self._deps(q, reads, writes)
        pool = self.dma_pool[q]
        i = self.dma_rr[q]
        self.dma_rr[q] = (i + 1) % len(pool)
        ent = pool[i]
        if ent[1] > 0:
            self._wait(q, (ent[0], ent[1]))
        ent[1] += 16
        self.eng[q].dma_start(out=out, in_=in_).then_inc(ent[0], 16)
        ev = (ent[0], ent[1])
        self._commit(ev, reads, writes)
        return ev

    def collective(self, fn, reads=(), writes=()):
        q = "pool"
        self._deps(q, reads, writes)
        if not hasattr(self, "cc_sem"):
            self.cc_sem = [self.nc.alloc_semaphore(name="s_cc"), 0]
        ent = self.cc_sem
        if ent[1] > 0:
            self._wait(q, (ent[0], ent[1]))
        ent[1] += 16
        fn().then_inc(ent[0], 16)
        ev = (ent[0], ent[1])
        self._commit(ev, reads, writes)
        return ev

    def barrier(self):
        evs = []
        for e in ("pe", "act", "dve", "pool"):
            if self.cnt[e] > 0:
                evs.append((self.sem[e], self.cnt[e]))
        for q, pool in self.dma_pool.items():
            for s, v in pool:
                if v > 0:
                    evs.append((s, v))
        if hasattr(self, "cc_sem") and self.cc_sem[1] > 0:
            evs.append((self.cc_sem[0], self.cc_sem[1]))
        for e in ("pe", "act", "dve", "pool", "sp"):
            for ev in evs:
                self._wait(e, ev)


class Rot:
    def __init__(self, items):
        self.items = items
        self.i = 0

    def next(self):
        it = self.items[self.i]
        self.i = (self.i + 1) % len(self.items)
        return it


def head_of(c, u):
    return 8 * (2 * (c // 8) + u) + (c % 8)


def q_perm():
    perm = np.zeros(D, dtype=np.int64)
    for c in range(16):
        for u in range(2):
            h = head_of(c, u)
            perm[c * 128 + u * 64: c * 128 + u * 64 + 64] = np.arange(h * 64, h * 64 + 64)
    return perm


def my_slopes():
    sl = np.zeros(NH, dtype=np.float64)
    for c in range(16):
        for u in range(2):
            sl[c * 2 + u] = 2.0 ** (-8.0 * (head_of(c, u) + 1) / NH)
    return sl


def build_tables(p):
    sl = my_slopes()[None, :, None]
    s = np.arange(128, dtype=np.float64)[:, None, None]
    t = np.arange(128, dtype=np.float64)[None, None, :]
    d = t - s + 0.0 * sl
    causal = np.where(d >= 0, np.exp(-sl * d), 0.0)
    swaprev = np.where(d < 0, np.exp(-sl * (d + 128.0)), 0.0)
    full1 = np.exp(-sl * (d + 128.0))
    full0 = np.exp(-sl * (d + 256.0))
    zeros = np.zeros_like(causal)
    f = lambda a: np.ascontiguousarray(a.reshape(128, NH * 128).astype(np.float32))
    tabs = {
        "tab_swa": np.stack([f(swaprev), f(causal)], axis=1),
        "tab_swa0": np.stack([f(zeros if p == 0 else swaprev), f(causal)], axis=1),
        "tab_full": np.stack([f(full0), f(full1)], axis=1),
        "tab_own": np.stack([f(causal if p == 0 else full1), f(zeros if p == 0 else causal)], axis=1),
        "tab_swaA": np.stack([f(zeros if p == 0 else swaprev), f(causal)], axis=1),
        "tab_swaB": np.stack([f(swaprev if p == 0 else zeros), f(causal)], axis=1),
        "tab_full_l": np.stack([f(full0 if p == 0 else full1), f(full1 if p == 0 else full0)], axis=1),
        "tab_own_l": np.stack([f(causal), f(zeros if p == 0 else full1)], axis=1),
    }
    fr = np.zeros((NH, 15), dtype=np.float64)
    for k in range(15):
        delta = 15 - k
        fr[:, k] = np.exp(-my_slopes() * 128.0 * (2 * delta + p - 2))
    tabs["ftab"] = np.ascontiguousarray(fr.reshape(1, NH * 15).astype(np.float32))
    return tabs


class Builder:
    def __init__(self, nc):
        self.nc = nc
        self.P = Prog(nc)
        self.dram = {}
        self.dres = {}

    def din(self, name, shape, dt=F32):
        self.dram[name] = self.nc.dram_tensor(name, list(shape), dt, kind="ExternalInput").ap()
        return self.dram[name]

    def dout(self, name, shape, dt=F32):
        self.dram[name] = self.nc.dram_tensor(name, list(shape), dt, kind="ExternalOutput").ap()
        return self.dram[name]

    def dtmp(self, name, shape, dt=F32):
        self.dram[name] = self.nc.dram_tensor(name, list(shape), dt, kind="Internal").ap()
        return self.dram[name]

    def grow(self, row):
        r = getattr(self, "gbase", 0) + row
        return self.dram["gains"][r:r + 1, :]

    def dr(self, name, idx=0):
        key = (name, idx)
        if key not in self.dres:
            self.dres[key] = Res(f"{name}[{idx}]")
        return self.dres[key]

    def uname(self, name):
        self.uid = getattr(self, "uid", 0) + 1
        return f"{name}_{self.uid}"

    def alloc(self, es, name, shape, dt):
        return es.enter_context(self.nc.sbuf_tensor(self.uname(name), list(shape), dt))

    def palloc(self, es, name, shape, dt=F32):
        return es.enter_context(self.nc.psum_tensor(self.uname(name), list(shape), dt))

    def bufs(self, es, name, shape, dt, n, psum=False):
        items = []
        for i in range(n):
            t = (self.palloc if psum else self.alloc)(es, f"{name}{i}", shape, dt)
            items.append((t, Res(f"{name}{i}", excl=psum)))
        return Rot(items)

    def load_bc(self, es, name, src_row):
        n = src_row.shape[-1]
        t = self.alloc(es, name, [128, n], F32)
        r = Res(name)
        self.P.dma("sp", t[:], src_row.partition_broadcast(128), writes=[r])
        return t, r

    def setup_consts(self, es):
        nc, P = self.nc, self.P
        self.ident = self.alloc(es, "ident", [128, 128], BF16)
        self.R_ident = Res("ident")
        P.dma("pool", self.ident[:], self.dram["ident"][:, :], writes=[self.R_ident])
        self.epst = self.alloc(es, "epst", [128, 1], F32)
        self.R_eps = Res("eps")
        P.op("pool", lambda: nc.gpsimd.memset(self.epst[:], EPS), writes=[self.R_eps])

    def rstd_from_ms(self, ms_ap, R_ms, st, R_st):
        nc, P = self.nc, self.P
        P.op("act", lambda: nc.scalar.activation(out=st[:, 0:1], in_=ms_ap, func=AF.Sqrt, bias=self.epst[:, 0:1], scale=1.0),
             reads=[R_ms, self.R_eps], writes=[R_st])
        P.op("dve", lambda: nc.vector.reciprocal(out=st[:, 0:1], in_=st[:, 0:1]), reads=[R_st], writes=[R_st])

    def norm_tile(self, src_ap, src_res, xrot, hrot, strot, junk, R_junk, gbc, R_g):
        nc, P = self.nc, self.P
        x, R_x = xrot.next()
        h, R_h = hrot.next()
        st, R_st = strot.next()
        P.dma("sp", x[:], src_ap, reads=[src_res], writes=[R_x])
        P.op("act", lambda: nc.scalar.activation(out=junk[:], in_=x[:], func=AF.Square, scale=RS, accum_out=st[:, 1:2]),
             reads=[R_x], writes=[R_st])
        self.rstd_from_ms(st[:, 1:2], R_st, st, R_st)
        P.op("dve", lambda: nc.vector.scalar_tensor_tensor(out=h[:], in0=x[:], scalar=st[:, 0:1], in1=gbc[:], op0=ALU.mult, op1=ALU.mult),
             reads=[R_x, R_st, R_g], writes=[R_h])
        return h, R_h, x, R_x

    def transpose_tile(self, h, R_h, dst_fn, R_dst, ptrot):
        nc, P = self.nc, self.P
        for cq in range(4):
            pT, R_pT = ptrot.next()
            for j in range(4):
                c = cq * 4 + j
                P.op("pe", lambda c=c, j=j, pT=pT: nc.tensor.transpose(out=pT[:, j, :], in_=h[:, c * 128:(c + 1) * 128], identity=self.ident[:]),
                     reads=[R_h, self.R_ident], writes=[R_pT], inc=(j == 3))
            if cq % 2 == 0:
                P.op("act", lambda cq=cq, pT=pT: nc.scalar.copy(out=dst_fn(cq), in_=pT[:]), reads=[R_pT], writes=[R_dst])
            else:
                P.op("dve", lambda cq=cq, pT=pT: nc.vector.tensor_copy(out=dst_fn(cq), in_=pT[:]), reads=[R_pT], writes=[R_dst])

    def wslab_src(self, wname, c0, c1):
        return self.dram[wname].rearrange("(k p) n -> p k n", p=128)[:, :, c0:c1]

    def phase_qkv(self, g, layer, xsrc, gain_row, wq_name, qoT, R_qo, kT=None, R_kT=None, Vaug=None, R_V=None,
                  xprev=None, wkv_name=None, gate_sb=None, R_gate=None, kmean=None, R_kmean=None):
        nc, P = self.nc, self.P
        with ExitStack() as es:
            xrot = self.bufs(es, "p1x", [128, D], F32, 2)
            hrot = self.bufs(es, "p1h", [128, D], BF16, 2)
            strot = self.bufs(es, "p1st", [128, 2], F32, 4)
            junk = self.alloc(es, "p1junk", [128, D], BF16)
            R_junk = Res("junk")
            hTrot = self.bufs(es, "p1hT", [128, 16, 512], BF16, 2)
            wqrot = self.bufs(es, "p1wq", [128, 16, 512], BF16, 2)
            gbc, R_g = self.load_bc(es, "p1g", self.grow(gain_row))
            ptrot = self.bufs(es, "p1pT", [128, 4, 128], BF16, 2, psum=True)
            pmrot = self.bufs(es, "p1pm", [128, 512], F32, 3, psum=True)
            if layer == 0:
                wkv = self.alloc(es, "p1wkv", [128, 16, 512], BF16)
                R_wkv = Res("wkv")
                P.dma("pool", wkv[:], self.wslab_src(wkv_name, 0, 512), writes=[R_wkv])
                P.op("pool", lambda: nc.gpsimd.memset(Vaug[:, :, :, 64:65], 1.0), writes=R_V)
            else:
                qfrot = self.bufs(es, "p1qf", [128, 512], F32, 2)
                pg = self.palloc(es, "p1pg", [128, 4, 2, 16], F32)
                R_pg = Res("pg", excl=True)

            blocks = []
            if layer == 0:
                blocks += [("prev", 0), ("prev", 1)]
            blocks += [("mine", 0), ("mine", 1)]
            slab_buf = {}

            def issue_slab(n):
                if n >= 4:
                    return
                w, R_w = wqrot.next()
                P.dma("pool", w[:], self.wslab_src(wq_name, n * 512, (n + 1) * 512), writes=[R_w])
                slab_buf[n] = (w, R_w)
            issue_slab(0)
            issue_slab(1)
            evac_i = 0
            mine_blocks = []
            for (kind, b) in blocks:
                hT, R_hT = hTrot.next()
                for t in range(4):
                    i = g * GT + b * 4 + t
                    if kind == "prev":
                        src, sres = xprev[i, :, :], self.dr("xprev", i)
                    else:
                        src, sres = self.dram[xsrc][i, :, :], self.dr(xsrc, i)
                    h, R_h, _, _ = self.norm_tile(src, sres, xrot, hrot, strot, junk, R_junk, gbc, R_g)
                    self.transpose_tile(h, R_h, lambda cq, hT=hT, t=t: hT[:, cq * 4:(cq + 1) * 4, t * 128:(t + 1) * 128], R_hT, ptrot)
                if layer == 0:
                    col0 = (0 if kind == "prev" else GTOK) + b * 512
                    vt0 = (0 if kind == "prev" else GT) + b * 4
                    for kc in range(2):
                        pm, R_pm = pmrot.next()
                        for k in range(16):
                            P.op("pe", lambda k=k, kc=kc, pm=pm, hT=hT: nc.tensor.matmul(pm[:], lhsT=wkv[:, k, kc * 128:(kc + 1) * 128], rhs=hT[:, k, :], start=(k == 0), stop=(k == 15)),
                                 reads=[R_wkv, R_hT], writes=[R_pm], inc=(k == 15))
                        P.op("act", lambda kc=kc, pm=pm, col0=col0: nc.scalar.copy(out=kT[:, kc, col0:col0 + 512], in_=pm[:]),
                             reads=[R_pm], writes=[R_kT[(col0 // 128) + j] for j in range(4)])
                    for t in range(4):
                        pm, R_pm = pmrot.next()
                        for k in range(16):
                            P.op("pe", lambda k=k, t=t, pm=pm, hT=hT: nc.tensor.matmul(pm[:, 0:256], lhsT=hT[:, k, t * 128:(t + 1) * 128], rhs=wkv[:, k, 256:512], start=(k == 0), stop=(k == 15)),
                                 reads=[R_wkv, R_hT], writes=[R_pm], inc=(k == 15))
                        P.op("dve", lambda t=t, pm=pm, vt0=vt0: nc.vector.tensor_copy(out=Vaug[:, vt0 + t, :, 0:64], in_=pm[:, 0:256].rearrange("p (a b) -> p a b", a=4)),
                             reads=[R_pm], writes=[R_V[vt0 + t]])
                if kind == "mine":
                    mine_blocks.append((hT, R_hT, b))
            for sq in range(4):
                w, R_w = slab_buf.pop(sq)
                for (hT, R_hT, b) in mine_blocks:
                    for cc in range(4):
                        c = sq * 4 + cc
                        pm, R_pm = pmrot.next()
                        for k in range(16):
                            P.op("pe", lambda k=k, cc=cc, pm=pm, w=w, hT=hT: nc.tensor.matmul(pm[:], lhsT=w[:, k, cc * 128:(cc + 1) * 128], rhs=hT[:, k, :], start=(k == 0), stop=(k == 15)),
                                 reads=[R_w, R_hT], writes=[R_pm], inc=(k == 15))
                        dst = qoT[:, c, b * 512:(b + 1) * 512]
                        wr = [R_qo[b * 4 + j] for j in range(4)]
                        if layer == 0:
                            if evac_i % 2 == 0:
                                P.op("act", lambda pm=pm, dst=dst: nc.scalar.copy(out=dst, in_=pm[:]), reads=[R_pm], writes=wr)
                            else:
                                P.op("dve", lambda pm=pm, dst=dst: nc.vector.tensor_copy(out=dst, in_=pm[:]), reads=[R_pm], writes=wr)
                            evac_i += 1
                        else:
                            P.op("act", lambda pm=pm, dst=dst: nc.scalar.copy(out=dst, in_=pm[:]), reads=[R_pm], writes=wr)
                            qf, R_qf = qfrot.next()
                            P.op("dve", lambda pm=pm, qf=qf: nc.vector.tensor_copy(out=qf[:], in_=pm[:]), reads=[R_pm], writes=[R_qf])
                            kc = c // 8
                            n = 0
                            for t in range(4):
                                for u in range(2):
                                    P.op("pe", lambda t=t, u=u, qf=qf, kc=kc: nc.tensor.matmul(pg[:, t, u, :], lhsT=qf[u * 64:(u + 1) * 64, t * 128:(t + 1) * 128],
                                                                                              rhs=kmean[u * 64:(u + 1) * 64, kc, :], start=True, stop=True),
                                         reads=[R_qf, R_kmean], writes=[R_pg], inc=(n == 7))
                                    n += 1
                            P.op("dve", lambda c=c, b=b: nc.vector.tensor_copy(out=gate_sb[:, b * 4:(b + 1) * 4, 2 * c:2 * c + 2, :], in_=pg[:]),
                                 reads=[R_pg], writes=[R_gate])
                issue_slab(sq + 2)
        P.barrier()

    def finish_tile(self, o_t, R_o, qoT, R_qo_t, tl, ptrot):
        self.transpose_tile(o_t, R_o, lambda cq: qoT[:, cq * 4:(cq + 1) * 4, tl * 128:(tl + 1) * 128], R_qo_t, ptrot)

    def score_block(self, pS, R_pS, kT, R_k0, R_k1, kcol0, kcol1, kc, u, qoT, R_q, c0, tl, tab4, R_tab, pte, R_pte, ptm, R_ptm, mul_eng="dve"):
        nc, P = self.nc, self.P
        for bb, (kcol, R_k) in enumerate(((kcol0, R_k0), (kcol1, R_k1))):
            P.op("pe", lambda bb=bb, kcol=kcol: nc.tensor.matmul(pS[:, bb, :], lhsT=kT[u * 64:(u + 1) * 64, kc, kcol:kcol + 128],
                                                           rhs=qoT[u * 64:(u + 1) * 64, c0:c0 + 4, tl * 128:(tl + 1) * 128], start=True, stop=True),
                 reads=[R_k, R_q], writes=[R_pS], inc=(bb == 1))
        P.op("act", lambda: nc.scalar.activation(out=pte[:], in_=pS[:], func=AF.Exp, scale=0.125), reads=[R_pS], writes=[R_pte])
        meng = nc.vector if mul_eng == "dve" else nc.gpsimd
        P.op(mul_eng, lambda: meng.tensor_tensor(out=ptm[:].rearrange("p b (a t) -> p b a t", a=4), in0=pte[:].rearrange("p b (a t) -> p b a t", a=4),
                                                 in1=tab4, op=ALU.mult), reads=[R_pte, R_tab], writes=[R_ptm])

    def pv_block(self, pO, R_pO, ptm, R_ptm, Vaug, vt0, vt1, R_v0, R_v1, grp):
        nc, P = self.nc, self.P
        n = 0
        for hh in range(4):
            for bb, vt in enumerate((vt0, vt1)):
                P.op("pe", lambda hh=hh, bb=bb, vt=vt: nc.tensor.matmul(pO[:, hh, 0:65], lhsT=ptm[:, bb, hh * 128:(hh + 1) * 128], rhs=Vaug[:, vt, grp, 0:65],
                                                                    start=(bb == 0), stop=(bb == 1)),
                     reads=[R_ptm, R_v0, R_v1], writes=[R_pO], inc=(n == 7))
                n += 1

    def load_tab(self, es, name, dname):
        t = self.alloc(es, name, [128, 2, 16, 2, 128], BF16)
        r = Res(name)
        self.P.dma("pool", t[:].rearrange("p b c u t -> p b (c u t)"), self.dram[dname][:, :, :], writes=[r])
        return t, r

    def run_pipelined(self, units):
        if not units:
            return
        units[0][0]()
        for n in range(len(units)):
            if n + 1 < len(units):
                units[n + 1][0]()
            units[n][1]()

    def phase_swa(self, g, qoT, R_qo, kT, R_kT, Vaug, R_V, special=None):
        nc, P = self.nc, self.P
        special = special or {0: "tab_swa0"}
        with ExitStack() as es:
            tab, R_tab = self.load_tab(es, "swtab", "tab_swa")
            sp_tabs = {}
            for ti, nm in special.items():
                if g * GT <= ti < (g + 1) * GT:
                    sp_tabs[ti] = self.load_tab(es, "swtab0", nm)
            snk, R_snk = self.load_bc(es, "snk", self.dram["sinks"][0:1, :])
            P.op("act", lambda: nc.scalar.activation(out=snk[:], in_=snk[:], func=AF.Exp), reads=[R_snk], writes=[R_snk])
            snk_v = snk[:].rearrange("p (c u) -> p c u", u=2)
            pSrot = self.bufs(es, "swS", [128, 2, 512], F32, 2, psum=True)
            pOrot = self.bufs(es, "swO", [128, 4, VW], F32, 2, psum=True)
            ptrot = self.bufs(es, "swpT", [128, 4, 128], BF16, 2, psum=True)
            pterot = self.bufs(es, "swpte", [128, 2, 512], BF16, 3)
            ptmrot = self.bufs(es, "swptm", [128, 2, 512], BF16, 3)
            orot = self.bufs(es, "swo", [128, D], BF16, 2)
            denrot = self.bufs(es, "swden", [128, 8], F32, 4)
            units = []
            for tl in range(GT):
                i = g * GT + tl
                tb, R_tb = sp_tabs.get(i, (tab, R_tab))
                tctx = {}
                batches = [(kc, u, c0) for kc in range(2) for u in range(2) for c0 in (8 * kc, 8 * kc + 4)]
                for bi, (kc, u, c0) in enumerate(batches):
                    st = {}

                    def score(tl=tl, kc=kc, u=u, c0=c0, st=st, tctx=tctx, first=(bi == 0), tb=tb, R_tb=R_tb):
                        if first:
                            tctx["o"] = orot.next()
                        pS, R_pS = pSrot.next()
                        pte, R_pte = pterot.next()
                        st["ptm"] = ptmrot.next()
                        self.score_block(pS, R_pS, kT, R_kT[tl], R_kT[GT + tl], tl * 128, GTOK + tl * 128, kc, u, qoT, R_qo[tl], c0, tl,
                                         tb[:, :, c0:c0 + 4, u, :], R_tb, pte, R_pte, st["ptm"][0], st["ptm"][1])

                    def post(tl=tl, kc=kc, u=u, c0=c0, st=st, tctx=tctx, last=(bi == len(batches) - 1)):
                        grp = 2 * kc + u
                        ptm, R_ptm = st["ptm"]
                        o_t, R_o = tctx["o"]
                        o_v = o_t[:].rearrange("p (c u d) -> p c u d", u=2, d=64)
                        pO, R_pO = pOrot.next()
                        self.pv_block(pO, R_pO, ptm, R_ptm, Vaug, tl, GT + tl, R_V[tl], R_V[GT + tl], grp)
                        den, R_den = denrot.next()
                        P.op("dve", lambda: nc.vector.tensor_tensor(out=den[:, 0:4], in0=pO[:, :, 64], in1=snk_v[:, c0:c0 + 4, u], op=ALU.add),
                             reads=[R_pO, R_snk], writes=[R_den])
                        P.op("dve", lambda: nc.vector.reciprocal(out=den[:, 4:8], in_=den[:, 0:4]), reads=[R_den], writes=[R_den])
                        P.op("dve", lambda: nc.vector.tensor_tensor(out=o_v[:, c0:c0 + 4, u, :], in0=pO[:, :, 0:64],
                                                                    in1=den[:, 4:8].unsqueeze(2).to_broadcast([128, 4, 64]), op=ALU.mult),
                             reads=[R_pO, R_den], writes=[R_o])
                        if last:
                            self.finish_tile(o_t, R_o, qoT, R_qo[tl], tl, ptrot)
                    units.append((score, post))
            self.run_pipelined(units)
        P.barrier()

    def moba_select(self, tl, i, gate_sb, R_gate, w_t, R_w, ft_v, R_ft, gw, eq, mx, R_sel):
        nc, P = self.nc, self.P
        npast = i
        if npast < 1:
            return
        gv = gate_sb[:, tl, :, 0:npast]
        fsl = ft_v[:, :, 15 - npast:15]
        wv = w_t[:, :, 0:npast]
        if npast <= 3:
            P.op("dve", lambda: nc.vector.tensor_copy(out=wv, in_=fsl), reads=[R_ft], writes=[R_w])
            return
        gwv = gw[:, :, 0:npast]
        eqv = eq[:, :, 0:npast]
        mb = mx[:].unsqueeze(2).to_broadcast([128, NH, npast])
        rs = [R_gate, R_sel]
        P.op("dve", lambda: nc.vector.tensor_reduce(out=mx[:], in_=gv, axis=AX.X, op=ALU.max), reads=rs, writes=[R_sel])
        P.op("dve", lambda: nc.vector.tensor_tensor(out=eqv, in0=gv, in1=mb, op=ALU.is_ge), reads=rs, writes=[R_sel])
        P.op("dve", lambda: nc.vector.scalar_tensor_tensor(out=gwv, in0=eqv, scalar=NEG, in1=gv, op0=ALU.mult, op1=ALU.add), reads=rs, writes=[R_sel])
        P.op("dve", lambda: nc.vector.tensor_reduce(out=mx[:], in_=gwv, axis=AX.X, op=ALU.max), reads=rs, writes=[R_sel])
        P.op("dve", lambda: nc.vector.tensor_tensor(out=eqv, in0=gwv, in1=mb, op=ALU.is_ge), reads=rs, writes=[R_sel])
        P.op("dve", lambda: nc.vector.scalar_tensor_tensor(out=gwv, in0=eqv, scalar=NEG, in1=gwv, op0=ALU.mult, op1=ALU.add), reads=rs, writes=[R_sel])
        P.op("dve", lambda: nc.vector.tensor_reduce(out=mx[:], in_=gwv, axis=AX.X, op=ALU.max), reads=rs, writes=[R_sel])
        P.op("dve", lambda: nc.vector.tensor_tensor(out=eqv, in0=gv, in1=mb, op=ALU.is_ge), reads=rs, writes=[R_sel])
        P.op("dve", lambda: nc.vector.tensor_tensor(out=wv, in0=eqv, in1=fsl, op=ALU.mult), reads=[R_sel, R_ft], writes=[R_w])

    def phase_moba(self, g, qoT, R_qo, kTa, R_kTa, Va, R_Va, gate_sb, R_gate):
        nc, P = self.nc, self.P
        with ExitStack() as es:
            tabF, R_tabF = self.load_tab(es, "mbF", "tab_full_l")
            tabO, R_tabO = self.load_tab(es, "mbO", "tab_own_l")
            ft, R_ft = self.load_bc(es, "mbft", self.dram["ftab"][0:1, :])
            ft_v = ft[:].rearrange("p (h k) -> p h k", k=15)
            pSrot = self.bufs(es, "mbS", [128, 2, 512], F32, 2, psum=True)
            pOrot = self.bufs(es, "mbOp", [128, 4, VW], F32, 2, psum=True)
            ptrot = self.bufs(es, "mbpT", [128, 4, 128], BF16, 2, psum=True)
            pterot = self.bufs(es, "mbpte", [128, 2, 512], BF16, 3)
            ptmrot = self.bufs(es, "mbptm", [128, 2, 512], BF16, 3)
            orot = self.bufs(es, "mbo", [128, D], BF16, 2)
            accrot = self.bufs(es, "mbacc", [128, 4, VW], F32, 3)
            denrot = self.bufs(es, "mbden", [128, 4], F32, 3)
            wrot = self.bufs(es, "mbw", [128, NH, 15], F32, 2)
            gw = self.alloc(es, "mbgw", [128, NH, 15], F32)
            eq = self.alloc(es, "mbeq", [128, NH, 15], F32)
            mx = self.alloc(es, "mbmx", [128, NH], F32)
            R_sel = Res("selwork")
            units = []
            for tl in range(GT):
                i = g * GT + tl
                npast = i
                tctx = {}
                batches = [(kc, u, c0) for kc in range(2) for u in range(2) for c0 in (8 * kc, 8 * kc + 4)]
                for bi, (kc, u, c0) in enumerate(batches):
                    bctx = {}
                    blocks = [i] + list(range(npast))
                    for ji, j in enumerate(blocks):
                        st = {}

                        def score(tl=tl, i=i, kc=kc, u=u, c0=c0, j=j, st=st, tctx=tctx, bctx=bctx,
                                  first_tile=(bi == 0 and ji == 0), first_batch=(ji == 0), mul_eng=("pool" if len(units) % 2 else "dve")):
                            if first_tile:
                                tctx["o"] = orot.next()
                                tctx["w"] = wrot.next()
                                self.moba_select(tl, i, gate_sb, R_gate, tctx["w"][0], tctx["w"][1], ft_v, R_ft, gw, eq, mx, R_sel)
                            if first_batch:
                                bctx["acc"] = accrot.next()
                            own = (j == i)
                            tb, R_tb = (tabO, R_tabO) if own else (tabF, R_tabF)
                            pS, R_pS = pSrot.next()
                            pte, R_pte = pterot.next()
                            st["ptm"] = ptmrot.next()
                            self.score_block(pS, R_pS, kTa, R_kTa[j], R_kTa[16 + j], j * 128, (16 + j) * 128, kc, u, qoT, R_qo[tl], c0, tl,
                                             tb[:, :, c0:c0 + 4, u, :], R_tb, pte, R_pte, st["ptm"][0], st["ptm"][1], mul_eng=mul_eng)

                        def post(tl=tl, i=i, kc=kc, u=u, c0=c0, j=j, st=st, tctx=tctx, bctx=bctx,
                                 last_batch=(ji == len(blocks) - 1), last_tile=(bi == len(batches) - 1 and ji == len(blocks) - 1)):
                            grp = 2 * kc + u
                            ptm, R_ptm = st["ptm"]
                            o_t, R_o = tctx["o"]
                            w_t, R_w = tctx["w"]
                            acc, R_acc = bctx["acc"]
                            o_v = o_t[:].rearrange("p (c u d) -> p c u d", u=2, d=64)
                            pO, R_pO = pOrot.next()
                            self.pv_block(pO, R_pO, ptm, R_ptm, Va, j, 16 + j, R_Va[j], R_Va[16 + j], grp)
                            if j == i:
                                P.op("act", lambda: nc.scalar.copy(out=acc[:, :, 0:65], in_=pO[:, :, 0:65]), reads=[R_pO], writes=[R_acc])
                            else:
                                for hh in range(4):
                                    hi = (c0 + hh) * 2 + u
                                    P.op("dve", lambda hh=hh, hi=hi: nc.vector.scalar_tensor_tensor(
                                        out=acc[:, hh, 0:65], in0=pO[:, hh, 0:65], scalar=w_t[:, hi, j:j + 1], in1=acc[:, hh, 0:65], op0=ALU.mult, op1=ALU.add),
                                        reads=[R_pO, R_w, R_acc], writes=[R_acc])
                            if last_batch:
                                den, R_den = denrot.next()
                                P.op("dve", lambda: nc.vector.reciprocal(out=den[:], in_=acc[:, :, 64]), reads=[R_acc], writes=[R_den])
                                P.op("dve", lambda: nc.vector.tensor_tensor(out=o_v[:, c0:c0 + 4, u, :], in0=acc[:, :, 0:64],
                                                                            in1=den[:].unsqueeze(2).to_broadcast([128, 4, 64]), op=ALU.mult),
                                     reads=[R_acc, R_den], writes=[R_o])
                            if last_tile:
                                self.finish_tile(o_t, R_o, qoT, R_qo[tl], tl, ptrot)
                        units.append((score, post))
            self.run_pipelined(units)
        P.barrier()

    def phase_oproj(self, g, qoT, R_qo, wo_name, gain_row, xsrc, xdst):
        nc, P = self.nc, self.P
        with ExitStack() as es:
            wo = self.alloc(es, "p3wo", [128, 16, D], BF16)
            R_wo = [Res(f"wo{n}") for n in range(4)]
            for nb in range(4):
                P.dma("pool", wo[:, :, nb * 512:(nb + 1) * 512], self.wslab_src(wo_name, nb * 512, (nb + 1) * 512), writes=[R_wo[nb]])
            gbc, R_g = self.load_bc(es, "p3g", self.grow(gain_row))
            xrot = self.bufs(es, "p3x", [128, D], F32, 2)
            tmprot = self.bufs(es, "p3tmp", [128, D], F32, 2)
            xorot = self.bufs(es, "p3xo", [128, D], F32, 2)
            strot = self.bufs(es, "p3st", [128, 8], F32, 3)
            junk = self.alloc(es, "p3junk", [128, 512], BF16)
            R_junk = Res("junk")
            pmrot = self.bufs(es, "p3pm", [128, 512], F32, 8, psum=True)
            for tl in range(GT):
                i = g * GT + tl
                x, R_x = xrot.next()
                P.dma("sp", x[:], self.dram[xsrc][i, :, :], reads=[self.dr(xsrc, i)], writes=[R_x])
                st, R_st = strot.next()
                pms = []
                for nb in range(4):
                    pm, R_pm = pmrot.next()
                    pms.append((pm, R_pm))
                    for k in range(16):
                        P.op("pe", lambda k=k, nb=nb, pm=pm: nc.tensor.matmul(pm[:], lhsT=qoT[:, k, tl * 128:(tl + 1) * 128], rhs=wo[:, k, nb * 512:(nb + 1) * 512], start=(k == 0), stop=(k == 15)),
                             reads=[R_qo[tl], R_wo[nb]], writes=[R_pm], inc=(k == 15))
                    P.op("act", lambda nb=nb, pm=pm, st=st: nc.scalar.activation(out=junk[:], in_=pm[:], func=AF.Square, scale=RS, accum_out=st[:, 2 + nb:3 + nb]),
                         reads=[R_pm], writes=[R_st])
                P.op("dve", lambda st=st: nc.vector.tensor_reduce(out=st[:, 1:2], in_=st[:, 2:6], axis=AX.X, op=ALU.add), reads=[R_st], writes=[R_st])
                self.rstd_from_ms(st[:, 1:2], R_st, st, R_st)
                tmp, R_tmp = tmprot.next()
                for nb in range(4):
                    pm, R_pm = pms[nb]
                    P.op("dve", lambda nb=nb, pm=pm, st=st, tmp=tmp: nc.vector.scalar_tensor_tensor(out=tmp[:, nb * 512:(nb + 1) * 512], in0=pm[:], scalar=st[:, 0:1],
                                                                                                in1=gbc[:, nb * 512:(nb + 1) * 512], op0=ALU.mult, op1=ALU.mult),
                         reads=[R_pm, R_st, R_g], writes=[R_tmp])
                xo, R_xo = xorot.next()
                P.op("pool", lambda tmp=tmp, x=x, xo=xo: nc.gpsimd.tensor_tensor(out=xo[:], in0=tmp[:], in1=x[:], op=ALU.add), reads=[R_tmp, R_x], writes=[R_xo])
                P.dma("sp", self.dram[xdst][i, :, :], xo[:], reads=[R_xo], writes=[self.dr(xdst, i)])
        P.barrier()

    def phase_mlp(self, g, wup_name, wdn_name, gain_pre, gain_post, xsrc, xdst):
        nc, P = self.nc, self.P
        with ExitStack() as es0:
            h2T = self.alloc(es0, "p4h2T", [128, 16, GTOK], BF16)
            R_h2T = [Res(f"h2T{t}") for t in range(GT)]
            yacc = self.alloc(es0, "p4y", [128, GT, D], F32)
            R_y = [Res(f"y{t}") for t in range(GT)]
            esW = ExitStack()
            wuprot = self.bufs(esW, "p4wu", [128, 16, 512], BF16, 2)
            wdnrot = self.bufs(esW, "p4wd", [128, 4, D], BF16, 2)
            NS = DFF // 512
            wup_b, wdn_b, aT_b = {}, {}, {}
            wdn_src = self.dram[wdn_name].rearrange("(s c p) n -> s p c n", p=128, c=4)

            def load_up(s):
                if s < NS:
                    w, R_w = wuprot.next()
                    P.dma("pool", w[:], self.wslab_src(wup_name, s * 512, (s + 1) * 512), writes=[R_w])
                    wup_b[s] = (w, R_w)

            def load_dn(s):
                if s < NS:
                    w, R_w = wdnrot.next()
                    P.dma("pool", w[:], wdn_src[s], writes=[R_w])
                    wdn_b[s] = (w, R_w)
            load_up(0)
            load_dn(0)
            load_up(1)
            load_dn(1)
            with ExitStack() as es:
                xrot = self.bufs(es, "p4x", [128, D], F32, 2)
                hrot = self.bufs(es, "p4h", [128, D], BF16, 2)
                strot = self.bufs(es, "p4st", [128, 2], F32, 4)
                junk = self.alloc(es, "p4junk", [128, D], BF16)
                R_junk = Res("junk")
                gbc, R_g = self.load_bc(es, "p4g", self.grow(gain_pre))
                ptrot = self.bufs(es, "p4pT", [128, 4, 128], BF16, 2, psum=True)
                for tl in range(GT):
                    i = g * GT + tl
                    h, R_h, _, _ = self.norm_tile(self.dram[xsrc][i, :, :], self.dr(xsrc, i), xrot, hrot, strot, junk, R_junk, gbc, R_g)
                    self.transpose_tile(h, R_h, lambda cq, tl=tl: h2T[:, cq * 4:(cq + 1) * 4, tl * 128:(tl + 1) * 128], R_h2T[tl], ptrot)
            P.barrier()
            with ExitStack() as es:
                aTrot = self.bufs(es, "p4aT", [128, 4, GTOK], BF16, 2)
                rrot = self.bufs(es, "p4r", [128, 512], F32, 3)
                purot = self.bufs(es, "p4pu", [128, 512], F32, 3, psum=True)
                pdrot = self.bufs(es, "p4pd", [128, 512], F32, 4, psum=True)
                def up(s):
                    w, R_w = wup_b.pop(s)
                    aT, R_aT = aTrot.next()
                    aT_b[s] = (aT, R_aT)
                    for cc in range(4):
                        for tb in range(2):
                            pu, R_pu = purot.next()
                            for k in range(16):
                                P.op("pe", lambda k=k, cc=cc, tb=tb, pu=pu: nc.tensor.matmul(pu[:], lhsT=w[:, k, cc * 128:(cc + 1) * 128], rhs=h2T[:, k, tb * 512:(tb + 1) * 512], start=(k == 0), stop=(k == 15)),
                                     reads=[R_w] + R_h2T[tb * 4:(tb + 1) * 4], writes=[R_pu], inc=(k == 15))
                            r, R_r = rrot.next()
                            P.op("act", lambda pu=pu, r=r: nc.scalar.activation(out=r[:], in_=pu[:], func=AF.Relu), reads=[R_pu], writes=[R_r])
                            P.op("dve", lambda r=r, cc=cc, tb=tb, aT=aT: nc.vector.tensor_tensor(out=aT[:, cc, tb * 512:(tb + 1) * 512], in0=r[:], in1=r[:], op=ALU.mult),
                                 reads=[R_r], writes=[R_aT])

                def down(s):
                    w, R_w = wdn_b.pop(s)
                    aT, R_aT = aT_b.pop(s)
                    for tl in range(GT):
                        for nb in range(4):
                            pd, R_pd = pdrot.next()
                            for cc in range(4):
                                P.op("pe", lambda cc=cc, nb=nb, tl=tl, pd=pd: nc.tensor.matmul(pd[:], lhsT=aT[:, cc, tl * 128:(tl + 1) * 128], rhs=w[:, cc, nb * 512:(nb + 1) * 512], start=(cc == 0), stop=(cc == 3)),
                                     reads=[R_aT, R_w], writes=[R_pd], inc=(cc == 3))
                            ydst = yacc[:, tl, nb * 512:(nb + 1) * 512]
                            if s == 0:
                                P.op("act", lambda pd=pd, ydst=ydst: nc.scalar.copy(out=ydst, in_=pd[:]), reads=[R_pd], writes=[R_y[tl]])
                            else:
                                P.op("dve", lambda pd=pd, ydst=ydst: nc.vector.tensor_tensor(out=ydst, in0=pd[:], in1=ydst, op=ALU.add), reads=[R_pd, R_y[tl]], writes=[R_y[tl]])

                up(0)
                for s in range(NS):
                    if s + 1 < NS:
                        up(s + 1)
                    load_up(s + 2)
                    down(s)
                    load_dn(s + 2)
            esW.close()
            P.barrier()
            with ExitStack() as es:
                xrot = self.bufs(es, "p4x2", [128, D], F32, 2)
                tmprot = self.bufs(es, "p4tmp", [128, D], F32, 2)
                xorot = self.bufs(es, "p4xo", [128, D], F32, 2)
                strot = self.bufs(es, "p4st2", [128, 2], F32, 4)
                junk = self.alloc(es, "p4junk2", [128, D], BF16)
                R_junk = Res("junk")
                gbc, R_g = self.load_bc(es, "p4g2", self.grow(gain_post))
                for tl in range(GT):
                    i = g * GT + tl
                    x, R_x = xrot.next()
                    P.dma("sp", x[:], self.dram[xsrc][i, :, :], reads=[self.dr(xsrc, i)], writes=[R_x])
                    st, R_st = strot.next()
                    P.op("act", lambda tl=tl, st=st: nc.scalar.activation(out=junk[:], in_=yacc[:, tl, :], func=AF.Square, scale=RS, accum_out=st[:, 1:2]),
                         reads=[R_y[tl]], writes=[R_st])
                    self.rstd_from_ms(st[:, 1:2], R_st, st, R_st)
                    tmp, R_tmp = tmprot.next()
                    P.op("dve", lambda tl=tl, st=st, tmp=tmp: nc.vector.scalar_tensor_tensor(out=tmp[:], in0=yacc[:, tl, :], scalar=st[:, 0:1], in1=gbc[:], op0=ALU.mult, op1=ALU.mult),
                         reads=[R_y[tl], R_st, R_g], writes=[R_tmp])
                    xo, R_xo = xorot.next()
                    P.op("pool", lambda tmp=tmp, x=x, xo=xo: nc.gpsimd.tensor_tensor(out=xo[:], in0=tmp[:], in1=x[:], op=ALU.add), reads=[R_tmp, R_x], writes=[R_xo])
                    P.dma("sp", self.dram[xdst][i, :, :], xo[:], reads=[R_xo], writes=[self.dr(xdst, i)])
        P.barrier()

    def phase_kvshared(self, xsrc, kdst, vdst, ksdst, R_dst, ntiles=NT):
        nc, P = self.nc, self.P
        with ExitStack() as es:
            xrot = self.bufs(es, "p5x", [128, D], F32, 2)
            hrot = self.bufs(es, "p5h", [128, D], BF16, 2)
            strot = self.bufs(es, "p5st", [128, 2], F32, 4)
            junk = self.alloc(es, "p5junk", [128, D], BF16)
            R_junk = Res("junk")
            hTrot = self.bufs(es, "p5hT", [128, 16, 512], BF16, 2)
            gbc, R_g = self.load_bc(es, "p5g", self.grow(4))
            ptrot = self.bufs(es, "p5pT", [128, 4, 128], BF16, 2, psum=True)
            pmrot = self.bufs(es, "p5pm", [128, 512], F32, 3, psum=True)
            wkv = self.alloc(es, "p5wkv", [128, 16, 512], BF16)
            R_wkv = Res("wkvs")
            P.dma("pool", wkv[:], self.wslab_src("wkvs", 0, 512), writes=[R_wkv])
            ksum = self.alloc(es, "p5ksum", [128, 2, ntiles], F32)
            R_ksum = Res("ksum")
            kstrot = self.bufs(es, "p5kst", [128, 512], F32, 2)
            vstrot = self.bufs(es, "p5vst", [128, 256], F32, 2)
            for b in range(ntiles // 4):
                hT, R_hT = hTrot.next()
                for t in range(4):
                    i = b * 4 + t
                    h, R_h, _, _ = self.norm_tile(self.dram[xsrc][i, :, :], self.dr(xsrc, i), xrot, hrot, strot, junk, R_junk, gbc, R_g)
                    self.transpose_tile(h, R_h, lambda cq, hT=hT, t=t: hT[:, cq * 4:(cq + 1) * 4, t * 128:(t + 1) * 128], R_hT, ptrot)
                for kc in range(2):
                    pm, R_pm = pmrot.next()
                    for k in range(16):
                        P.op("pe", lambda k=k, kc=kc, pm=pm, hT=hT: nc.tensor.matmul(pm[:], lhsT=wkv[:, k, kc * 128:(kc + 1) * 128], rhs=hT[:, k, :], start=(k == 0), stop=(k == 15)),
                             reads=[R_wkv, R_hT], writes=[R_pm], inc=(k == 15))
                    kst, R_kst = kstrot.next()
                    P.op("act", lambda pm=pm, kst=kst: nc.scalar.copy(out=kst[:], in_=pm[:]), reads=[R_pm], writes=[R_kst])
                    P.op("dve", lambda pm=pm, kc=kc, b=b: nc.vector.tensor_reduce(out=ksum[:, kc, b * 4:(b + 1) * 4], in_=pm[:].rearrange("p (a t) -> p a t", a=4), axis=AX.X, op=ALU.add),
                         reads=[R_pm, R_ksum], writes=[R_ksum])
                    P.dma("sp", kdst(kc, b), kst[:], reads=[R_kst], writes=[R_dst])
                for t in range(4):
                    i = b * 4 + t
                    pm, R_pm = pmrot.next()
                    for k in range(16):
                        P.op("pe", lambda k=k, t=t, pm=pm, hT=hT: nc.tensor.matmul(pm[:, 0:256], lhsT=hT[:, k, t * 128:(t + 1) * 128], rhs=wkv[:, k, 256:512], start=(k == 0), stop=(k == 15)),
                             reads=[R_wkv, R_hT], writes=[R_pm], inc=(k == 15))
                    vst, R_vst = vstrot.next()
                    P.op("dve", lambda pm=pm, vst=vst: nc.vector.tensor_copy(out=vst[:], in_=pm[:, 0:256]), reads=[R_pm], writes=[R_vst])
                    P.dma("sp", vdst(i), vst[:], reads=[R_vst], writes=[R_dst])
            P.dma("sp", ksdst, ksum[:], reads=[R_ksum], writes=[R_dst])
        P.barrier()

    def layer0(self, xin, x1, x2, ngroups=NG, special=None):
        nc, P = self.nc, self.P
        for g in range(ngroups):
            with ExitStack() as es:
                qoT = self.alloc(es, "qoT", [128, 16, GTOK], BF16)
                R_qo = [Res(f"qo{t}") for t in range(GT)]
                kT = self.alloc(es, "kT", [128, 2, 2 * GTOK], BF16)
                R_kT = [Res(f"kT{t}") for t in range(2 * GT)]
                Vaug = self.alloc(es, "Vaug", [128, 2 * GT, 4, VW], BF16)
                R_V = [Res(f"V{t}") for t in range(2 * GT)]
                self.phase_qkv(g, 0, xin, 0, "wq0", qoT, R_qo, kT=kT, R_kT=R_kT, Vaug=Vaug, R_V=R_V, xprev=self.dram["xprev"], wkv_name="wkv0")
                self.phase_swa(g, qoT, R_qo, kT, R_kT, Vaug, R_V, special=special)
                self.phase_oproj(g, qoT, R_qo, "wo0", 1, xin, x1)
            P.barrier()
            self.phase_mlp(g, "wup0", "wdn0", 2, 3, x1, x2)

    def load_kv_host(self, kt_src, v_src, ksa, ksb):
        def f(kTa, R_kTa, Va, R_Va, kmean, R_kmean, ksb_t, R_ksb):
            P = self.P
            P.dma("pool", kTa[:], kt_src, writes=R_kTa)
            v4 = v_src.rearrange("p t (g d) -> p t g d", g=4)
            for q4 in range(4):
                P.dma("pool", Va[:, q4 * 8:(q4 + 1) * 8, :, 0:64], v4[:, q4 * 8:(q4 + 1) * 8, :, :], writes=R_Va[q4 * 8:(q4 + 1) * 8])
            P.dma("sp", kmean[:], ksa, writes=[R_kmean])
            P.dma("sp", ksb_t[:], ksb, writes=[R_ksb])
        return f

    def load_kv_gathered(self, ex, R_ex):
        def f(kTa, R_kTa, Va, R_Va, kmean, R_kmean, ksb_t, R_ksb):
            P = self.P
            kv = kTa[:].rearrange("p c (i r t) -> p c i r t", r=2, t=128)
            vv = Va[:].rearrange("p (i r) g w -> p i r g w", r=2)
            for r in range(2):
                rows = ex[r * 128:(r + 1) * 128, :]
                P.dma("pool", kv[:, :, :, r, :], rows[:, 0:4096].rearrange("p (c i t) -> p c i t", c=2, i=16), reads=[R_ex], writes=R_kTa)
                vsrc = rows[:, 4096:8192].rearrange("p (i g d) -> p i g d", g=4, d=64)
                for gq in range(4):
                    P.dma("pool", vv[:, :, r, gq, 0:64], vsrc[:, :, gq, :], reads=[R_ex], writes=R_Va)
            P.dma("sp", kmean[:], ex[0:128, 8192:8224].rearrange("p (c j) -> p c j", c=2), reads=[R_ex], writes=[R_kmean])
            P.dma("sp", ksb_t[:], ex[128:256, 8192:8224].rearrange("p (c j) -> p c j", c=2), reads=[R_ex], writes=[R_ksb])
        return f

    def layer1(self, xin, x3, xout, loader):
        nc, P = self.nc, self.P
        for g in range(NG):
            with ExitStack() as es:
                kTa = self.alloc(es, "kTa", [128, 2, 32 * 128], BF16)
                R_kTa = [Res(f"kTa{t}") for t in range(32)]
                Va = self.alloc(es, "Va", [128, 32, 4, VW], BF16)
                R_Va = [Res(f"Va{t}") for t in range(32)]
                kmean = self.alloc(es, "kmean", [128, 2, 16], F32)
                R_kmean = Res("kmean")
                ksb_t = self.alloc(es, "ksb_t", [128, 2, 16], F32)
                R_ksb = Res("ksb")
                loader(kTa, R_kTa, Va, R_Va, kmean, R_kmean, ksb_t, R_ksb)
                P.op("pool", lambda: nc.gpsimd.memset(Va[:, :, :, 64:65], 1.0), reads=R_Va, writes=R_Va)
                P.op("dve", lambda: nc.vector.tensor_tensor(out=kmean[:], in0=kmean[:], in1=ksb_t[:], op=ALU.add), reads=[R_kmean, R_ksb], writes=[R_kmean])
                P.op("dve", lambda: nc.vector.tensor_scalar(out=kmean[:], in0=kmean[:], scalar1=1.0 / 256.0, scalar2=None, op0=ALU.mult), reads=[R_kmean], writes=[R_kmean])
                qoT = self.alloc(es, "qoT1", [128, 16, GTOK], BF16)
                R_qo = [Res(f"qo{t}") for t in range(GT)]
                gate_sb = self.alloc(es, "gate", [128, GT, NH, 16], F32)
                R_gate = Res("gate")
                self.phase_qkv(g, 1, xin, 0, "wq1", qoT, R_qo, gate_sb=gate_sb, R_gate=R_gate, kmean=kmean, R_kmean=R_kmean)
                self.phase_moba(g, qoT, R_qo, kTa, R_kTa, Va, R_Va, gate_sb, R_gate)
                self.phase_oproj(g, qoT, R_qo, "wo1", 1, xin, x3)
            P.barrier()
            self.phase_mlp(g, "wup1", "wdn1", 2, 3, x3, xout)
        P.barrier()


def build_stage1():
    nc = bass.Bass("TRN2", target_bir_lowering=False)
    B = Builder(nc)
    B.din("xmine", [NT, 128, D])
    B.din("xprev", [NT, 128, D])
    B.din("wq0", [D, D])
    B.din("wkv0", [D, 512])
    B.din("wo0", [D, D])
    B.din("wup0", [D, DFF])
    B.din("wdn0", [DFF, D])
    B.din("wkvs", [D, 512])
    B.din("gains", [5, D])
    B.din("sinks", [1, NH])
    B.din("ident", [128, 128])
    B.din("tab_swa", [128, 2, NH * 128])
    B.din("tab_swa0", [128, 2, NH * 128])
    B.dtmp("x1", [NT, 128, D])
    B.dout("x2", [NT, 128, D])
    B.dout("kts", [128, 2, NT * 128])
    B.dout("vs", [128, NT, 256])
    B.dout("ksum", [128, 2, NT])
    with ExitStack() as es:
        B.setup_consts(es)
        B.layer0("xmine", "x1", "x2")
        B.phase_kvshared("x2", lambda kc, b: B.dram["kts"][:, kc, b * 512:(b + 1) * 512], lambda i: B.dram["vs"][:, i, :],
                         B.dram["ksum"][:, :, :], Res("kvout"))
        B.P.barrier()
    return nc


def build_stage2():
    nc = bass.Bass("TRN2", target_bir_lowering=False)
    B = Builder(nc)
    B.din("x2", [NT, 128, D])
    B.din("kt_all", [128, 2, 32 * 128])
    B.din("v_all", [128, 32, 256])
    B.din("ksa", [128, 2, 16])
    B.din("ksb", [128, 2, 16])
    B.din("wq1", [D, D])
    B.din("wo1", [D, D])
    B.din("wup1", [D, DFF])
    B.din("wdn1", [DFF, D])
    B.din("gains", [4, D])
    B.din("ident", [128, 128])
    B.din("tab_full", [128, 2, NH * 128])
    B.din("tab_own", [128, 2, NH * 128])
    B.din("ftab", [1, NH * 15])
    B.dtmp("x3", [NT, 128, D])
    B.dout("out", [NT, 128, D])
    with ExitStack() as es:
        B.setup_consts(es)
        B.layer1("x2", "x3", "out", B.load_kv_host(B.dram["kt_all"][:, :, :], B.dram["v_all"][:, :, :], B.dram["ksa"][:, :, :], B.dram["ksb"][:, :, :]))
        B.P.barrier()
    return nc


def core_tiles(x, b, p):
    xb = x[b].reshape(32, 128, D)
    mine = np.ascontiguousarray(xb[p::2])
    prev = np.zeros_like(mine)
    for i in range(NT):
        gt = 2 * i + p - 1
        if gt >= 0:
            prev[i] = xb[gt]
    return mine, prev


def stage1_inputs(inputs):
    perm = q_perm()
    wqkv = inputs["w_qkv_a"][0]
    common = {
        "wq0": np.ascontiguousarray(wqkv[:, :D][:, perm]),
        "wkv0": np.ascontiguousarray(wqkv[:, D:]),
        "wo0": np.ascontiguousarray(inputs["w_o_a"][0][perm, :]),
        "wup0": np.ascontiguousarray(inputs["w_up"][0]),
        "wdn0": np.ascontiguousarray(inputs["w_down"][0]),
        "wkvs": np.ascontiguousarray(inputs["w_kv_shared"]),
        "gains": np.ascontiguousarray(np.stack([inputs["norm_attn_pre"][0], inputs["norm_attn_post"][0], inputs["norm_mlp_pre"][0],
                                               inputs["norm_mlp_post"][0], inputs["kv_norm"]]).astype(np.float32)),
        "sinks": np.ascontiguousarray(inputs["sinks_a"][0][[head_of(c, u) for c in range(16) for u in range(2)]].reshape(1, NH)),
        "ident": np.eye(128, dtype=np.float32),
    }
    tabs = [build_tables(0), build_tables(1)]
    maps = []
    for c in range(8):
        b, p = divmod(c, 2)
        mine, prev = core_tiles(inputs["x"], b, p)
        m = dict(common)
        m["xmine"] = mine
        m["xprev"] = prev
        m["tab_swa"] = tabs[p]["tab_swa"]
        m["tab_swa0"] = tabs[p]["tab_swa0"]
        maps.append(m)
    return maps


def stage2_inputs(inputs, r1):
    perm = q_perm()
    common = {
        "wq1": np.ascontiguousarray(inputs["w_q_b"][0][:, perm]),
        "wo1": np.ascontiguousarray(inputs["w_o_b"][0][perm, :]),
        "wup1": np.ascontiguousarray(inputs["w_up"][1]),
        "wdn1": np.ascontiguousarray(inputs["w_down"][1]),
        "gains": np.ascontiguousarray(np.stack([inputs["norm_attn_pre"][1], inputs["norm_attn_post"][1], inputs["norm_mlp_pre"][1],
                                               inputs["norm_mlp_post"][1]]).astype(np.float32)),
        "ident": np.eye(128, dtype=np.float32),
    }
    tabs = [build_tables(0), build_tables(1)]
    maps = []
    for c in range(8):
        b, p = divmod(c, 2)
        ra, rb = r1[2 * b], r1[2 * b + 1]
        kt_all = np.zeros((128, 2, 32, 128), dtype=np.float32)
        kt_all[:, :, 0::2, :] = ra["kts"].reshape(128, 2, NT, 128)
        kt_all[:, :, 1::2, :] = rb["kts"].reshape(128, 2, NT, 128)
        v_all = np.zeros((128, 32, 256), dtype=np.float32)
        v_all[:, 0::2, :] = ra["vs"]
        v_all[:, 1::2, :] = rb["vs"]
        m = dict(common)
        m["x2"] = np.ascontiguousarray(r1[c]["x2"])
        m["kt_all"] = kt_all.reshape(128, 2, 32 * 128)
        m["v_all"] = v_all
        m["ksa"] = np.ascontiguousarray(ra["ksum"])
        m["ksb"] = np.ascontiguousarray(rb["ksum"])
        m["tab_full"] = tabs[p]["tab_full"]
        m["tab_own"] = tabs[p]["tab_own"]
        m["ftab"] = tabs[p]["ftab"]
        maps.append(m)
    return maps


def assemble(outs):
    res = np.zeros((4, 32, 128, D), dtype=np.float32)
    for c in range(8):
        b, p = divmod(c, 2)
        res[b, p::2] = outs[c]
    return res.reshape(4, 4096, D)


NT0 = 32


def build_fused(n_cores=8):
    nc = bass.Bass("TRN2", target_bir_lowering=False)
    B = Builder(nc)
    B.din("xall", [NT0, 128, D])
    B.din("xprev", [NT0, 128, D])
    for nm, shp in (("wq0", [D, D]), ("wkv0", [D, 512]), ("wo0", [D, D]), ("wup0", [D, DFF]), ("wdn0", [DFF, D]), ("wkvs", [D, 512]),
                    ("wq1", [D, D]), ("wo1", [D, D]), ("wup1", [D, DFF]), ("wdn1", [DFF, D])):
        B.din(nm, shp)
    B.din("gains", [9, D])
    B.din("sinks", [1, NH])
    B.din("ident", [128, 128])
    for nm in ("tab_swa", "tab_swaA", "tab_swaB", "tab_full_l", "tab_own_l"):
        B.din(nm, [128, 2, NH * 128])
    B.din("ftab", [1, NH * 15])
    B.dtmp("x1", [NT0, 128, D])
    B.dtmp("x2", [NT0, 128, D])
    B.dtmp("x3", [NT, 128, D])
    kts = B.dtmp("kts", [128, 2, NT0 * 128])
    vs = B.dtmp("vs", [128, NT0, 256])
    ksum = B.dtmp("ksum", [128, 2, NT0])
    B.dout("out", [NT, 128, D])
    with ExitStack() as es:
        B.setup_consts(es)
        B.gbase = 0
        B.layer0("xall", "x1", "x2", ngroups=NT0 // GT, special={0: "tab_swaA", 16: "tab_swaB"})
        B.phase_kvshared("x2", lambda kc, b: kts[:, kc, b * 512:(b + 1) * 512], lambda i: vs[:, i, :], ksum[:, :, :], Res("kvout"), ntiles=NT0)
        B.gbase = 5
        B.layer1("x2", "x3", "out", B.load_kv_host(kts[:, :, :], vs[:, :, :], ksum[:, :, 0:16], ksum[:, :, 16:32]))
        B.P.barrier()
    return nc


def local_tiles(x, b, p):
    xb = x[b].reshape(32, 128, D)
    order = list(range(p, 32, 2)) + list(range(1 - p, 32, 2))
    xall = np.ascontiguousarray(xb[order])
    prev = np.zeros_like(xall)
    for L, gt in enumerate(order):
        if gt >= 1:
            prev[L] = xb[gt - 1]
    return xall, prev


def fused_inputs(inputs):
    perm = q_perm()
    wqkv = inputs["w_qkv_a"][0]
    common = {
        "wq0": np.ascontiguousarray(wqkv[:, :D][:, perm]),
        "wkv0": np.ascontiguousarray(wqkv[:, D:]),
        "wo0": np.ascontiguousarray(inputs["w_o_a"][0][perm, :]),
        "wup0": np.ascontiguousarray(inputs["w_up"][0]),
        "wdn0": np.ascontiguousarray(inputs["w_down"][0]),
        "wkvs": np.ascontiguousarray(inputs["w_kv_shared"]),
        "wq1": np.ascontiguousarray(inputs["w_q_b"][0][:, perm]),
        "wo1": np.ascontiguousarray(inputs["w_o_b"][0][perm, :]),
        "wup1": np.ascontiguousarray(inputs["w_up"][1]),
        "wdn1": np.ascontiguousarray(inputs["w_down"][1]),
        "gains": np.ascontiguousarray(np.stack([inputs["norm_attn_pre"][0], inputs["norm_attn_post"][0], inputs["norm_mlp_pre"][0],
                                               inputs["norm_mlp_post"][0], inputs["kv_norm"], inputs["norm_attn_pre"][1],
                                               inputs["norm_attn_post"][1], inputs["norm_mlp_pre"][1], inputs["norm_mlp_post"][1]]).astype(np.float32)),
        "sinks": np.ascontiguousarray(inputs["sinks_a"][0][[head_of(c, u) for c in range(16) for u in range(2)]].reshape(1, NH)),
        "ident": np.eye(128, dtype=np.float32),
    }
    tabs = [build_tables(0), build_tables(1)]
    maps = []
    for c in range(8):
        b, p = divmod(c, 2)
        m = dict(common)
        m["xall"], m["xprev"] = local_tiles(inputs["x"], b, p)
        for nm in ("tab_swa", "tab_swaA", "tab_swaB", "tab_full_l", "tab_own_l", "ftab"):
            m[nm] = tabs[p][nm]
        maps.append(m)
    return maps


def kernel(**inputs):
    inputs = {k: np.asarray(v) for k, v in inputs.items()}
    nc = build_fused(8)
    r = run_bass_kernel_spmd(nc, fused_inputs(inputs), core_ids=list(range(8))).results
    return assemble([x["out"] for x in r])
```

```python
import numpy as np
from contextlib import ExitStack
import concourse.bass as bass
import concourse.mybir as mybir
from concourse.bass_utils import run_bass_kernel_spmd

F32 = mybir.dt.float32
BF16 = mybir.dt.bfloat16
AF = mybir.ActivationFunctionType
ALU = mybir.AluOpType
AX = mybir.AxisListType

D = 2048
DFF = 8192
NT = 16
NG = 2
GT = 8
GTOK = GT * 128
NH = 32
EPS = 1e-6
RS = 1.0 / float(np.sqrt(D))
NEG = -1.0e30
VW = 72


class Res:
    __slots__ = ("name", "writers", "readers", "excl")

    def __init__(self, name, excl=False):
        self.name = name
        self.writers = []
        self.readers = []
        self.excl = excl


class Prog:
    def __init__(self, nc, n_dma_sems=8):
        self.nc = nc
        self.eng = {"pe": nc.tensor, "act": nc.scalar, "dve": nc.vector, "pool": nc.gpsimd, "sp": nc.sync}
        self.sem = {}
        self.cnt = {}
        for e in ("pe", "act", "dve", "pool"):
            self.sem[e] = nc.alloc_semaphore(name="s_" + e)
            self.cnt[e] = 0
        self.known = {}
        self.dma_pool = {}
        for q in ("sp", "pool"):
            self.dma_pool[q] = [[nc.alloc_semaphore(name=f"d_{q}{i}"), 0] for i in range(n_dma_sems)]
        self.dma_rr = {"sp": 0, "pool": 0}

    def _wait(self, ename, ev):
        s, v = ev
        key = (ename, s.name)
        if self.known.get(key, 0) >= v:
            return
        self.known[key] = v
        self.eng[ename].wait_ge(s, v)

    def _deps(self, ename, reads, writes):
        evs = {}

        def add(ev):
            s, v = ev
            if s.name not in evs or evs[s.name][1] < v:
                evs[s.name] = ev
        for r in reads:
            for ev in r.writers:
                add(ev)
        for w in writes:
            for ev in w.writers:
                add(ev)
            for ev in w.readers:
                add(ev)
        for ev in evs.values():
            if ename == "pe" and ev[0] is self.sem["pe"]:
                continue
            self._wait(ename, ev)

    def _commit(self, ev, reads, writes):
        for r in reads:
            r.readers.append(ev)
            if len(r.readers) > 48:
                best = {}
                for s, v in r.readers:
                    if s.name not in best or best[s.name][1] < v:
                        best[s.name] = (s, v)
                r.readers = list(best.values())
        for w in writes:
            w.writers = [ev]
            w.readers = []

    def op(self, ename, fn, reads=(), writes=(), inc=True):
        if ename != "pe":
            ex = [r for r in reads if r.excl]
            if ex:
                writes = list(writes) + ex
                reads = [r for r in reads if not r.excl]
        self._deps(ename, reads, writes)
        ins = fn()
        seq = self.cnt[ename] + 1
        if inc:
            ins.then_inc(self.sem[ename], 1)
            self.cnt[ename] = seq
        self._commit((self.sem[ename], seq), reads, writes)
        return ins

    def dma(self, q, out, in_, reads=(), writes=()):
        self._deps(q, reads, writes)
        pool = self.dma_pool[q]
        i = self.dma_rr[q]
        self.dma_rr[q] = (i + 1) % len(pool)
        ent = pool[i]
        if ent[1] > 0:
            self._wait(q, (ent[0], ent[1]))
        ent[1] += 16
        self.eng[q].dma_start(out=out, in_=in_).then_inc(ent[0], 16)
        ev = (ent[0], ent[1])
        self._commit(ev, reads, writes)
        return ev

    def collective(self, fn, reads=(), writes=()):
        q = "pool"
        self._deps(q, reads, writes)
        if not hasattr(self, "cc_sem"):
            self.cc_sem = [self.nc.alloc_semaphore(name="s_cc"), 0]
        ent = self.cc_sem
        if ent[1] > 0:
            self._wait(q, (ent[0], ent[1]))
        ent[1] += 16
        fn().then_inc(ent[0], 16)
        ev = (ent[0], ent[1])
        self._commit(ev, reads, writes)
        return ev

    def barrier(self):
        evs = []
        for e in ("pe", "act", "dve", "pool"):
            if self.cnt[e] > 0:
                evs.append((self.sem[e], self.cnt[e]))
        for q, pool in self.dma_pool.items():
            for s, v in pool:
                if v > 0:
                    evs.append((s, v))
        if hasattr(self, "cc_sem") and self.cc_sem[1] > 0:
            evs.append((self.cc_sem[0], self.cc_sem[1]))
        for e in ("pe", "act", "dve", "pool", "sp"):
            for ev in evs:
                self._wait(e, ev)


class Rot:
    def __init__(self, items):
        self.items = items
        self.i = 0

    def next(self):
        it = self.items[self.i]
        self.i = (self.i + 1) % len(self.items)
        return it


def head_of(c, u):
    return 8 * (2 * (c // 8) + u) + (c % 8)


def q_perm():
    perm = np.zeros(D, dtype=np.int64)
    for c in range(16):
        for u in range(2):
            h = head_of(c, u)
            perm[c * 128 + u * 64: c * 128 + u * 64 + 64] = np.arange(h * 64, h * 64 + 64)
    return perm


def my_slopes():
    sl = np.zeros(NH, dtype=np.float64)
    for c in range(16):
        for u in range(2):
            sl[c * 2 + u] = 2.0 ** (-8.0 * (head_of(c, u) + 1) / NH)
    return sl


def build_tables(p):
    sl = my_slopes()[None, :, None]
    s = np.arange(128, dtype=np.float64)[:, None, None]
    t = np.arange(128, dtype=np.float64)[None, None, :]
    d = t - s + 0.0 * sl
    causal = np.where(d >= 0, np.exp(-sl * d), 0.0)
    swaprev = np.where(d < 0, np.exp(-sl * (d + 128.0)), 0.0)
    full1 = np.exp(-sl * (d + 128.0))
    full0 = np.exp(-sl * (d + 256.0))
    zeros = np.zeros_like(causal)
    f = lambda a: np.ascontiguousarray(a.reshape(128, NH * 128).astype(np.float32))
    tabs = {
        "tab_swa": np.stack([f(swaprev), f(causal)], axis=1),
        "tab_swa0": np.stack([f(zeros if p == 0 else swaprev), f(causal)], axis=1),
        "tab_full": np.stack([f(full0), f(full1)], axis=1),
        "tab_own": np.stack([f(causal if p == 0 else full1), f(zeros if p == 0 else causal)], axis=1),
        "tab_swaA": np.stack([f(zeros if p == 0 else swaprev), f(causal)], axis=1),
        "tab_swaB": np.stack([f(swaprev if p == 0 else zeros), f(causal)], axis=1),
        "tab_full_l": np.stack([f(full0 if p == 0 else full1), f(full1 if p == 0 else full0)], axis=1),
        "tab_own_l": np.stack([f(causal), f(zeros if p == 0 else full1)], axis=1),
    }
    fr = np.zeros((NH, 15), dtype=np.float64)
    for k in range(15):
        delta = 15 - k
        fr[:, k] = np.exp(-my_slopes() * 128.0 * (2 * delta + p - 2))
    tabs["ftab"] = np.ascontiguousarray(fr.reshape(1, NH * 15).astype(np.float32))
    return tabs


class Builder:
    def __init__(self, nc):
        self.nc = nc
        self.P = Prog(nc)
        self.dram = {}
        self.dres = {}

    def din(self, name, shape, dt=F32):
        self.dram[name] = self.nc.dram_tensor(name, list(shape), dt, kind="ExternalInput").ap()
        return self.dram[name]

    def dout(self, name, shape, dt=F32):
        self.dram[name] = self.nc.dram_tensor(name, list(shape), dt, kind="ExternalOutput").ap()
        return self.dram[name]

    def dtmp(self, name, shape, dt=F32):
        self.dram[name] = self.nc.dram_tensor(name, list(shape), dt, kind="Internal").ap()
        return self.dram[name]

    def grow(self, row):
        r = getattr(self, "gbase", 0) + row
        return self.dram["gains"][r:r + 1, :]

    def dr(self, name, idx=0):
        key = (name, idx)
        if key not in self.dres:
            self.dres[key] = Res(f"{name}[{idx}]")
        return self.dres[key]

    def uname(self, name):
        self.uid = getattr(self, "uid", 0) + 1
        return f"{name}_{self.uid}"

    def alloc(self, es, name, shape, dt):
        return es.enter_context(self.nc.sbuf_tensor(self.uname(name), list(shape), dt))

    def palloc(self, es, name, shape, dt=F32):
        return es.enter_context(self.nc.psum_tensor(self.uname(name), list(shape), dt))

    def bufs(self, es, name, shape, dt, n, psum=False):
        items = []
        for i in range(n):
            t = (self.palloc if psum else self.alloc)(es, f"{name}{i}", shape, dt)
            items.append((t, Res(f"{name}{i}", excl=psum)))
        return Rot(items)

    def load_bc(self, es, name, src_row):
        n = src_row.shape[-1]
        t = self.alloc(es, name, [128, n], F32)
        r = Res(name)
        self.P.dma("sp", t[:], src_row.partition_broadcast(128), writes=[r])
        return t, r

    def setup_consts(self, es):
        nc, P = self.nc, self.P
        self.ident = self.alloc(es, "ident", [128, 128], BF16)
        self.R_ident = Res("ident")
        P.dma("pool", self.ident[:], self.dram["ident"][:, :], writes=[self.R_ident])
        self.epst = self.alloc(es, "epst", [128, 1], F32)
        self.R_eps = Res("eps")
        P.op("pool", lambda: nc.gpsimd.memset(self.epst[:], EPS), writes=[self.R_eps])

    def rstd_from_ms(self, ms_ap, R_ms, st, R_st):
        nc, P = self.nc, self.P
        P.op("act", lambda: nc.scalar.activation(out=st[:, 0:1], in_=ms_ap, func=AF.Sqrt, bias=self.epst[:, 0:1], scale=1.0),
             reads=[R_ms, self.R_eps], writes=[R_st])
        P.op("dve", lambda: nc.vector.reciprocal(out=st[:, 0:1], in_=st[:, 0:1]), reads=[R_st], writes=[R_st])

    def norm_tile(self, src_ap, src_res, xrot, hrot, strot, junk, R_junk, gbc, R_g):
        nc, P = self.nc, self.P
        x, R_x = xrot.next()
        h, R_h = hrot.next()
        st, R_st = strot.next()
        P.dma("sp", x[:], src_ap, reads=[src_res], writes=[R_x])
        P.op("act", lambda: nc.scalar.activation(out=junk[:], in_=x[:], func=AF.Square, scale=RS, accum_out=st[:, 1:2]),
             reads=[R_x], writes=[R_st])
        self.rstd_from_ms(st[:, 1:2], R_st, st, R_st)
        P.op("dve", lambda: nc.vector.scalar_tensor_tensor(out=h[:], in0=x[:], scalar=st[:, 0:1], in1=gbc[:], op0=ALU.mult, op1=ALU.mult),
             reads=[R_x, R_st, R_g], writes=[R_h])
        return h, R_h, x, R_x

    def transpose_tile(self, h, R_h, dst_fn, R_dst, ptrot):
        nc, P = self.nc, self.P
        for cq in range(4):
            pT, R_pT = ptrot.next()
            for j in range(4):
                c = cq * 4 + j
                P.op("pe", lambda c=c, j=j, pT=pT: nc.tensor.transpose(out=pT[:, j, :], in_=h[:, c * 128:(c + 1) * 128], identity=self.ident[:]),
                     reads=[R_h, self.R_ident], writes=[R_pT], inc=(j == 3))
            if cq % 2 == 0:
                P.op("act", lambda cq=cq, pT=pT: nc.scalar.copy(out=dst_fn(cq), in_=pT[:]), reads=[R_pT], writes=[R_dst])
            else:
                P.op("dve", lambda cq=cq, pT=pT: nc.vector.tensor_copy(out=dst_fn(cq), in_=pT[:]), reads=[R_pT], writes=[R_dst])

    def wslab_src(self, wname, c0, c1):
        return self.dram[wname].rearrange("(k p) n -> p k n", p=128)[:, :, c0:c1]

    def phase_qkv(self, g, layer, xsrc, gain_row, wq_name, qoT, R_qo, kT=None, R_kT=None, Vaug=None, R_V=None,
                  xprev=None, wkv_name=None, gate_sb=None, R_gate=None, kmean=None, R_kmean=None, after_first_loads=None):
        nc, P = self.nc, self.P
        with ExitStack() as es:
            xrot = self.bufs(es, "p1x", [128, D], F32, 2)
            hrot = self.bufs(es, "p1h", [128, D], BF16, 2)
            strot = self.bufs(es, "p1st", [128, 2], F32, 4)
            junk = self.alloc(es, "p1junk", [128, D], BF16)
            R_junk = Res("junk")
            hTrot = self.bufs(es, "p1hT", [128, 16, 512], BF16, 2)
            wqrot = self.bufs(es, "p1wq", [128, 16, 512], BF16, 2)
            gbc, R_g = self.load_bc(es, "p1g", self.grow(gain_row))
            ptrot = self.bufs(es, "p1pT", [128, 4, 128], BF16, 2, psum=True)
            pmrot = self.bufs(es, "p1pm", [128, 512], F32, 3, psum=True)
            if layer == 0:
                wkv = self.alloc(es, "p1wkv", [128, 16, 512], BF16)
                R_wkv = Res("wkv")
                P.dma("pool", wkv[:], self.wslab_src(wkv_name, 0, 512), writes=[R_wkv])
                P.op("pool", lambda: nc.gpsimd.memset(Vaug[:, :, :, 64:65], 1.0), writes=R_V)
            else:
                qfrot = self.bufs(es, "p1qf", [128, 512], F32, 2)
                pg = self.palloc(es, "p1pg", [128, 4, 2, 16], F32)
                R_pg = Res("pg", excl=True)

            blocks = []
            if layer == 0:
                blocks += [("prev", 0), ("prev", 1)]
            blocks += [("mine", 0), ("mine", 1)]
            slab_buf = {}

            def issue_slab(n):
                if n >= 4:
                    return
                w, R_w = wqrot.next()
                P.dma("pool", w[:], self.wslab_src(wq_name, n * 512, (n + 1) * 512), writes=[R_w])
                slab_buf[n] = (w, R_w)
            issue_slab(0)
            issue_slab(1)
            if after_first_loads is not None:
                after_first_loads()
            evac_i = 0
            mine_blocks = []
            for (kind, b) in blocks:
                hT, R_hT = hTrot.next()
                for t in range(4):
                    i = g * GT + b * 4 + t
                    if kind == "prev":
                        src, sres = xprev[i, :, :], self.dr("xprev", i)
                    else:
                        src, sres = self.dram[xsrc][i, :, :], self.dr(xsrc, i)
                    h, R_h, _, _ = self.norm_tile(src, sres, xrot, hrot, strot, junk, R_junk, gbc, R_g)
                    self.transpose_tile(h, R_h, lambda cq, hT=hT, t=t: hT[:, cq * 4:(cq + 1) * 4, t * 128:(t + 1) * 128], R_hT, ptrot)
                if layer == 0:
                    col0 = (0 if kind == "prev" else GTOK) + b * 512
                    vt0 = (0 if kind == "prev" else GT) + b * 4
                    for kc in range(2):
                        pm, R_pm = pmrot.next()
                        for k in range(16):
                            P.op("pe", lambda k=k, kc=kc, pm=pm, hT=hT: nc.tensor.matmul(pm[:], lhsT=wkv[:, k, kc * 128:(kc + 1) * 128], rhs=hT[:, k, :], start=(k == 0), stop=(k == 15)),
                                 reads=[R_wkv, R_hT], writes=[R_pm], inc=(k == 15))
                        P.op("act", lambda kc=kc, pm=pm, col0=col0: nc.scalar.copy(out=kT[:, kc, col0:col0 + 512], in_=pm[:]),
                             reads=[R_pm], writes=[R_kT[(col0 // 128) + j] for j in range(4)])
                    for t in range(4):
                        pm, R_pm = pmrot.next()
                        for k in range(16):
                            P.op("pe", lambda k=k, t=t, pm=pm, hT=hT: nc.tensor.matmul(pm[:, 0:256], lhsT=hT[:, k, t * 128:(t + 1) * 128], rhs=wkv[:, k, 256:512], start=(k == 0), stop=(k == 15)),
                                 reads=[R_wkv, R_hT], writes=[R_pm], inc=(k == 15))
                        P.op("dve", lambda t=t, pm=pm, vt0=vt0: nc.vector.tensor_copy(out=Vaug[:, vt0 + t, :, 0:64], in_=pm[:, 0:256].rearrange("p (a b) -> p a b", a=4)),
                             reads=[R_pm], writes=[R_V[vt0 + t]])
                if kind == "mine":
                    mine_blocks.append((hT, R_hT, b))
            for sq in range(4):
                w, R_w = slab_buf.pop(sq)
                for (hT, R_hT, b) in mine_blocks:
                    for cc in range(4):
                        c = sq * 4 + cc
                        pm, R_pm = pmrot.next()
                        for k in range(16):
                            P.op("pe", lambda k=k, cc=cc, pm=pm, w=w, hT=hT: nc.tensor.matmul(pm[:], lhsT=w[:, k, cc * 128:(cc + 1) * 128], rhs=hT[:, k, :], start=(k == 0), stop=(k == 15)),
                                 reads=[R_w, R_hT], writes=[R_pm], inc=(k == 15))
                        dst = qoT[:, c, b * 512:(b + 1) * 512]
                        wr = [R_qo[b * 4 + j] for j in range(4)]
                        if layer == 0:
                            if evac_i % 2 == 0:
                                P.op("act", lambda pm=pm, dst=dst: nc.scalar.copy(out=dst, in_=pm[:]), reads=[R_pm], writes=wr)
                            else:
                                P.op("dve", lambda pm=pm, dst=dst: nc.vector.tensor_copy(out=dst, in_=pm[:]), reads=[R_pm], writes=wr)
                            evac_i += 1
                        else:
                            P.op("act", lambda pm=pm, dst=dst: nc.scalar.copy(out=dst, in_=pm[:]), reads=[R_pm], writes=wr)
                            qf, R_qf = qfrot.next()
                            P.op("dve", lambda pm=pm, qf=qf: nc.vector.tensor_copy(out=qf[:], in_=pm[:]), reads=[R_pm], writes=[R_qf])
                            kc = c // 8
                            n = 0
                            for t in range(4):
                                for u in range(2):
                                    P.op("pe", lambda t=t, u=u, qf=qf, kc=kc: nc.tensor.matmul(pg[:, t, u, :], lhsT=qf[u * 64:(u + 1) * 64, t * 128:(t + 1) * 128],
                                                                                              rhs=kmean[u * 64:(u + 1) * 64, kc, :], start=True, stop=True),
                                         reads=[R_qf, R_kmean], writes=[R_pg], inc=(n == 7))
                                    n += 1
                            P.op("dve", lambda c=c, b=b: nc.vector.tensor_copy(out=gate_sb[:, b * 4:(b + 1) * 4, 2 * c:2 * c + 2, :], in_=pg[:]),
                                 reads=[R_pg], writes=[R_gate])
                issue_slab(sq + 2)
        P.barrier()

    def finish_tile(self, o_t, R_o, qoT, R_qo_t, tl, ptrot):
        self.transpose_tile(o_t, R_o, lambda cq: qoT[:, cq * 4:(cq + 1) * 4, tl * 128:(tl + 1) * 128], R_qo_t, ptrot)

    def score_block(self, pS, R_pS, kT, R_k0, R_k1, kcol0, kcol1, kc, u, qoT, R_q, c0, tl, tab4, R_tab, pte, R_pte, ptm, R_ptm, mul_eng="dve"):
        nc, P = self.nc, self.P
        for bb, (kcol, R_k) in enumerate(((kcol0, R_k0), (kcol1, R_k1))):
            P.op("pe", lambda bb=bb, kcol=kcol: nc.tensor.matmul(pS[:, bb, :], lhsT=kT[u * 64:(u + 1) * 64, kc, kcol:kcol + 128],
                                                           rhs=qoT[u * 64:(u + 1) * 64, c0:c0 + 4, tl * 128:(tl + 1) * 128], start=True, stop=True),
                 reads=[R_k, R_q], writes=[R_pS], inc=(bb == 1))
        P.op("act", lambda: nc.scalar.activation(out=pte[:], in_=pS[:], func=AF.Exp, scale=0.125), reads=[R_pS], writes=[R_pte])
        meng = nc.vector if mul_eng == "dve" else nc.gpsimd
        P.op(mul_eng, lambda: meng.tensor_tensor(out=ptm[:].rearrange("p b (a t) -> p b a t", a=4), in0=pte[:].rearrange("p b (a t) -> p b a t", a=4),
                                                 in1=tab4, op=ALU.mult), reads=[R_pte, R_tab], writes=[R_ptm])

    def pv_block(self, pO, R_pO, ptm, R_ptm, Vaug, vt0, vt1, R_v0, R_v1, grp):
        nc, P = self.nc, self.P
        n = 0
        for hh in range(4):
            for bb, vt in enumerate((vt0, vt1)):
                P.op("pe", lambda hh=hh, bb=bb, vt=vt: nc.tensor.matmul(pO[:, hh, 0:65], lhsT=ptm[:, bb, hh * 128:(hh + 1) * 128], rhs=Vaug[:, vt, grp, 0:65],
                                                                    start=(bb == 0), stop=(bb == 1)),
                     reads=[R_ptm, R_v0, R_v1], writes=[R_pO], inc=(n == 7))
                n += 1

    def load_tab(self, es, name, dname):
        t = self.alloc(es, name, [128, 2, 16, 2, 128], BF16)
        r = Res(name)
        self.P.dma("pool", t[:].rearrange("p b c u t -> p b (c u t)"), self.dram[dname][:, :, :], writes=[r])
        return t, r

    def run_pipelined(self, units):
        if not units:
            return
        units[0][0]()
        for n in range(len(units)):
            if n + 1 < len(units):
                units[n + 1][0]()
            units[n][1]()

    def phase_swa(self, g, qoT, R_qo, kT, R_kT, Vaug, R_V, special=None, tabs=None):
        nc, P = self.nc, self.P
        special = special or {0: "tab_swa0"}
        with ExitStack() as es:
            sp_tabs = {}
            if tabs is not None:
                tab, R_tab = tabs["tab_swa"]
            else:
                tab, R_tab = self.load_tab(es, "swtab", "tab_swa")
            for ti, nm in special.items():
                if g * GT <= ti < (g + 1) * GT:
                    sp_tabs[ti] = self.load_tab(es, "swtab0", nm)
            snk, R_snk = self.load_bc(es, "snk", self.dram["sinks"][0:1, :])
            P.op("act", lambda: nc.scalar.activation(out=snk[:], in_=snk[:], func=AF.Exp), reads=[R_snk], writes=[R_snk])
            snk_v = snk[:].rearrange("p (c u) -> p c u", u=2)
            pSrot = self.bufs(es, "swS", [128, 2, 512], F32, 2, psum=True)
            pOrot = self.bufs(es, "swO", [128, 4, VW], F32, 2, psum=True)
            ptrot = self.bufs(es, "swpT", [128, 4, 128], BF16, 2, psum=True)
            pterot = self.bufs(es, "swpte", [128, 2, 512], BF16, 3)
            ptmrot = self.bufs(es, "swptm", [128, 2, 512], BF16, 3)
            orot = self.bufs(es, "swo", [128, D], BF16, 2)
            denrot = self.bufs(es, "swden", [128, 8], F32, 4)
            units = []
            for tl in range(GT):
                i = g * GT + tl
                tb, R_tb = sp_tabs.get(i, (tab, R_tab))
                tctx = {}
                batches = [(kc, u, c0) for kc in range(2) for u in range(2) for c0 in (8 * kc, 8 * kc + 4)]
                for bi, (kc, u, c0) in enumerate(batches):
                    st = {}

                    def score(tl=tl, kc=kc, u=u, c0=c0, st=st, tctx=tctx, first=(bi == 0), tb=tb, R_tb=R_tb):
                        if first:
                            tctx["o"] = orot.next()
                        pS, R_pS = pSrot.next()
                        pte, R_pte = pterot.next()
                        st["ptm"] = ptmrot.next()
                        self.score_block(pS, R_pS, kT, R_kT[tl], R_kT[GT + tl], tl * 128, GTOK + tl * 128, kc, u, qoT, R_qo[tl], c0, tl,
                                         tb[:, :, c0:c0 + 4, u, :], R_tb, pte, R_pte, st["ptm"][0], st["ptm"][1])

                    def post(tl=tl, kc=kc, u=u, c0=c0, st=st, tctx=tctx, last=(bi == len(batches) - 1)):
                        grp = 2 * kc + u
                        ptm, R_ptm = st["ptm"]
                        o_t, R_o = tctx["o"]
                        o_v = o_t[:].rearrange("p (c u d) -> p c u d", u=2, d=64)
                        pO, R_pO = pOrot.next()
                        self.pv_block(pO, R_pO, ptm, R_ptm, Vaug, tl, GT + tl, R_V[tl], R_V[GT + tl], grp)
                        den, R_den = denrot.next()
                        P.op("dve", lambda: nc.vector.tensor_tensor(out=den[:, 0:4], in0=pO[:, :, 64], in1=snk_v[:, c0:c0 + 4, u], op=ALU.add),
                             reads=[R_pO, R_snk], writes=[R_den])
                        P.op("dve", lambda: nc.vector.reciprocal(out=den[:, 4:8], in_=den[:, 0:4]), reads=[R_den], writes=[R_den])
                        P.op("dve", lambda: nc.vector.tensor_tensor(out=o_v[:, c0:c0 + 4, u, :], in0=pO[:, :, 0:64],
                                                                    in1=den[:, 4:8].unsqueeze(2).to_broadcast([128, 4, 64]), op=ALU.mult),
                             reads=[R_pO, R_den], writes=[R_o])
                        if last:
                            self.finish_tile(o_t, R_o, qoT, R_qo[tl], tl, ptrot)
                    units.append((score, post))
            self.run_pipelined(units)
        P.barrier()

    def moba_select(self, tl, i, gate_sb, R_gate, w_t, R_w, ft_v, R_ft, gw, eq, mx, R_sel):
        nc, P = self.nc, self.P
        npast = i
        if npast < 1:
            return
        gv = gate_sb[:, tl, :, 0:npast]
        fsl = ft_v[:, :, 15 - npast:15]
        wv = w_t[:, :, 0:npast]
        if npast <= 3:
            P.op("dve", lambda: nc.vector.tensor_copy(out=wv, in_=fsl), reads=[R_ft], writes=[R_w])
            return
        gwv = gw[:, :, 0:npast]
        eqv = eq[:, :, 0:npast]
        mb = mx[:].unsqueeze(2).to_broadcast([128, NH, npast])
        rs = [R_gate, R_sel]
        P.op("dve", lambda: nc.vector.tensor_reduce(out=mx[:], in_=gv, axis=AX.X, op=ALU.max), reads=rs, writes=[R_sel])
        P.op("dve", lambda: nc.vector.tensor_tensor(out=eqv, in0=gv, in1=mb, op=ALU.is_ge), reads=rs, writes=[R_sel])
        P.op("dve", lambda: nc.vector.scalar_tensor_tensor(out=gwv, in0=eqv, scalar=NEG, in1=gv, op0=ALU.mult, op1=ALU.add), reads=rs, writes=[R_sel])
        P.op("dve", lambda: nc.vector.tensor_reduce(out=mx[:], in_=gwv, axis=AX.X, op=ALU.max), reads=rs, writes=[R_sel])
        P.op("dve", lambda: nc.vector.tensor_tensor(out=eqv, in0=gwv, in1=mb, op=ALU.is_ge), reads=rs, writes=[R_sel])
        P.op("dve", lambda: nc.vector.scalar_tensor_tensor(out=gwv, in0=eqv, scalar=NEG, in1=gwv, op0=ALU.mult, op1=ALU.add), reads=rs, writes=[R_sel])
        P.op("dve", lambda: nc.vector.tensor_reduce(out=mx[:], in_=gwv, axis=AX.X, op=ALU.max), reads=rs, writes=[R_sel])
        P.op("dve", lambda: nc.vector.tensor_tensor(out=eqv, in0=gv, in1=mb, op=ALU.is_ge), reads=rs, writes=[R_sel])
        P.op("dve", lambda: nc.vector.tensor_tensor(out=wv, in0=eqv, in1=fsl, op=ALU.mult), reads=[R_sel, R_ft], writes=[R_w])

    def phase_moba(self, g, qoT, R_qo, kTa, R_kTa, Va, R_Va, gate_sb, R_gate, tabs=None):
        nc, P = self.nc, self.P
        with ExitStack() as es:
            if tabs is not None:
                tabF, R_tabF = tabs["tab_full_l"]
                tabO, R_tabO = tabs["tab_own_l"]
            else:
                tabF, R_tabF = self.load_tab(es, "mbF", "tab_full_l")
                tabO, R_tabO = self.load_tab(es, "mbO", "tab_own_l")
            ft, R_ft = self.load_bc(es, "mbft", self.dram["ftab"][0:1, :])
            ft_v = ft[:].rearrange("p (h k) -> p h k", k=15)
            pSrot = self.bufs(es, "mbS", [128, 2, 512], F32, 2, psum=True)
            pOrot = self.bufs(es, "mbOp", [128, 4, VW], F32, 2, psum=True)
            ptrot = self.bufs(es, "mbpT", [128, 4, 128], BF16, 2, psum=True)
            pterot = self.bufs(es, "mbpte", [128, 2, 512], BF16, 3)
            ptmrot = self.bufs(es, "mbptm", [128, 2, 512], BF16, 3)
            orot = self.bufs(es, "mbo", [128, D], BF16, 2)
            accrot = self.bufs(es, "mbacc", [128, 4, VW], F32, 3)
            denrot = self.bufs(es, "mbden", [128, 4], F32, 3)
            wrot = self.bufs(es, "mbw", [128, NH, 15], F32, 2)
            gw = self.alloc(es, "mbgw", [128, NH, 15], F32)
            eq = self.alloc(es, "mbeq", [128, NH, 15], F32)
            mx = self.alloc(es, "mbmx", [128, NH], F32)
            R_sel = Res("selwork")
            units = []
            for tl in range(GT):
                i = g * GT + tl
                npast = i
                tctx = {}
                batches = [(kc, u, c0) for kc in range(2) for u in range(2) for c0 in (8 * kc, 8 * kc + 4)]
                for bi, (kc, u, c0) in enumerate(batches):
                    bctx = {}
                    blocks = [i] + list(range(npast))
                    for ji, j in enumerate(blocks):
                        st = {}

                        def score(tl=tl, i=i, kc=kc, u=u, c0=c0, j=j, st=st, tctx=tctx, bctx=bctx,
                                  first_tile=(bi == 0 and ji == 0), first_batch=(ji == 0), mul_eng="dve"):
                            if first_tile:
                                tctx["o"] = orot.next()
                                tctx["w"] = wrot.next()
                                self.moba_select(tl, i, gate_sb, R_gate, tctx["w"][0], tctx["w"][1], ft_v, R_ft, gw, eq, mx, R_sel)
                            if first_batch:
                                bctx["acc"] = accrot.next()
                            own = (j == i)
                            tb, R_tb = (tabO, R_tabO) if own else (tabF, R_tabF)
                            pS, R_pS = pSrot.next()
                            pte, R_pte = pterot.next()
                            st["ptm"] = ptmrot.next()
                            self.score_block(pS, R_pS, kTa, R_kTa[j], R_kTa[16 + j], j * 128, (16 + j) * 128, kc, u, qoT, R_qo[tl], c0, tl,
                                             tb[:, :, c0:c0 + 4, u, :], R_tb, pte, R_pte, st["ptm"][0], st["ptm"][1], mul_eng=mul_eng)

                        def post(tl=tl, i=i, kc=kc, u=u, c0=c0, j=j, st=st, tctx=tctx, bctx=bctx,
                                 last_batch=(ji == len(blocks) - 1), last_tile=(bi == len(batches) - 1 and ji == len(blocks) - 1)):
                            grp = 2 * kc + u
                            ptm, R_ptm = st["ptm"]
                            o_t, R_o = tctx["o"]
                            w_t, R_w = tctx["w"]
                            acc, R_acc = bctx["acc"]
                            o_v = o_t[:].rearrange("p (c u d) -> p c u d", u=2, d=64)
                            pO, R_pO = pOrot.next()
                            self.pv_block(pO, R_pO, ptm, R_ptm, Va, j, 16 + j, R_Va[j], R_Va[16 + j], grp)
                            if j == i:
                                P.op("act", lambda: nc.scalar.copy(out=acc[:, :, 0:65], in_=pO[:, :, 0:65]), reads=[R_pO], writes=[R_acc])
                            else:
                                for hh in range(4):
                                    hi = (c0 + hh) * 2 + u
                                    P.op("dve", lambda hh=hh, hi=hi: nc.vector.scalar_tensor_tensor(
                                        out=acc[:, hh, 0:65], in0=pO[:, hh, 0:65], scalar=w_t[:, hi, j:j + 1], in1=acc[:, hh, 0:65], op0=ALU.mult, op1=ALU.add),
                                        reads=[R_pO, R_w, R_acc], writes=[R_acc])
                            if last_batch:
                                den, R_den = denrot.next()
                                P.op("dve", lambda: nc.vector.reciprocal(out=den[:], in_=acc[:, :, 64]), reads=[R_acc], writes=[R_den])
                                P.op("dve", lambda: nc.vector.tensor_tensor(out=o_v[:, c0:c0 + 4, u, :], in0=acc[:, :, 0:64],
                                                                            in1=den[:].unsqueeze(2).to_broadcast([128, 4, 64]), op=ALU.mult),
                                     reads=[R_acc, R_den], writes=[R_o])
                            if last_tile:
                                self.finish_tile(o_t, R_o, qoT, R_qo[tl], tl, ptrot)
                        units.append((score, post))
            self.run_pipelined(units)
        P.barrier()

    def prefetch_wo(self, es, wo_name):
        wo = self.alloc(es, "p3wo", [128, 16, D], BF16)
        R_wo = [Res(f"wo{n}") for n in range(4)]
        for nb in range(4):
            self.P.dma("pool", wo[:, :, nb * 512:(nb + 1) * 512], self.wslab_src(wo_name, nb * 512, (nb + 1) * 512), writes=[R_wo[nb]])
        return wo, R_wo

    def phase_oproj(self, g, qoT, R_qo, wo_name, gain_row, xsrc, xdst, wo_pre=None):
        nc, P = self.nc, self.P
        with ExitStack() as es:
            if wo_pre is not None:
                wo, R_wo = wo_pre
            else:
                wo, R_wo = self.prefetch_wo(es, wo_name)
            gbc, R_g = self.load_bc(es, "p3g", self.grow(gain_row))
            xrot = self.bufs(es, "p3x", [128, D], F32, 2)
            tmprot = self.bufs(es, "p3tmp", [128, D], F32, 2)
            xorot = self.bufs(es, "p3xo", [128, D], F32, 2)
            strot = self.bufs(es, "p3st", [128, 8], F32, 3)
            junk = self.alloc(es, "p3junk", [128, 512], BF16)
            R_junk = Res("junk")
            pmrot = self.bufs(es, "p3pm", [128, 512], F32, 8, psum=True)
            for tl in range(GT):
                i = g * GT + tl
                x, R_x = xrot.next()
                P.dma("sp", x[:], self.dram[xsrc][i, :, :], reads=[self.dr(xsrc, i)], writes=[R_x])
                st, R_st = strot.next()
                pms = []
                for nb in range(4):
                    pm, R_pm = pmrot.next()
                    pms.append((pm, R_pm))
                    for k in range(16):
                        P.op("pe", lambda k=k, nb=nb, pm=pm: nc.tensor.matmul(pm[:], lhsT=qoT[:, k, tl * 128:(tl + 1) * 128], rhs=wo[:, k, nb * 512:(nb + 1) * 512], start=(k == 0), stop=(k == 15)),
                             reads=[R_qo[tl], R_wo[nb]], writes=[R_pm], inc=(k == 15))
                    P.op("act", lambda nb=nb, pm=pm, st=st: nc.scalar.activation(out=junk[:], in_=pm[:], func=AF.Square, scale=RS, accum_out=st[:, 2 + nb:3 + nb]),
                         reads=[R_pm], writes=[R_st])
                P.op("dve", lambda st=st: nc.vector.tensor_reduce(out=st[:, 1:2], in_=st[:, 2:6], axis=AX.X, op=ALU.add), reads=[R_st], writes=[R_st])
                self.rstd_from_ms(st[:, 1:2], R_st, st, R_st)
                tmp, R_tmp = tmprot.next()
                for nb in range(4):
                    pm, R_pm = pms[nb]
                    P.op("dve", lambda nb=nb, pm=pm, st=st, tmp=tmp: nc.vector.scalar_tensor_tensor(out=tmp[:, nb * 512:(nb + 1) * 512], in0=pm[:], scalar=st[:, 0:1],
                                                                                                in1=gbc[:, nb * 512:(nb + 1) * 512], op0=ALU.mult, op1=ALU.mult),
                         reads=[R_pm, R_st, R_g], writes=[R_tmp])
                xo, R_xo = xorot.next()
                P.op("pool", lambda tmp=tmp, x=x, xo=xo: nc.gpsimd.tensor_tensor(out=xo[:], in0=tmp[:], in1=x[:], op=ALU.add), reads=[R_tmp, R_x], writes=[R_xo])
                P.dma("sp", self.dram[xdst][i, :, :], xo[:], reads=[R_xo], writes=[self.dr(xdst, i)])
        P.barrier()

    def phase_mlp(self, g, wup_name, wdn_name, gain_pre, gain_post, xsrc, xdst):
        nc, P = self.nc, self.P
        with ExitStack() as es0:
            h2T = self.alloc(es0, "p4h2T", [128, 16, GTOK], BF16)
            R_h2T = [Res(f"h2T{t}") for t in range(GT)]
            yacc = self.alloc(es0, "p4y", [128, GT, D], F32)
            R_y = [Res(f"y{t}") for t in range(GT)]
            esW = ExitStack()
            wuprot = self.bufs(esW, "p4wu", [128, 16, 512], BF16, 2)
            wdnrot = self.bufs(esW, "p4wd", [128, 4, D], BF16, 2)
            NS = DFF // 512
            wup_b, wdn_b, aT_b = {}, {}, {}
            wdn_src = self.dram[wdn_name].rearrange("(s c p) n -> s p c n", p=128, c=4)

            def load_up(s):
                if s < NS:
                    w, R_w = wuprot.next()
                    P.dma("pool", w[:], self.wslab_src(wup_name, s * 512, (s + 1) * 512), writes=[R_w])
                    wup_b[s] = (w, R_w)

            def load_dn(s):
                if s < NS:
                    w, R_w = wdnrot.next()
                    P.dma("pool", w[:], wdn_src[s], writes=[R_w])
                    wdn_b[s] = (w, R_w)
            load_up(0)
            load_dn(0)
            load_up(1)
            load_dn(1)
            with ExitStack() as es:
                xrot = self.bufs(es, "p4x", [128, D], F32, 2)
                hrot = self.bufs(es, "p4h", [128, D], BF16, 2)
                strot = self.bufs(es, "p4st", [128, 2], F32, 4)
                junk = self.alloc(es, "p4junk", [128, D], BF16)
                R_junk = Res("junk")
                gbc, R_g = self.load_bc(es, "p4g", self.grow(gain_pre))
                ptrot = self.bufs(es, "p4pT", [128, 4, 128], BF16, 2, psum=True)
                for tl in range(GT):
                    i = g * GT + tl
                    h, R_h, _, _ = self.norm_tile(self.dram[xsrc][i, :, :], self.dr(xsrc, i), xrot, hrot, strot, junk, R_junk, gbc, R_g)
                    self.transpose_tile(h, R_h, lambda cq, tl=tl: h2T[:, cq * 4:(cq + 1) * 4, tl * 128:(tl + 1) * 128], R_h2T[tl], ptrot)
            P.barrier()
            with ExitStack() as es:
                aTrot = self.bufs(es, "p4aT", [128, 4, GTOK], BF16, 2)
                rrot = self.bufs(es, "p4r", [128, 512], F32, 3)
                purot = self.bufs(es, "p4pu", [128, 512], F32, 3, psum=True)
                pdrot = self.bufs(es, "p4pd", [128, 512], F32, 4, psum=True)
                def up(s):
                    w, R_w = wup_b.pop(s)
                    aT, R_aT = aTrot.next()
                    aT_b[s] = (aT, R_aT)
                    for cc in range(4):
                        for tb in range(2):
                            pu, R_pu = purot.next()
                            for k in range(16):
                                P.op("pe", lambda k=k, cc=cc, tb=tb, pu=pu: nc.tensor.matmul(pu[:], lhsT=w[:, k, cc * 128:(cc + 1) * 128], rhs=h2T[:, k, tb * 512:(tb + 1) * 512], start=(k == 0), stop=(k == 15)),
                                     reads=[R_w] + R_h2T[tb * 4:(tb + 1) * 4], writes=[R_pu], inc=(k == 15))
                            r, R_r = rrot.next()
                            P.op("act", lambda pu=pu, r=r: nc.scalar.activation(out=r[:], in_=pu[:], func=AF.Relu), reads=[R_pu], writes=[R_r])
                            P.op("dve", lambda r=r, cc=cc, tb=tb, aT=aT: nc.vector.tensor_tensor(out=aT[:, cc, tb * 512:(tb + 1) * 512], in0=r[:], in1=r[:], op=ALU.mult),
                                 reads=[R_r], writes=[R_aT])

                def down(s):
                    w, R_w = wdn_b.pop(s)
                    aT, R_aT = aT_b.pop(s)
                    for tl in range(GT):
                        for nb in range(4):
                            pd, R_pd = pdrot.next()
                            for cc in range(4):
                                P.op("pe", lambda cc=cc, nb=nb, tl=tl, pd=pd: nc.tensor.matmul(pd[:], lhsT=aT[:, cc, tl * 128:(tl + 1) * 128], rhs=w[:, cc, nb * 512:(nb + 1) * 512], start=(cc == 0), stop=(cc == 3)),
                                     reads=[R_aT, R_w], writes=[R_pd], inc=(cc == 3))
                            ydst = yacc[:, tl, nb * 512:(nb + 1) * 512]
                            if s == 0:
                                P.op("act", lambda pd=pd, ydst=ydst: nc.scalar.copy(out=ydst, in_=pd[:]), reads=[R_pd], writes=[R_y[tl]])
                            else:
                                P.op("dve", lambda pd=pd, ydst=ydst: nc.vector.tensor_tensor(out=ydst, in0=pd[:], in1=ydst, op=ALU.add), reads=[R_pd, R_y[tl]], writes=[R_y[tl]])

                up(0)
                for s in range(NS):
                    if s + 1 < NS:
                        up(s + 1)
                    load_up(s + 2)
                    down(s)
                    load_dn(s + 2)
            esW.close()
            P.barrier()
            with ExitStack() as es:
                xrot = self.bufs(es, "p4x2", [128, D], F32, 2)
                tmprot = self.bufs(es, "p4tmp", [128, D], F32, 2)
                xorot = self.bufs(es, "p4xo", [128, D], F32, 2)
                strot = self.bufs(es, "p4st2", [128, 2], F32, 4)
                junk = self.alloc(es, "p4junk2", [128, D], BF16)
                R_junk = Res("junk")
                gbc, R_g = self.load_bc(es, "p4g2", self.grow(gain_post))
                for tl in range(GT):
                    i = g * GT + tl
                    x, R_x = xrot.next()
                    P.dma("sp", x[:], self.dram[xsrc][i, :, :], reads=[self.dr(xsrc, i)], writes=[R_x])
                    st, R_st = strot.next()
                    P.op("act", lambda tl=tl, st=st: nc.scalar.activation(out=junk[:], in_=yacc[:, tl, :], func=AF.Square, scale=RS, accum_out=st[:, 1:2]),
                         reads=[R_y[tl]], writes=[R_st])
                    self.rstd_from_ms(st[:, 1:2], R_st, st, R_st)
                    tmp, R_tmp = tmprot.next()
                    P.op("dve", lambda tl=tl, st=st, tmp=tmp: nc.vector.scalar_tensor_tensor(out=tmp[:], in0=yacc[:, tl, :], scalar=st[:, 0:1], in1=gbc[:], op0=ALU.mult, op1=ALU.mult),
                         reads=[R_y[tl], R_st, R_g], writes=[R_tmp])
                    xo, R_xo = xorot.next()
                    P.op("pool", lambda tmp=tmp, x=x, xo=xo: nc.gpsimd.tensor_tensor(out=xo[:], in0=tmp[:], in1=x[:], op=ALU.add), reads=[R_tmp, R_x], writes=[R_xo])
                    P.dma("sp", self.dram[xdst][i, :, :], xo[:], reads=[R_xo], writes=[self.dr(xdst, i)])
        P.barrier()

    def phase_kvshared(self, xsrc, kdst, vdst, ksdst, R_dst, ntiles=NT):
        nc, P = self.nc, self.P
        with ExitStack() as es:
            xrot = self.bufs(es, "p5x", [128, D], F32, 2)
            hrot = self.bufs(es, "p5h", [128, D], BF16, 2)
            strot = self.bufs(es, "p5st", [128, 2], F32, 4)
            junk = self.alloc(es, "p5junk", [128, D], BF16)
            R_junk = Res("junk")
            hTrot = self.bufs(es, "p5hT", [128, 16, 512], BF16, 2)
            gbc, R_g = self.load_bc(es, "p5g", self.grow(4))
            ptrot = self.bufs(es, "p5pT", [128, 4, 128], BF16, 2, psum=True)
            pmrot = self.bufs(es, "p5pm", [128, 512], F32, 3, psum=True)
            wkv = self.alloc(es, "p5wkv", [128, 16, 512], BF16)
            R_wkv = Res("wkvs")
            P.dma("pool", wkv[:], self.wslab_src("wkvs", 0, 512), writes=[R_wkv])
            ksum = self.alloc(es, "p5ksum", [128, 2, ntiles], F32)
            R_ksum = Res("ksum")
            kstrot = self.bufs(es, "p5kst", [128, 512], F32, 2)
            vstrot = self.bufs(es, "p5vst", [128, 256], F32, 2)
            for b in range(ntiles // 4):
                hT, R_hT = hTrot.next()
                for t in range(4):
                    i = b * 4 + t
                    h, R_h, _, _ = self.norm_tile(self.dram[xsrc][i, :, :], self.dr(xsrc, i), xrot, hrot, strot, junk, R_junk, gbc, R_g)
                    self.transpose_tile(h, R_h, lambda cq, hT=hT, t=t: hT[:, cq * 4:(cq + 1) * 4, t * 128:(t + 1) * 128], R_hT, ptrot)
                for kc in range(2):
                    pm, R_pm = pmrot.next()
                    for k in range(16):
                        P.op("pe", lambda k=k, kc=kc, pm=pm, hT=hT: nc.tensor.matmul(pm[:], lhsT=wkv[:, k, kc * 128:(kc + 1) * 128], rhs=hT[:, k, :], start=(k == 0), stop=(k == 15)),
                             reads=[R_wkv, R_hT], writes=[R_pm], inc=(k == 15))
                    kst, R_kst = kstrot.next()
                    P.op("act", lambda pm=pm, kst=kst: nc.scalar.copy(out=kst[:], in_=pm[:]), reads=[R_pm], writes=[R_kst])
                    P.op("dve", lambda pm=pm, kc=kc, b=b: nc.vector.tensor_reduce(out=ksum[:, kc, b * 4:(b + 1) * 4], in_=pm[:].rearrange("p (a t) -> p a t", a=4), axis=AX.X, op=ALU.add),
                         reads=[R_pm, R_ksum], writes=[R_ksum])
                    P.dma("sp", kdst(kc, b), kst[:], reads=[R_kst], writes=[R_dst])
                for t in range(4):
                    i = b * 4 + t
                    pm, R_pm = pmrot.next()
                    for k in range(16):
                        P.op("pe", lambda k=k, t=t, pm=pm, hT=hT: nc.tensor.matmul(pm[:, 0:256], lhsT=hT[:, k, t * 128:(t + 1) * 128], rhs=wkv[:, k, 256:512], start=(k == 0), stop=(k == 15)),
                             reads=[R_wkv, R_hT], writes=[R_pm], inc=(k == 15))
                    vst, R_vst = vstrot.next()
                    P.op("dve", lambda pm=pm, vst=vst: nc.vector.tensor_copy(out=vst[:], in_=pm[:, 0:256]), reads=[R_pm], writes=[R_vst])
                    P.dma("sp", vdst(i), vst[:], reads=[R_vst], writes=[R_dst])
            P.dma("sp", ksdst, ksum[:], reads=[R_ksum], writes=[R_dst])
        P.barrier()

    def layer0(self, xin, x1, x2, ngroups=NG, special=None, tabs=None):
        nc, P = self.nc, self.P
        for g in range(ngroups):
            with ExitStack() as es:
                qoT = self.alloc(es, "qoT", [128, 16, GTOK], BF16)
                R_qo = [Res(f"qo{t}") for t in range(GT)]
                kT = self.alloc(es, "kT", [128, 2, 2 * GTOK], BF16)
                R_kT = [Res(f"kT{t}") for t in range(2 * GT)]
                Vaug = self.alloc(es, "Vaug", [128, 2 * GT, 4, VW], BF16)
                R_V = [Res(f"V{t}") for t in range(2 * GT)]
                tabs = {}
                tab_t = self.alloc(es, "swtab", [128, 2, 16, 2, 128], BF16)
                tabs["tab_swa"] = (tab_t, Res("swtab"))

                def load_tab_now(tab_t=tab_t, tabs=tabs):
                    P.dma("pool", tab_t[:].rearrange("p b c u t -> p b (c u t)"), self.dram["tab_swa"][:, :, :], writes=[tabs["tab_swa"][1]])
                self.phase_qkv(g, 0, xin, 0, "wq0", qoT, R_qo, kT=kT, R_kT=R_kT, Vaug=Vaug, R_V=R_V, xprev=self.dram["xprev"], wkv_name="wkv0",
                               after_first_loads=load_tab_now)
                wo_pre = self.prefetch_wo(es, "wo0")
                self.phase_swa(g, qoT, R_qo, kT, R_kT, Vaug, R_V, special=special, tabs=tabs)
                self.phase_oproj(g, qoT, R_qo, "wo0", 1, xin, x1, wo_pre=wo_pre)
            P.barrier()
            self.phase_mlp(g, "wup0", "wdn0", 2, 3, x1, x2)

    def load_kv_host(self, kt_src, v_src, ksa, ksb):
        def f(kTa, R_kTa, Va, R_Va, kmean, R_kmean, ksb_t, R_ksb):
            P = self.P
            P.dma("pool", kTa[:], kt_src, writes=R_kTa)
            v4 = v_src.rearrange("p t (g d) -> p t g d", g=4)
            for q4 in range(4):
                P.dma("pool", Va[:, q4 * 8:(q4 + 1) * 8, :, 0:64], v4[:, q4 * 8:(q4 + 1) * 8, :, :], writes=R_Va[q4 * 8:(q4 + 1) * 8])
            P.dma("sp", kmean[:], ksa, writes=[R_kmean])
            P.dma("sp", ksb_t[:], ksb, writes=[R_ksb])
        return f

    def load_kv_gathered(self, ex, R_ex):
        def f(kTa, R_kTa, Va, R_Va, kmean, R_kmean, ksb_t, R_ksb):
            P = self.P
            kv = kTa[:].rearrange("p c (i r t) -> p c i r t", r=2, t=128)
            vv = Va[:].rearrange("p (i r) g w -> p i r g w", r=2)
            for r in range(2):
                rows = ex[r * 128:(r + 1) * 128, :]
                P.dma("pool", kv[:, :, :, r, :], rows[:, 0:4096].rearrange("p (c i t) -> p c i t", c=2, i=16), reads=[R_ex], writes=R_kTa)
                vsrc = rows[:, 4096:8192].rearrange("p (i g d) -> p i g d", g=4, d=64)
                for gq in range(4):
                    P.dma("pool", vv[:, :, r, gq, 0:64], vsrc[:, :, gq, :], reads=[R_ex], writes=R_Va)
            P.dma("sp", kmean[:], ex[0:128, 8192:8224].rearrange("p (c j) -> p c j", c=2), reads=[R_ex], writes=[R_kmean])
            P.dma("sp", ksb_t[:], ex[128:256, 8192:8224].rearrange("p (c j) -> p c j", c=2), reads=[R_ex], writes=[R_ksb])
        return f

    def layer1(self, xin, x3, xout, loader, tabs=None):
        nc, P = self.nc, self.P
        for g in range(NG):
            with ExitStack() as es:
                kTa = self.alloc(es, "kTa", [128, 2, 32 * 128], BF16)
                R_kTa = [Res(f"kTa{t}") for t in range(32)]
                Va = self.alloc(es, "Va", [128, 32, 4, VW], BF16)
                R_Va = [Res(f"Va{t}") for t in range(32)]
                kmean = self.alloc(es, "kmean", [128, 2, 16], F32)
                R_kmean = Res("kmean")
                ksb_t = self.alloc(es, "ksb_t", [128, 2, 16], F32)
                R_ksb = Res("ksb")
                loader(kTa, R_kTa, Va, R_Va, kmean, R_kmean, ksb_t, R_ksb)
                P.op("pool", lambda: nc.gpsimd.memset(Va[:, :, :, 64:65], 1.0), reads=R_Va, writes=R_Va)
                P.op("dve", lambda: nc.vector.tensor_tensor(out=kmean[:], in0=kmean[:], in1=ksb_t[:], op=ALU.add), reads=[R_kmean, R_ksb], writes=[R_kmean])
                P.op("dve", lambda: nc.vector.tensor_scalar(out=kmean[:], in0=kmean[:], scalar1=1.0 / 256.0, scalar2=None, op0=ALU.mult), reads=[R_kmean], writes=[R_kmean])
                qoT = self.alloc(es, "qoT1", [128, 16, GTOK], BF16)
                R_qo = [Res(f"qo{t}") for t in range(GT)]
                gate_sb = self.alloc(es, "gate", [128, GT, NH, 16], F32)
                R_gate = Res("gate")
                self.phase_qkv(g, 1, xin, 0, "wq1", qoT, R_qo, gate_sb=gate_sb, R_gate=R_gate, kmean=kmean, R_kmean=R_kmean)
                self.phase_moba(g, qoT, R_qo, kTa, R_kTa, Va, R_Va, gate_sb, R_gate, tabs=tabs)
                self.phase_oproj(g, qoT, R_qo, "wo1", 1, xin, x3)
            P.barrier()
            self.phase_mlp(g, "wup1", "wdn1", 2, 3, x3, xout)
        P.barrier()


def build_stage1():
    nc = bass.Bass("TRN2", target_bir_lowering=False)
    B = Builder(nc)
    B.din("xmine", [NT, 128, D])
    B.din("xprev", [NT, 128, D])
    B.din("wq0", [D, D])
    B.din("wkv0", [D, 512])
    B.din("wo0", [D, D])
    B.din("wup0", [D, DFF])
    B.din("wdn0", [DFF, D])
    B.din("wkvs", [D, 512])
    B.din("gains", [5, D])
    B.din("sinks", [1, NH])
    B.din("ident", [128, 128])
    B.din("tab_swa", [128, 2, NH * 128])
    B.din("tab_swa0", [128, 2, NH * 128])
    B.dtmp("x1", [NT, 128, D])
    B.dout("x2", [NT, 128, D])
    B.dout("kts", [128, 2, NT * 128])
    B.dout("vs", [128, NT, 256])
    B.dout("ksum", [128, 2, NT])
    with ExitStack() as es:
        B.setup_consts(es)
        B.layer0("xmine", "x1", "x2")
        B.phase_kvshared("x2", lambda kc, b: B.dram["kts"][:, kc, b * 512:(b + 1) * 512], lambda i: B.dram["vs"][:, i, :],
                         B.dram["ksum"][:, :, :], Res("kvout"))
        B.P.barrier()
    return nc


def build_stage2():
    nc = bass.Bass("TRN2", target_bir_lowering=False)
    B = Builder(nc)
    B.din("x2", [NT, 128, D])
    B.din("kt_all", [128, 2, 32 * 128])
    B.din("v_all", [128, 32, 256])
    B.din("ksa", [128, 2, 16])
    B.din("ksb", [128, 2, 16])
    B.din("wq1", [D, D])
    B.din("wo1", [D, D])
    B.din("wup1", [D, DFF])
    B.din("wdn1", [DFF, D])
    B.din("gains", [4, D])
    B.din("ident", [128, 128])
    B.din("tab_full", [128, 2, NH * 128])
    B.din("tab_own", [128, 2, NH * 128])
    B.din("ftab", [1, NH * 15])
    B.dtmp("x3", [NT, 128, D])
    B.dout("out", [NT, 128, D])
    with ExitStack() as es:
        B.setup_consts(es)
        B.layer1("x2", "x3", "out", B.load_kv_host(B.dram["kt_all"][:, :, :], B.dram["v_all"][:, :, :], B.dram["ksa"][:, :, :], B.dram["ksb"][:, :, :]))
        B.P.barrier()
    return nc


def core_tiles(x, b, p):
    xb = x[b].reshape(32, 128, D)
    mine = np.ascontiguousarray(xb[p::2])
    prev = np.zeros_like(mine)
    for i in range(NT):
        gt = 2 * i + p - 1
        if gt >= 0:
            prev[i] = xb[gt]
    return mine, prev


def stage1_inputs(inputs):
    perm = q_perm()
    wqkv = inputs["w_qkv_a"][0]
    common = {
        "wq0": np.ascontiguousarray(wqkv[:, :D][:, perm]),
        "wkv0": np.ascontiguousarray(wqkv[:, D:]),
        "wo0": np.ascontiguousarray(inputs["w_o_a"][0][perm, :]),
        "wup0": np.ascontiguousarray(inputs["w_up"][0]),
        "wdn0": np.ascontiguousarray(inputs["w_down"][0]),
        "wkvs": np.ascontiguousarray(inputs["w_kv_shared"]),
        "gains": np.ascontiguousarray(np.stack([inputs["norm_attn_pre"][0], inputs["norm_attn_post"][0], inputs["norm_mlp_pre"][0],
                                               inputs["norm_mlp_post"][0], inputs["kv_norm"]]).astype(np.float32)),
        "sinks": np.ascontiguousarray(inputs["sinks_a"][0][[head_of(c, u) for c in range(16) for u in range(2)]].reshape(1, NH)),
        "ident": np.eye(128, dtype=np.float32),
    }
    tabs = [build_tables(0), build_tables(1)]
    maps = []
    for c in range(8):
        b, p = divmod(c, 2)
        mine, prev = core_tiles(inputs["x"], b, p)
        m = dict(common)
        m["xmine"] = mine
        m["xprev"] = prev
        m["tab_swa"] = tabs[p]["tab_swa"]
        m["tab_swa0"] = tabs[p]["tab_swa0"]
        maps.append(m)
    return maps


def stage2_inputs(inputs, r1):
    perm = q_perm()
    common = {
        "wq1": np.ascontiguousarray(inputs["w_q_b"][0][:, perm]),
        "wo1": np.ascontiguousarray(inputs["w_o_b"][0][perm, :]),
        "wup1": np.ascontiguousarray(inputs["w_up"][1]),
        "wdn1": np.ascontiguousarray(inputs["w_down"][1]),
        "gains": np.ascontiguousarray(np.stack([inputs["norm_attn_pre"][1], inputs["norm_attn_post"][1], inputs["norm_mlp_pre"][1],
                                               inputs["norm_mlp_post"][1]]).astype(np.float32)),
        "ident": np.eye(128, dtype=np.float32),
    }
    tabs = [build_tables(0), build_tables(1)]
    maps = []
    for c in range(8):
        b, p = divmod(c, 2)
        ra, rb = r1[2 * b], r1[2 * b + 1]
        kt_all = np.zeros((128, 2, 32, 128), dtype=np.float32)
        kt_all[:, :, 0::2, :] = ra["kts"].reshape(128, 2, NT, 128)
        kt_all[:, :, 1::2, :] = rb["kts"].reshape(128, 2, NT, 128)
        v_all = np.zeros((128, 32, 256), dtype=np.float32)
        v_all[:, 0::2, :] = ra["vs"]
        v_all[:, 1::2, :] = rb["vs"]
        m = dict(common)
        m["x2"] = np.ascontiguousarray(r1[c]["x2"])
        m["kt_all"] = kt_all.reshape(128, 2, 32 * 128)
        m["v_all"] = v_all
        m["ksa"] = np.ascontiguousarray(ra["ksum"])
        m["ksb"] = np.ascontiguousarray(rb["ksum"])
        m["tab_full"] = tabs[p]["tab_full"]
        m["tab_own"] = tabs[p]["tab_own"]
        m["ftab"] = tabs[p]["ftab"]
        maps.append(m)
    return maps


def assemble(outs):
    res = np.zeros((4, 32, 128, D), dtype=np.float32)
    for c in range(8):
        b, p = divmod(c, 2)
        res[b, p::2] = outs[c]
    return res.reshape(4, 4096, D)


NT0 = 32


def build_fused(n_cores=8):
    nc = bass.Bass("TRN2", target_bir_lowering=False)
    B = Builder(nc)
    B.din("xall", [NT0, 128, D])
    B.din("xprev", [NT0, 128, D])
    for nm, shp in (("wq0", [D, D]), ("wkv0", [D, 512]), ("wo0", [D, D]), ("wup0", [D, DFF]), ("wdn0", [DFF, D]), ("wkvs", [D, 512]),
                    ("wq1", [D, D]), ("wo1", [D, D]), ("wup1", [D, DFF]), ("wdn1", [DFF, D])):
        B.din(nm, shp)
    B.din("gains", [9, D])
    B.din("sinks", [1, NH])
    B.din("ident", [128, 128])
    for nm in ("tab_swa", "tab_swaA", "tab_swaB", "tab_full_l", "tab_own_l"):
        B.din(nm, [128, 2, NH * 128])
    B.din("ftab", [1, NH * 15])
    B.dtmp("x1", [NT0, 128, D])
    B.dtmp("x2", [NT0, 128, D])
    B.dtmp("x3", [NT, 128, D])
    kts = B.dtmp("kts", [128, 2, NT0 * 128])
    vs = B.dtmp("vs", [128, NT0, 256])
    ksum = B.dtmp("ksum", [128, 2, NT0])
    B.dout("out", [NT, 128, D])
    with ExitStack() as es:
        B.setup_consts(es)
        B.gbase = 0
        B.layer0("xall", "x1", "x2", ngroups=NT0 // GT, special={0: "tab_swaA", 16: "tab_swaB"})
        B.phase_kvshared("x2", lambda kc, b: kts[:, kc, b * 512:(b + 1) * 512], lambda i: vs[:, i, :], ksum[:, :, :], Res("kvout"), ntiles=NT0)
        B.gbase = 5
        B.layer1("x2", "x3", "out", B.load_kv_host(kts[:, :, :], vs[:, :, :], ksum[:, :, 0:16], ksum[:, :, 16:32]))
        B.P.barrier()
    return nc


def local_tiles(x, b, p):
    xb = x[b].reshape(32, 128, D)
    order = list(range(p, 32, 2)) + list(range(1 - p, 32, 2))
    xall = np.ascontiguousarray(xb[order])
    prev = np.zeros_like(xall)
    for L, gt in enumerate(order):
        if gt >= 1:
            prev[L] = xb[gt - 1]
    return xall, prev


def fused_inputs(inputs):
    perm = q_perm()
    wqkv = inputs["w_qkv_a"][0]
    common = {
        "wq0": np.ascontiguousarray(wqkv[:, :D][:, perm]),
        "wkv0": np.ascontiguousarray(wqkv[:, D:]),
        "wo0": np.ascontiguousarray(inputs["w_o_a"][0][perm, :]),
        "wup0": np.ascontiguousarray(inputs["w_up"][0]),
        "wdn0": np.ascontiguousarray(inputs["w_down"][0]),
        "wkvs": np.ascontiguousarray(inputs["w_kv_shared"]),
        "wq1": np.ascontiguousarray(inputs["w_q_b"][0][:, perm]),
        "wo1": np.ascontiguousarray(inputs["w_o_b"][0][perm, :]),
        "wup1": np.ascontiguousarray(inputs["w_up"][1]),
        "wdn1": np.ascontiguousarray(inputs["w_down"][1]),
        "gains": np.ascontiguousarray(np.stack([inputs["norm_attn_pre"][0], inputs["norm_attn_post"][0], inputs["norm_mlp_pre"][0],
                                               inputs["norm_mlp_post"][0], inputs["kv_norm"], inputs["norm_attn_pre"][1],
                                               inputs["norm_attn_post"][1], inputs["norm_mlp_pre"][1], inputs["norm_mlp_post"][1]]).astype(np.float32)),
        "sinks": np.ascontiguousarray(inputs["sinks_a"][0][[head_of(c, u) for c in range(16) for u in range(2)]].reshape(1, NH)),
        "ident": np.eye(128, dtype=np.float32),
    }
    tabs = [build_tables(0), build_tables(1)]
    maps = []
    for c in range(8):
        b, p = divmod(c, 2)
        m = dict(common)
        m["xall"], m["xprev"] = local_tiles(inputs["x"], b, p)
        for nm in ("tab_swa", "tab_swaA", "tab_swaB", "tab_full_l", "tab_own_l", "ftab"):
            m[nm] = tabs[p][nm]
        maps.append(m)
    return maps


def kernel(**inputs):
    inputs = {k: np.asarray(v) for k, v in inputs.items()}
    nc = build_fused(8)
    r = run_bass_kernel_spmd(nc, fused_inputs(inputs), core_ids=list(range(8))).results
    return assemble([x["out"] for x in r])
```

```python
import numpy as np
from contextlib import ExitStack
import concourse.bass as bass
import concourse.mybir as mybir
from concourse.bass_utils import run_bass_kernel_spmd

F32 = mybir.dt.float32
BF16 = mybir.dt.bfloat16
AF = mybir.ActivationFunctionType
ALU = mybir.AluOpType
AX = mybir.AxisListType

D = 2048
DFF = 8192
NT = 16
NG = 2
GT = 8
GTOK = GT * 128
NH = 32
EPS = 1e-6
RS = 1.0 / float(np.sqrt(D))
NEG = -1.0e30
VW = 72


class Res:
    __slots__ = ("name", "writers", "readers", "excl")

    def __init__(self, name, excl=False):
        self.name = name
        self.writers = []
        self.readers = []
        self.excl = excl


class Prog:
    def __init__(self, nc, n_dma_sems=8):
        self.nc = nc
        self.eng = {"pe": nc.tensor, "act": nc.scalar, "dve": nc.vector, "pool": nc.gpsimd, "sp": nc.sync}
        self.sem = {}
        self.cnt = {}
        for e in ("pe", "act", "dve", "pool"):
            self.sem[e] = nc.alloc_semaphore(name="s_" + e)
            self.cnt[e] = 0
        self.known = {}
        self.dma_pool = {}
        for q in ("sp", "pool"):
            self.dma_pool[q] = [[nc.alloc_semaphore(name=f"d_{q}{i}"), 0] for i in range(n_dma_sems)]
        self.dma_rr = {"sp": 0, "pool": 0}

    def _wait(self, ename, ev):
        s, v = ev
        key = (ename, s.name)
        if self.known.get(key, 0) >= v:
            return
        self.known[key] = v
        self.eng[ename].wait_ge(s, v)

    def _deps(self, ename, reads, writes):
        evs = {}

        def add(ev):
            s, v = ev
            if s.name not in evs or evs[s.name][1] < v:
                evs[s.name] = ev
        for r in reads:
            for ev in r.writers:
                add(ev)
        for w in writes:
            for ev in w.writers:
                add(ev)
            for ev in w.readers:
                add(ev)
        for ev in evs.values():
            if ename == "pe" and ev[0] is self.sem["pe"]:
                continue
            self._wait(ename, ev)

    def _commit(self, ev, reads, writes):
        for r in reads:
            r.readers.append(ev)
            if len(r.readers) > 48:
                best = {}
                for s, v in r.readers:
                    if s.name not in best or best[s.name][1] < v:
                        best[s.name] = (s, v)
                r.readers = list(best.values())
        for w in writes:
            w.writers = [ev]
            w.readers = []

    def op(self, ename, fn, reads=(), writes=(), inc=True):
        if ename != "pe":
            ex = [r for r in reads if r.excl]
            if ex:
                writes = list(writes) + ex
                reads = [r for r in reads if not r.excl]
        self._deps(ename, reads, writes)
        ins = fn()
        seq = self.cnt[ename] + 1
        if inc:
            ins.then_inc(self.sem[ename], 1)
            self.cnt[ename] = seq
        self._commit((self.sem[ename], seq), reads, writes)
        return ins

    def dma(self, q, out, in_, reads=(), writes=()):
        self._deps(q, reads, writes)
        pool = self.dma_pool[q]
        i = self.dma_rr[q]
        self.dma_rr[q] = (i + 1) % len(pool)
        ent = pool[i]
        if ent[1] > 0:
            self._wait(q, (ent[0], ent[1]))
        ent[1] += 16
        self.eng[q].dma_start(out=out, in_=in_).then_inc(ent[0], 16)
        ev = (ent[0], ent[1])
        self._commit(ev, reads, writes)
        return ev

    def collective(self, fn, reads=(), writes=()):
        q = "pool"
        self._deps(q, reads, writes)
        if not hasattr(self, "cc_sem"):
            self.cc_sem = [self.nc.alloc_semaphore(name="s_cc"), 0]
        ent = self.cc_sem
        if ent[1] > 0:
            self._wait(q, (ent[0], ent[1]))
        ent[1] += 16
        fn().then_inc(ent[0], 16)
        ev = (ent[0], ent[1])
        self._commit(ev, reads, writes)
        return ev

    def barrier(self):
        evs = []
        for e in ("pe", "act", "dve", "pool"):
            if self.cnt[e] > 0:
                evs.append((self.sem[e], self.cnt[e]))
        for q, pool in self.dma_pool.items():
            for s, v in pool:
                if v > 0:
                    evs.append((s, v))
        if hasattr(self, "cc_sem") and self.cc_sem[1] > 0:
            evs.append((self.cc_sem[0], self.cc_sem[1]))
        for e in ("pe", "act", "dve", "pool", "sp"):
            for ev in evs:
                self._wait(e, ev)


class Rot:
    def __init__(self, items):
        self.items = items
        self.i = 0

    def next(self):
        it = self.items[self.i]
        self.i = (self.i + 1) % len(self.items)
        return it


def head_of(c, u):
    return 8 * (2 * (c // 8) + u) + (c % 8)


def q_perm():
    perm = np.zeros(D, dtype=np.int64)
    for c in range(16):
        for u in range(2):
            h = head_of(c, u)
            perm[c * 128 + u * 64: c * 128 + u * 64 + 64] = np.arange(h * 64, h * 64 + 64)
    return perm


def my_slopes():
    sl = np.zeros(NH, dtype=np.float64)
    for c in range(16):
        for u in range(2):
            sl[c * 2 + u] = 2.0 ** (-8.0 * (head_of(c, u) + 1) / NH)
    return sl


def build_tables(p):
    sl = my_slopes()[None, :, None]
    s = np.arange(128, dtype=np.float64)[:, None, None]
    t = np.arange(128, dtype=np.float64)[None, None, :]
    d = t - s + 0.0 * sl
    causal = np.where(d >= 0, np.exp(-sl * d), 0.0)
    swaprev = np.where(d < 0, np.exp(-sl * (d + 128.0)), 0.0)
    full1 = np.exp(-sl * (d + 128.0))
    full0 = np.exp(-sl * (d + 256.0))
    zeros = np.zeros_like(causal)
    f = lambda a: np.ascontiguousarray(a.reshape(128, NH * 128).astype(np.float32))
    tabs = {
        "tab_swa": np.stack([f(swaprev), f(causal)], axis=1),
        "tab_swa0": np.stack([f(zeros if p == 0 else swaprev), f(causal)], axis=1),
        "tab_full": np.stack([f(full0), f(full1)], axis=1),
        "tab_own": np.stack([f(causal if p == 0 else full1), f(zeros if p == 0 else causal)], axis=1),
        "tab_swaA": np.stack([f(zeros if p == 0 else swaprev), f(causal)], axis=1),
        "tab_swaB": np.stack([f(swaprev if p == 0 else zeros), f(causal)], axis=1),
        "tab_full_l": np.stack([f(full0 if p == 0 else full1), f(full1 if p == 0 else full0)], axis=1),
        "tab_own_l": np.stack([f(causal), f(zeros if p == 0 else full1)], axis=1),
    }
    fr = np.zeros((NH, 15), dtype=np.float64)
    for k in range(15):
        delta = 15 - k
        fr[:, k] = np.exp(-my_slopes() * 128.0 * (2 * delta + p - 2))
    tabs["ftab"] = np.ascontiguousarray(fr.reshape(1, NH * 15).astype(np.float32))
    return tabs


class Builder:
    def __init__(self, nc):
        self.nc = nc
        self.P = Prog(nc)
        self.dram = {}
        self.dres = {}

    def din(self, name, shape, dt=F32):
        self.dram[name] = self.nc.dram_tensor(name, list(shape), dt, kind="ExternalInput").ap()
        return self.dram[name]

    def dout(self, name, shape, dt=F32):
        self.dram[name] = self.nc.dram_tensor(name, list(shape), dt, kind="ExternalOutput").ap()
        return self.dram[name]

    def dtmp(self, name, shape, dt=F32):
        self.dram[name] = self.nc.dram_tensor(name, list(shape), dt, kind="Internal").ap()
        return self.dram[name]

    def grow(self, row):
        r = getattr(self, "gbase", 0) + row
        return self.dram["gains"][r:r + 1, :]

    def dr(self, name, idx=0):
        key = (name, idx)
        if key not in self.dres:
            self.dres[key] = Res(f"{name}[{idx}]")
        return self.dres[key]

    def uname(self, name):
        self.uid = getattr(self, "uid", 0) + 1
        return f"{name}_{self.uid}"

    def alloc(self, es, name, shape, dt):
        return es.enter_context(self.nc.sbuf_tensor(self.uname(name), list(shape), dt))

    def palloc(self, es, name, shape, dt=F32):
        return es.enter_context(self.nc.psum_tensor(self.uname(name), list(shape), dt))

    def bufs(self, es, name, shape, dt, n, psum=False):
        items = []
        for i in range(n):
            t = (self.palloc if psum else self.alloc)(es, f"{name}{i}", shape, dt)
            items.append((t, Res(f"{name}{i}", excl=psum)))
        return Rot(items)

    def load_bc(self, es, name, src_row):
        n = src_row.shape[-1]
        t = self.alloc(es, name, [128, n], F32)
        r = Res(name)
        self.P.dma("sp", t[:], src_row.partition_broadcast(128), writes=[r])
        return t, r

    def setup_consts(self, es):
        nc, P = self.nc, self.P
        self.ident = self.alloc(es, "ident", [128, 128], BF16)
        self.R_ident = Res("ident")
        P.dma("pool", self.ident[:], self.dram["ident"][:, :], writes=[self.R_ident])
        self.epst = self.alloc(es, "epst", [128, 1], F32)
        self.R_eps = Res("eps")
        P.op("pool", lambda: nc.gpsimd.memset(self.epst[:], EPS), writes=[self.R_eps])

    def rstd_from_ms(self, ms_ap, R_ms, st, R_st):
        nc, P = self.nc, self.P
        P.op("act", lambda: nc.scalar.activation(out=st[:, 0:1], in_=ms_ap, func=AF.Sqrt, bias=self.epst[:, 0:1], scale=1.0),
             reads=[R_ms, self.R_eps], writes=[R_st])
        P.op("dve", lambda: nc.vector.reciprocal(out=st[:, 0:1], in_=st[:, 0:1]), reads=[R_st], writes=[R_st])

    def norm_tile(self, src_ap, src_res, xrot, hrot, strot, junk, R_junk, gbc, R_g):
        nc, P = self.nc, self.P
        x, R_x = xrot.next()
        h, R_h = hrot.next()
        st, R_st = strot.next()
        P.dma("sp", x[:], src_ap, reads=[src_res], writes=[R_x])
        P.op("act", lambda: nc.scalar.activation(out=junk[:], in_=x[:], func=AF.Square, scale=RS, accum_out=st[:, 1:2]),
             reads=[R_x], writes=[R_st])
        self.rstd_from_ms(st[:, 1:2], R_st, st, R_st)
        P.op("dve", lambda: nc.vector.scalar_tensor_tensor(out=h[:], in0=x[:], scalar=st[:, 0:1], in1=gbc[:], op0=ALU.mult, op1=ALU.mult),
             reads=[R_x, R_st, R_g], writes=[R_h])
        return h, R_h, x, R_x

    def transpose_tile(self, h, R_h, dst_fn, R_dst, ptrot):
        nc, P = self.nc, self.P
        for cq in range(4):
            pT, R_pT = ptrot.next()
            for j in range(4):
                c = cq * 4 + j
                P.op("pe", lambda c=c, j=j, pT=pT: nc.tensor.transpose(out=pT[:, j, :], in_=h[:, c * 128:(c + 1) * 128], identity=self.ident[:]),
                     reads=[R_h, self.R_ident], writes=[R_pT], inc=(j == 3))
            if cq % 2 == 0:
                P.op("act", lambda cq=cq, pT=pT: nc.scalar.copy(out=dst_fn(cq), in_=pT[:]), reads=[R_pT], writes=[R_dst])
            else:
                P.op("dve", lambda cq=cq, pT=pT: nc.vector.tensor_copy(out=dst_fn(cq), in_=pT[:]), reads=[R_pT], writes=[R_dst])

    def wslab_src(self, wname, c0, c1):
        return self.dram[wname].rearrange("(k p) n -> p k n", p=128)[:, :, c0:c1]

    def phase_qkv(self, g, layer, xsrc, gain_row, wq_name, qoT, R_qo, kT=None, R_kT=None, Vaug=None, R_V=None,
                  xprev=None, wkv_name=None, gate_sb=None, R_gate=None, kmean=None, R_kmean=None, after_first_loads=None):
        nc, P = self.nc, self.P
        with ExitStack() as es:
            xrot = self.bufs(es, "p1x", [128, D], F32, 2)
            hrot = self.bufs(es, "p1h", [128, D], BF16, 2)
            strot = self.bufs(es, "p1st", [128, 2], F32, 4)
            junk = self.alloc(es, "p1junk", [128, D], BF16)
            R_junk = Res("junk")
            hTrot = self.bufs(es, "p1hT", [128, 16, 512], BF16, 2)
            wqrot = self.bufs(es, "p1wq", [128, 16, 512], BF16, 2)
            gbc, R_g = self.load_bc(es, "p1g", self.grow(gain_row))
            ptrot = self.bufs(es, "p1pT", [128, 4, 128], BF16, 3, psum=True)
            pmrot = self.bufs(es, "p1pm", [128, 512], F32, 3, psum=True)
            if layer == 0:
                wkv = self.alloc(es, "p1wkv", [128, 16, 512], BF16)
                R_wkv = Res("wkv")
                P.dma("pool", wkv[:], self.wslab_src(wkv_name, 0, 512), writes=[R_wkv])
                P.op("pool", lambda: nc.gpsimd.memset(Vaug[:, :, :, 64:65], 1.0), writes=R_V)
            else:
                qfrot = self.bufs(es, "p1qf", [128, 512], F32, 2)
                pg = self.palloc(es, "p1pg", [128, 4, 2, 16], F32)
                R_pg = Res("pg", excl=True)

            blocks = []
            if layer == 0:
                blocks += [("prev", 0), ("prev", 1)]
            blocks += [("mine", 0), ("mine", 1)]
            slab_buf = {}

            def issue_slab(n):
                if n >= 4:
                    return
                w, R_w = wqrot.next()
                P.dma("pool", w[:], self.wslab_src(wq_name, n * 512, (n + 1) * 512), writes=[R_w])
                slab_buf[n] = (w, R_w)
            issue_slab(0)
            issue_slab(1)
            if after_first_loads is not None:
                after_first_loads()
            evac_i = 0
            mine_blocks = []
            for (kind, b) in blocks:
                hT, R_hT = hTrot.next()
                for t in range(4):
                    i = g * GT + b * 4 + t
                    if kind == "prev":
                        src, sres = xprev[i, :, :], self.dr("xprev", i)
                    else:
                        src, sres = self.dram[xsrc][i, :, :], self.dr(xsrc, i)
                    h, R_h, _, _ = self.norm_tile(src, sres, xrot, hrot, strot, junk, R_junk, gbc, R_g)
                    self.transpose_tile(h, R_h, lambda cq, hT=hT, t=t: hT[:, cq * 4:(cq + 1) * 4, t * 128:(t + 1) * 128], R_hT, ptrot)
                if layer == 0:
                    col0 = (0 if kind == "prev" else GTOK) + b * 512
                    vt0 = (0 if kind == "prev" else GT) + b * 4
                    for kc in range(2):
                        pm, R_pm = pmrot.next()
                        for k in range(16):
                            P.op("pe", lambda k=k, kc=kc, pm=pm, hT=hT: nc.tensor.matmul(pm[:], lhsT=wkv[:, k, kc * 128:(kc + 1) * 128], rhs=hT[:, k, :], start=(k == 0), stop=(k == 15)),
                                 reads=[R_wkv, R_hT], writes=[R_pm], inc=(k == 15))
                        P.op("act", lambda kc=kc, pm=pm, col0=col0: nc.scalar.copy(out=kT[:, kc, col0:col0 + 512], in_=pm[:]),
                             reads=[R_pm], writes=[R_kT[(col0 // 128) + j] for j in range(4)])
                    for t in range(4):
                        pm, R_pm = pmrot.next()
                        for k in range(16):
                            P.op("pe", lambda k=k, t=t, pm=pm, hT=hT: nc.tensor.matmul(pm[:, 0:256], lhsT=hT[:, k, t * 128:(t + 1) * 128], rhs=wkv[:, k, 256:512], start=(k == 0), stop=(k == 15)),
                                 reads=[R_wkv, R_hT], writes=[R_pm], inc=(k == 15))
                        P.op("dve", lambda t=t, pm=pm, vt0=vt0: nc.vector.tensor_copy(out=Vaug[:, vt0 + t, :, 0:64], in_=pm[:, 0:256].rearrange("p (a b) -> p a b", a=4)),
                             reads=[R_pm], writes=[R_V[vt0 + t]])
                if kind == "mine":
                    mine_blocks.append((hT, R_hT, b))
            for sq in range(4):
                w, R_w = slab_buf.pop(sq)
                for (hT, R_hT, b) in mine_blocks:
                    for cc in range(4):
                        c = sq * 4 + cc
                        pm, R_pm = pmrot.next()
                        for k in range(16):
                            P.op("pe", lambda k=k, cc=cc, pm=pm, w=w, hT=hT: nc.tensor.matmul(pm[:], lhsT=w[:, k, cc * 128:(cc + 1) * 128], rhs=hT[:, k, :], start=(k == 0), stop=(k == 15)),
                                 reads=[R_w, R_hT], writes=[R_pm], inc=(k == 15))
                        dst = qoT[:, c, b * 512:(b + 1) * 512]
                        wr = [R_qo[b * 4 + j] for j in range(4)]
                        if layer == 0:
                            if evac_i % 2 == 0:
                                P.op("act", lambda pm=pm, dst=dst: nc.scalar.copy(out=dst, in_=pm[:]), reads=[R_pm], writes=wr)
                            else:
                                P.op("dve", lambda pm=pm, dst=dst: nc.vector.tensor_copy(out=dst, in_=pm[:]), reads=[R_pm], writes=wr)
                            evac_i += 1
                        else:
                            P.op("act", lambda pm=pm, dst=dst: nc.scalar.copy(out=dst, in_=pm[:]), reads=[R_pm], writes=wr)
                            qf, R_qf = qfrot.next()
                            P.op("dve", lambda pm=pm, qf=qf: nc.vector.tensor_copy(out=qf[:], in_=pm[:]), reads=[R_pm], writes=[R_qf])
                            kc = c // 8
                            n = 0
                            for t in range(4):
                                for u in range(2):
                                    P.op("pe", lambda t=t, u=u, qf=qf, kc=kc: nc.tensor.matmul(pg[:, t, u, :], lhsT=qf[u * 64:(u + 1) * 64, t * 128:(t + 1) * 128],
                                                                                              rhs=kmean[u * 64:(u + 1) * 64, kc, :], start=True, stop=True),
                                         reads=[R_qf, R_kmean], writes=[R_pg], inc=(n == 7))
                                    n += 1
                            P.op("dve", lambda c=c, b=b: nc.vector.tensor_copy(out=gate_sb[:, b * 4:(b + 1) * 4, 2 * c:2 * c + 2, :], in_=pg[:]),
                                 reads=[R_pg], writes=[R_gate])
                issue_slab(sq + 2)
        P.barrier()

    def finish_tile(self, o_t, R_o, qoT, R_qo_t, tl, ptrot):
        self.transpose_tile(o_t, R_o, lambda cq: qoT[:, cq * 4:(cq + 1) * 4, tl * 128:(tl + 1) * 128], R_qo_t, ptrot)

    def score_block(self, pS, R_pS, kT, R_k0, R_k1, kcol0, kcol1, kc, u, qoT, R_q, c0, tl, tab4, R_tab, pte, R_pte, ptm, R_ptm, mul_eng="dve"):
        nc, P = self.nc, self.P
        for bb, (kcol, R_k) in enumerate(((kcol0, R_k0), (kcol1, R_k1))):
            P.op("pe", lambda bb=bb, kcol=kcol: nc.tensor.matmul(pS[:, bb, :], lhsT=kT[u * 64:(u + 1) * 64, kc, kcol:kcol + 128],
                                                           rhs=qoT[u * 64:(u + 1) * 64, c0:c0 + 4, tl * 128:(tl + 1) * 128], start=True, stop=True),
                 reads=[R_k, R_q], writes=[R_pS], inc=(bb == 1))
        P.op("act", lambda: nc.scalar.activation(out=pte[:], in_=pS[:], func=AF.Exp, scale=0.125), reads=[R_pS], writes=[R_pte])
        meng = nc.vector if mul_eng == "dve" else nc.gpsimd
        P.op(mul_eng, lambda: meng.tensor_tensor(out=ptm[:].rearrange("p b (a t) -> p b a t", a=4), in0=pte[:].rearrange("p b (a t) -> p b a t", a=4),
                                                 in1=tab4, op=ALU.mult), reads=[R_pte, R_tab], writes=[R_ptm])

    def pv_block(self, pO, R_pO, ptm, R_ptm, Vaug, vt0, vt1, R_v0, R_v1, grp):
        nc, P = self.nc, self.P
        n = 0
        for hh in range(4):
            for bb, vt in enumerate((vt0, vt1)):
                P.op("pe", lambda hh=hh, bb=bb, vt=vt: nc.tensor.matmul(pO[:, hh, 0:65], lhsT=ptm[:, bb, hh * 128:(hh + 1) * 128], rhs=Vaug[:, vt, grp, 0:65],
                                                                    start=(bb == 0), stop=(bb == 1)),
                     reads=[R_ptm, R_v0, R_v1], writes=[R_pO], inc=(n == 7))
                n += 1

    def load_tab(self, es, name, dname):
        t = self.alloc(es, name, [128, 2, 16, 2, 128], BF16)
        r = Res(name)
        self.P.dma("pool", t[:].rearrange("p b c u t -> p b (c u t)"), self.dram[dname][:, :, :], writes=[r])
        return t, r

    def run_pipelined(self, units):
        if not units:
            return
        units[0][0]()
        for n in range(len(units)):
            if n + 1 < len(units):
                units[n + 1][0]()
            units[n][1]()

    def phase_swa(self, g, qoT, R_qo, kT, R_kT, Vaug, R_V, special=None, tabs=None):
        nc, P = self.nc, self.P
        special = special or {0: "tab_swa0"}
        with ExitStack() as es:
            sp_tabs = {}
            if tabs is not None:
                tab, R_tab = tabs["tab_swa"]
            else:
                tab, R_tab = self.load_tab(es, "swtab", "tab_swa")
            for ti, nm in special.items():
                if g * GT <= ti < (g + 1) * GT:
                    sp_tabs[ti] = self.load_tab(es, "swtab0", nm)
            snk, R_snk = self.load_bc(es, "snk", self.dram["sinks"][0:1, :])
            P.op("act", lambda: nc.scalar.activation(out=snk[:], in_=snk[:], func=AF.Exp), reads=[R_snk], writes=[R_snk])
            snk_v = snk[:].rearrange("p (c u) -> p c u", u=2)
            pSrot = self.bufs(es, "swS", [128, 2, 512], F32, 2, psum=True)
            pOrot = self.bufs(es, "swO", [128, 4, VW], F32, 2, psum=True)
            ptrot = self.bufs(es, "swpT", [128, 4, 128], BF16, 2, psum=True)
            pterot = self.bufs(es, "swpte", [128, 2, 512], BF16, 3)
            ptmrot = self.bufs(es, "swptm", [128, 2, 512], BF16, 3)
            orot = self.bufs(es, "swo", [128, D], BF16, 2)
            denrot = self.bufs(es, "swden", [128, 8], F32, 4)
            units = []
            for tl in range(GT):
                i = g * GT + tl
                tb, R_tb = sp_tabs.get(i, (tab, R_tab))
                tctx = {}
                batches = [(kc, u, c0) for kc in range(2) for u in range(2) for c0 in (8 * kc, 8 * kc + 4)]
                for bi, (kc, u, c0) in enumerate(batches):
                    st = {}

                    def score(tl=tl, kc=kc, u=u, c0=c0, st=st, tctx=tctx, first=(bi == 0), tb=tb, R_tb=R_tb):
                        if first:
                            tctx["o"] = orot.next()
                        pS, R_pS = pSrot.next()
                        pte, R_pte = pterot.next()
                        st["ptm"] = ptmrot.next()
                        self.score_block(pS, R_pS, kT, R_kT[tl], R_kT[GT + tl], tl * 128, GTOK + tl * 128, kc, u, qoT, R_qo[tl], c0, tl,
                                         tb[:, :, c0:c0 + 4, u, :], R_tb, pte, R_pte, st["ptm"][0], st["ptm"][1])

                    def post(tl=tl, kc=kc, u=u, c0=c0, st=st, tctx=tctx, last=(bi == len(batches) - 1)):
                        grp = 2 * kc + u
                        ptm, R_ptm = st["ptm"]
                        o_t, R_o = tctx["o"]
                        o_v = o_t[:].rearrange("p (c u d) -> p c u d", u=2, d=64)
                        pO, R_pO = pOrot.next()
                        self.pv_block(pO, R_pO, ptm, R_ptm, Vaug, tl, GT + tl, R_V[tl], R_V[GT + tl], grp)
                        den, R_den = denrot.next()
                        P.op("dve", lambda: nc.vector.tensor_tensor(out=den[:, 0:4], in0=pO[:, :, 64], in1=snk_v[:, c0:c0 + 4, u], op=ALU.add),
                             reads=[R_pO, R_snk], writes=[R_den])
                        P.op("dve", lambda: nc.vector.reciprocal(out=den[:, 4:8], in_=den[:, 0:4]), reads=[R_den], writes=[R_den])
                        P.op("dve", lambda: nc.vector.tensor_tensor(out=o_v[:, c0:c0 + 4, u, :], in0=pO[:, :, 0:64],
                                                                    in1=den[:, 4:8].unsqueeze(2).to_broadcast([128, 4, 64]), op=ALU.mult),
                             reads=[R_pO, R_den], writes=[R_o])
                        if last:
                            self.finish_tile(o_t, R_o, qoT, R_qo[tl], tl, ptrot)
                    units.append((score, post))
            self.run_pipelined(units)
        P.barrier()

    def moba_select(self, tl, i, gate_sb, R_gate, w_t, R_w, ft_v, R_ft, gw, eq, mx, R_sel):
        nc, P = self.nc, self.P
        npast = i
        if npast < 1:
            return
        gv = gate_sb[:, tl, :, 0:npast]
        fsl = ft_v[:, :, 15 - npast:15]
        wv = w_t[:, :, 0:npast]
        if npast <= 3:
            P.op("dve", lambda: nc.vector.tensor_copy(out=wv, in_=fsl), reads=[R_ft], writes=[R_w])
            return
        gwv = gw[:, :, 0:npast]
        eqv = eq[:, :, 0:npast]
        mb = mx[:].unsqueeze(2).to_broadcast([128, NH, npast])
        rs = [R_gate, R_sel]
        P.op("dve", lambda: nc.vector.tensor_reduce(out=mx[:], in_=gv, axis=AX.X, op=ALU.max), reads=rs, writes=[R_sel])
        P.op("dve", lambda: nc.vector.tensor_tensor(out=eqv, in0=gv, in1=mb, op=ALU.is_ge), reads=rs, writes=[R_sel])
        P.op("dve", lambda: nc.vector.scalar_tensor_tensor(out=gwv, in0=eqv, scalar=NEG, in1=gv, op0=ALU.mult, op1=ALU.add), reads=rs, writes=[R_sel])
        P.op("dve", lambda: nc.vector.tensor_reduce(out=mx[:], in_=gwv, axis=AX.X, op=ALU.max), reads=rs, writes=[R_sel])
        P.op("dve", lambda: nc.vector.tensor_tensor(out=eqv, in0=gwv, in1=mb, op=ALU.is_ge), reads=rs, writes=[R_sel])
        P.op("dve", lambda: nc.vector.scalar_tensor_tensor(out=gwv, in0=eqv, scalar=NEG, in1=gwv, op0=ALU.mult, op1=ALU.add), reads=rs, writes=[R_sel])
        P.op("dve", lambda: nc.vector.tensor_reduce(out=mx[:], in_=gwv, axis=AX.X, op=ALU.max), reads=rs, writes=[R_sel])
        P.op("dve", lambda: nc.vector.tensor_tensor(out=eqv, in0=gv, in1=mb, op=ALU.is_ge), reads=rs, writes=[R_sel])
        P.op("dve", lambda: nc.vector.tensor_tensor(out=wv, in0=eqv, in1=fsl, op=ALU.mult), reads=[R_sel, R_ft], writes=[R_w])

    def phase_moba(self, g, qoT, R_qo, kTa, R_kTa, Va, R_Va, gate_sb, R_gate, tabs=None):
        nc, P = self.nc, self.P
        with ExitStack() as es:
            if tabs is not None:
                tabF, R_tabF = tabs["tab_full_l"]
                tabO, R_tabO = tabs["tab_own_l"]
            else:
                tabF, R_tabF = self.load_tab(es, "mbF", "tab_full_l")
                tabO, R_tabO = self.load_tab(es, "mbO", "tab_own_l")
            ft, R_ft = self.load_bc(es, "mbft", self.dram["ftab"][0:1, :])
            ft_v = ft[:].rearrange("p (h k) -> p h k", k=15)
            pSrot = self.bufs(es, "mbS", [128, 2, 512], F32, 2, psum=True)
            pOrot = self.bufs(es, "mbOp", [128, 4, VW], F32, 2, psum=True)
            ptrot = self.bufs(es, "mbpT", [128, 4, 128], BF16, 2, psum=True)
            pterot = self.bufs(es, "mbpte", [128, 2, 512], BF16, 3)
            ptmrot = self.bufs(es, "mbptm", [128, 2, 512], BF16, 3)
            orot = self.bufs(es, "mbo", [128, D], BF16, 2)
            accrot = self.bufs(es, "mbacc", [128, 4, VW], F32, 3)
            denrot = self.bufs(es, "mbden", [128, 4], F32, 3)
            wrot = self.bufs(es, "mbw", [128, NH, 15], F32, 2)
            gw = self.alloc(es, "mbgw", [128, NH, 15], F32)
            eq = self.alloc(es, "mbeq", [128, NH, 15], F32)
            mx = self.alloc(es, "mbmx", [128, NH], F32)
            R_sel = Res("selwork")
            units = []
            for tl in range(GT):
                i = g * GT + tl
                npast = i
                tctx = {}
                batches = [(kc, u, c0) for kc in range(2) for u in range(2) for c0 in (8 * kc, 8 * kc + 4)]
                for bi, (kc, u, c0) in enumerate(batches):
                    bctx = {}
                    blocks = [i] + list(range(npast))
                    for ji, j in enumerate(blocks):
                        st = {}

                        def score(tl=tl, i=i, kc=kc, u=u, c0=c0, j=j, st=st, tctx=tctx, bctx=bctx,
                                  first_tile=(bi == 0 and ji == 0), first_batch=(ji == 0), mul_eng="dve"):
                            if first_tile:
                                tctx["o"] = orot.next()
                                tctx["w"] = wrot.next()
                                self.moba_select(tl, i, gate_sb, R_gate, tctx["w"][0], tctx["w"][1], ft_v, R_ft, gw, eq, mx, R_sel)
                            if first_batch:
                                bctx["acc"] = accrot.next()
                            own = (j == i)
                            tb, R_tb = (tabO, R_tabO) if own else (tabF, R_tabF)
                            pS, R_pS = pSrot.next()
                            pte, R_pte = pterot.next()
                            st["ptm"] = ptmrot.next()
                            self.score_block(pS, R_pS, kTa, R_kTa[j], R_kTa[16 + j], j * 128, (16 + j) * 128, kc, u, qoT, R_qo[tl], c0, tl,
                                             tb[:, :, c0:c0 + 4, u, :], R_tb, pte, R_pte, st["ptm"][0], st["ptm"][1], mul_eng=mul_eng)

                        def post(tl=tl, i=i, kc=kc, u=u, c0=c0, j=j, st=st, tctx=tctx, bctx=bctx,
                                 last_batch=(ji == len(blocks) - 1), last_tile=(bi == len(batches) - 1 and ji == len(blocks) - 1)):
                            grp = 2 * kc + u
                            ptm, R_ptm = st["ptm"]
                            o_t, R_o = tctx["o"]
                            w_t, R_w = tctx["w"]
                            acc, R_acc = bctx["acc"]
                            o_v = o_t[:].rearrange("p (c u d) -> p c u d", u=2, d=64)
                            pO, R_pO = pOrot.next()
                            self.pv_block(pO, R_pO, ptm, R_ptm, Va, j, 16 + j, R_Va[j], R_Va[16 + j], grp)
                            if j == i:
                                P.op("act", lambda: nc.scalar.copy(out=acc[:, :, 0:65], in_=pO[:, :, 0:65]), reads=[R_pO], writes=[R_acc])
                            else:
                                for hh in range(4):
                                    hi = (c0 + hh) * 2 + u
                                    P.op("dve", lambda hh=hh, hi=hi: nc.vector.scalar_tensor_tensor(
                                        out=acc[:, hh, 0:65], in0=pO[:, hh, 0:65], scalar=w_t[:, hi, j:j + 1], in1=acc[:, hh, 0:65], op0=ALU.mult, op1=ALU.add),
                                        reads=[R_pO, R_w, R_acc], writes=[R_acc])
                            if last_batch:
                                den, R_den = denrot.next()
                                P.op("dve", lambda: nc.vector.reciprocal(out=den[:], in_=acc[:, :, 64]), reads=[R_acc], writes=[R_den])
                                P.op("dve", lambda: nc.vector.tensor_tensor(out=o_v[:, c0:c0 + 4, u, :], in0=acc[:, :, 0:64],
                                                                            in1=den[:].unsqueeze(2).to_broadcast([128, 4, 64]), op=ALU.mult),
                                     reads=[R_acc, R_den], writes=[R_o])
                            if last_tile:
                                self.finish_tile(o_t, R_o, qoT, R_qo[tl], tl, ptrot)
                        units.append((score, post))
            self.run_pipelined(units)
        P.barrier()

    def prefetch_wo(self, es, wo_name):
        wo = self.alloc(es, "p3wo", [128, 16, D], BF16)
        R_wo = [Res(f"wo{n}") for n in range(4)]
        for nb in range(4):
            self.P.dma("pool", wo[:, :, nb * 512:(nb + 1) * 512], self.wslab_src(wo_name, nb * 512, (nb + 1) * 512), writes=[R_wo[nb]])
        return wo, R_wo

    def phase_oproj(self, g, qoT, R_qo, wo_name, gain_row, xsrc, xdst, wo_pre=None):
        nc, P = self.nc, self.P
        with ExitStack() as es:
            if wo_pre is not None:
                wo, R_wo = wo_pre
            else:
                wo, R_wo = self.prefetch_wo(es, wo_name)
            gbc, R_g = self.load_bc(es, "p3g", self.grow(gain_row))
            xrot = self.bufs(es, "p3x", [128, D], F32, 2)
            tmprot = self.bufs(es, "p3tmp", [128, D], F32, 2)
            xorot = self.bufs(es, "p3xo", [128, D], F32, 2)
            strot = self.bufs(es, "p3st", [128, 8], F32, 3)
            junk = self.alloc(es, "p3junk", [128, 512], BF16)
            R_junk = Res("junk")
            pmrot = self.bufs(es, "p3pm", [128, 512], F32, 8, psum=True)
            for tl in range(GT):
                i = g * GT + tl
                x, R_x = xrot.next()
                P.dma("sp", x[:], self.dram[xsrc][i, :, :], reads=[self.dr(xsrc, i)], writes=[R_x])
                st, R_st = strot.next()
                pms = []
                for nb in range(4):
                    pm, R_pm = pmrot.next()
                    pms.append((pm, R_pm))
                    for k in range(16):
                        P.op("pe", lambda k=k, nb=nb, pm=pm: nc.tensor.matmul(pm[:], lhsT=qoT[:, k, tl * 128:(tl + 1) * 128], rhs=wo[:, k, nb * 512:(nb + 1) * 512], start=(k == 0), stop=(k == 15)),
                             reads=[R_qo[tl], R_wo[nb]], writes=[R_pm], inc=(k == 15))
                    P.op("act", lambda nb=nb, pm=pm, st=st: nc.scalar.activation(out=junk[:], in_=pm[:], func=AF.Square, scale=RS, accum_out=st[:, 2 + nb:3 + nb]),
                         reads=[R_pm], writes=[R_st])
                P.op("dve", lambda st=st: nc.vector.tensor_reduce(out=st[:, 1:2], in_=st[:, 2:6], axis=AX.X, op=ALU.add), reads=[R_st], writes=[R_st])
                self.rstd_from_ms(st[:, 1:2], R_st, st, R_st)
                tmp, R_tmp = tmprot.next()
                for nb in range(4):
                    pm, R_pm = pms[nb]
                    P.op("dve", lambda nb=nb, pm=pm, st=st, tmp=tmp: nc.vector.scalar_tensor_tensor(out=tmp[:, nb * 512:(nb + 1) * 512], in0=pm[:], scalar=st[:, 0:1],
                                                                                                in1=gbc[:, nb * 512:(nb + 1) * 512], op0=ALU.mult, op1=ALU.mult),
                         reads=[R_pm, R_st, R_g], writes=[R_tmp])
                xo, R_xo = xorot.next()
                P.op("pool", lambda tmp=tmp, x=x, xo=xo: nc.gpsimd.tensor_tensor(out=xo[:], in0=tmp[:], in1=x[:], op=ALU.add), reads=[R_tmp, R_x], writes=[R_xo])
                P.dma("sp", self.dram[xdst][i, :, :], xo[:], reads=[R_xo], writes=[self.dr(xdst, i)])
        P.barrier()

    def phase_mlp(self, g, wup_name, wdn_name, gain_pre, gain_post, xsrc, xdst):
        nc, P = self.nc, self.P
        with ExitStack() as es0:
            h2T = self.alloc(es0, "p4h2T", [128, 16, GTOK], BF16)
            R_h2T = [Res(f"h2T{t}") for t in range(GT)]
            yacc = self.alloc(es0, "p4y", [128, GT, D], F32)
            R_y = [Res(f"y{t}") for t in range(GT)]
            esW = ExitStack()
            wuprot = self.bufs(esW, "p4wu", [128, 16, 512], BF16, 2)
            wdnrot = self.bufs(esW, "p4wd", [128, 4, D], BF16, 2)
            NS = DFF // 512
            wup_b, wdn_b, aT_b = {}, {}, {}
            wdn_src = self.dram[wdn_name].rearrange("(s c p) n -> s p c n", p=128, c=4)

            def load_up(s):
                if s < NS:
                    w, R_w = wuprot.next()
                    P.dma("pool", w[:], self.wslab_src(wup_name, s * 512, (s + 1) * 512), writes=[R_w])
                    wup_b[s] = (w, R_w)

            def load_dn(s):
                if s < NS:
                    w, R_w = wdnrot.next()
                    P.dma("pool", w[:], wdn_src[s], writes=[R_w])
                    wdn_b[s] = (w, R_w)
            load_up(0)
            load_dn(0)
            load_up(1)
            load_dn(1)
            with ExitStack() as es:
                xrot = self.bufs(es, "p4x", [128, D], F32, 2)
                hrot = self.bufs(es, "p4h", [128, D], BF16, 2)
                strot = self.bufs(es, "p4st", [128, 2], F32, 4)
                junk = self.alloc(es, "p4junk", [128, D], BF16)
                R_junk = Res("junk")
                gbc, R_g = self.load_bc(es, "p4g", self.grow(gain_pre))
                ptrot = self.bufs(es, "p4pT", [128, 4, 128], BF16, 4, psum=True)
                for tl in range(GT):
                    i = g * GT + tl
                    h, R_h, _, _ = self.norm_tile(self.dram[xsrc][i, :, :], self.dr(xsrc, i), xrot, hrot, strot, junk, R_junk, gbc, R_g)
                    self.transpose_tile(h, R_h, lambda cq, tl=tl: h2T[:, cq * 4:(cq + 1) * 4, tl * 128:(tl + 1) * 128], R_h2T[tl], ptrot)
            P.barrier()
            with ExitStack() as es:
                aTrot = self.bufs(es, "p4aT", [128, 4, GTOK], BF16, 2)
                rrot = self.bufs(es, "p4r", [128, 512], F32, 3)
                purot = self.bufs(es, "p4pu", [128, 512], F32, 3, psum=True)
                pdrot = self.bufs(es, "p4pd", [128, 512], F32, 4, psum=True)
                def up(s):
                    w, R_w = wup_b.pop(s)
                    aT, R_aT = aTrot.next()
                    aT_b[s] = (aT, R_aT)
                    for cc in range(4):
                        for tb in range(2):
                            pu, R_pu = purot.next()
                            for k in range(16):
                                P.op("pe", lambda k=k, cc=cc, tb=tb, pu=pu: nc.tensor.matmul(pu[:], lhsT=w[:, k, cc * 128:(cc + 1) * 128], rhs=h2T[:, k, tb * 512:(tb + 1) * 512], start=(k == 0), stop=(k == 15)),
                                     reads=[R_w] + R_h2T[tb * 4:(tb + 1) * 4], writes=[R_pu], inc=(k == 15))
                            r, R_r = rrot.next()
                            P.op("act", lambda pu=pu, r=r: nc.scalar.activation(out=r[:], in_=pu[:], func=AF.Relu), reads=[R_pu], writes=[R_r])
                            P.op("dve", lambda r=r, cc=cc, tb=tb, aT=aT: nc.vector.tensor_tensor(out=aT[:, cc, tb * 512:(tb + 1) * 512], in0=r[:], in1=r[:], op=ALU.mult),
                                 reads=[R_r], writes=[R_aT])

                def down(s):
                    w, R_w = wdn_b.pop(s)
                    aT, R_aT = aT_b.pop(s)
                    for tl in range(GT):
                        for nb in range(4):
                            pd, R_pd = pdrot.next()
                            for cc in range(4):
                                P.op("pe", lambda cc=cc, nb=nb, tl=tl, pd=pd: nc.tensor.matmul(pd[:], lhsT=aT[:, cc, tl * 128:(tl + 1) * 128], rhs=w[:, cc, nb * 512:(nb + 1) * 512], start=(cc == 0), stop=(cc == 3)),
                                     reads=[R_aT, R_w], writes=[R_pd], inc=(cc == 3))
                            ydst = yacc[:, tl, nb * 512:(nb + 1) * 512]
                            if s == 0:
                                P.op("act", lambda pd=pd, ydst=ydst: nc.scalar.copy(out=ydst, in_=pd[:]), reads=[R_pd], writes=[R_y[tl]])
                            else:
                                P.op("dve", lambda pd=pd, ydst=ydst: nc.vector.tensor_tensor(out=ydst, in0=pd[:], in1=ydst, op=ALU.add), reads=[R_pd, R_y[tl]], writes=[R_y[tl]])

                up(0)
                for s in range(NS):
                    if s + 1 < NS:
                        up(s + 1)
                    load_up(s + 2)
                    down(s)
                    load_dn(s + 2)
            esW.close()
            P.barrier()
            with ExitStack() as es:
                xrot = self.bufs(es, "p4x2", [128, D], F32, 3)
                tmprot = self.bufs(es, "p4tmp", [128, D], F32, 3)
                xorot = self.bufs(es, "p4xo", [128, D], F32, 3)
                strot = self.bufs(es, "p4st2", [128, 2], F32, 4)
                junk = self.alloc(es, "p4junk2", [128, D], BF16)
                R_junk = Res("junk")
                gbc, R_g = self.load_bc(es, "p4g2", self.grow(gain_post))
                for tl in range(GT):
                    i = g * GT + tl
                    x, R_x = xrot.next()
                    P.dma("sp", x[:], self.dram[xsrc][i, :, :], reads=[self.dr(xsrc, i)], writes=[R_x])
                    st, R_st = strot.next()
                    P.op("act", lambda tl=tl, st=st: nc.scalar.activation(out=junk[:], in_=yacc[:, tl, :], func=AF.Square, scale=RS, accum_out=st[:, 1:2]),
                         reads=[R_y[tl]], writes=[R_st])
                    self.rstd_from_ms(st[:, 1:2], R_st, st, R_st)
                    tmp, R_tmp = tmprot.next()
                    P.op("dve", lambda tl=tl, st=st, tmp=tmp: nc.vector.scalar_tensor_tensor(out=tmp[:], in0=yacc[:, tl, :], scalar=st[:, 0:1], in1=gbc[:], op0=ALU.mult, op1=ALU.mult),
                         reads=[R_y[tl], R_st, R_g], writes=[R_tmp])
                    xo, R_xo = xorot.next()
                    P.op("pool", lambda tmp=tmp, x=x, xo=xo: nc.gpsimd.tensor_tensor(out=xo[:], in0=tmp[:], in1=x[:], op=ALU.add), reads=[R_tmp, R_x], writes=[R_xo])
                    P.dma("sp", self.dram[xdst][i, :, :], xo[:], reads=[R_xo], writes=[self.dr(xdst, i)])
        P.barrier()

    def phase_kvshared(self, xsrc, kdst, vdst, ksdst, R_dst, ntiles=NT):
        nc, P = self.nc, self.P
        with ExitStack() as es:
            xrot = self.bufs(es, "p5x", [128, D], F32, 4)
            hrot = self.bufs(es, "p5h", [128, D], BF16, 4)
            strot = self.bufs(es, "p5st", [128, 2], F32, 6)
            junk = self.alloc(es, "p5junk", [128, D], BF16)
            R_junk = Res("junk")
            hTrot = self.bufs(es, "p5hT", [128, 16, 512], BF16, 2)
            gbc, R_g = self.load_bc(es, "p5g", self.grow(4))
            ptrot = self.bufs(es, "p5pT", [128, 4, 128], BF16, 4, psum=True)
            pmrot = self.bufs(es, "p5pm", [128, 512], F32, 3, psum=True)
            wkv = self.alloc(es, "p5wkv", [128, 16, 512], BF16)
            R_wkv = Res("wkvs")
            P.dma("pool", wkv[:], self.wslab_src("wkvs", 0, 512), writes=[R_wkv])
            ksum = self.alloc(es, "p5ksum", [128, 2, ntiles], F32)
            R_ksum = Res("ksum")
            kstrot = self.bufs(es, "p5kst", [128, 512], F32, 2)
            vstrot = self.bufs(es, "p5vst", [128, 256], F32, 2)
            for b in range(ntiles // 4):
                hT, R_hT = hTrot.next()
                for t in range(4):
                    i = b * 4 + t
                    h, R_h, _, _ = self.norm_tile(self.dram[xsrc][i, :, :], self.dr(xsrc, i), xrot, hrot, strot, junk, R_junk, gbc, R_g)
                    self.transpose_tile(h, R_h, lambda cq, hT=hT, t=t: hT[:, cq * 4:(cq + 1) * 4, t * 128:(t + 1) * 128], R_hT, ptrot)
                for kc in range(2):
                    pm, R_pm = pmrot.next()
                    for k in range(16):
                        P.op("pe", lambda k=k, kc=kc, pm=pm, hT=hT: nc.tensor.matmul(pm[:], lhsT=wkv[:, k, kc * 128:(kc + 1) * 128], rhs=hT[:, k, :], start=(k == 0), stop=(k == 15)),
                             reads=[R_wkv, R_hT], writes=[R_pm], inc=(k == 15))
                    kst, R_kst = kstrot.next()
                    P.op("act", lambda pm=pm, kst=kst: nc.scalar.copy(out=kst[:], in_=pm[:]), reads=[R_pm], writes=[R_kst])
                    P.op("dve", lambda pm=pm, kc=kc, b=b: nc.vector.tensor_reduce(out=ksum[:, kc, b * 4:(b + 1) * 4], in_=pm[:].rearrange("p (a t) -> p a t", a=4), axis=AX.X, op=ALU.add),
                         reads=[R_pm, R_ksum], writes=[R_ksum])
                    P.dma("sp", kdst(kc, b), kst[:], reads=[R_kst], writes=[R_dst])
                for t in range(4):
                    i = b * 4 + t
                    pm, R_pm = pmrot.next()
                    for k in range(16):
                        P.op("pe", lambda k=k, t=t, pm=pm, hT=hT: nc.tensor.matmul(pm[:, 0:256], lhsT=hT[:, k, t * 128:(t + 1) * 128], rhs=wkv[:, k, 256:512], start=(k == 0), stop=(k == 15)),
                             reads=[R_wkv, R_hT], writes=[R_pm], inc=(k == 15))
                    vst, R_vst = vstrot.next()
                    P.op("dve", lambda pm=pm, vst=vst: nc.vector.tensor_copy(out=vst[:], in_=pm[:, 0:256]), reads=[R_pm], writes=[R_vst])
                    P.dma("sp", vdst(i), vst[:], reads=[R_vst], writes=[R_dst])
            P.dma("sp", ksdst, ksum[:], reads=[R_ksum], writes=[R_dst])
        P.barrier()

    def layer0(self, xin, x1, x2, ngroups=NG, special=None, tabs=None):
        nc, P = self.nc, self.P
        for g in range(ngroups):
            with ExitStack() as es:
                qoT = self.alloc(es, "qoT", [128, 16, GTOK], BF16)
                R_qo = [Res(f"qo{t}") for t in range(GT)]
                kT = self.alloc(es, "kT", [128, 2, 2 * GTOK], BF16)
                R_kT = [Res(f"kT{t}") for t in range(2 * GT)]
                Vaug = self.alloc(es, "Vaug", [128, 2 * GT, 4, VW], BF16)
                R_V = [Res(f"V{t}") for t in range(2 * GT)]
                tabs = {}
                tab_t = self.alloc(es, "swtab", [128, 2, 16, 2, 128], BF16)
                tabs["tab_swa"] = (tab_t, Res("swtab"))

                def load_tab_now(tab_t=tab_t, tabs=tabs):
                    P.dma("pool", tab_t[:].rearrange("p b c u t -> p b (c u t)"), self.dram["tab_swa"][:, :, :], writes=[tabs["tab_swa"][1]])
                self.phase_qkv(g, 0, xin, 0, "wq0", qoT, R_qo, kT=kT, R_kT=R_kT, Vaug=Vaug, R_V=R_V, xprev=self.dram["xprev"], wkv_name="wkv0",
                               after_first_loads=load_tab_now)
                wo_pre = self.prefetch_wo(es, "wo0")
                self.phase_swa(g, qoT, R_qo, kT, R_kT, Vaug, R_V, special=special, tabs=tabs)
                self.phase_oproj(g, qoT, R_qo, "wo0", 1, xin, x1, wo_pre=wo_pre)
            P.barrier()
            self.phase_mlp(g, "wup0", "wdn0", 2, 3, x1, x2)

    def load_kv_host(self, kt_src, v_src, ksa, ksb):
        def f(kTa, R_kTa, Va, R_Va, kmean, R_kmean, ksb_t, R_ksb):
            P = self.P
            P.dma("pool", kTa[:], kt_src, writes=R_kTa)
            v4 = v_src.rearrange("p t (g d) -> p t g d", g=4)
            for q4 in range(4):
                P.dma("pool", Va[:, q4 * 8:(q4 + 1) * 8, :, 0:64], v4[:, q4 * 8:(q4 + 1) * 8, :, :], writes=R_Va[q4 * 8:(q4 + 1) * 8])
            P.dma("sp", kmean[:], ksa, writes=[R_kmean])
            P.dma("sp", ksb_t[:], ksb, writes=[R_ksb])
        return f

    def load_kv_gathered(self, ex, R_ex):
        def f(kTa, R_kTa, Va, R_Va, kmean, R_kmean, ksb_t, R_ksb):
            P = self.P
            kv = kTa[:].rearrange("p c (i r t) -> p c i r t", r=2, t=128)
            vv = Va[:].rearrange("p (i r) g w -> p i r g w", r=2)
            for r in range(2):
                rows = ex[r * 128:(r + 1) * 128, :]
                P.dma("pool", kv[:, :, :, r, :], rows[:, 0:4096].rearrange("p (c i t) -> p c i t", c=2, i=16), reads=[R_ex], writes=R_kTa)
                vsrc = rows[:, 4096:8192].rearrange("p (i g d) -> p i g d", g=4, d=64)
                for gq in range(4):
                    P.dma("pool", vv[:, :, r, gq, 0:64], vsrc[:, :, gq, :], reads=[R_ex], writes=R_Va)
            P.dma("sp", kmean[:], ex[0:128, 8192:8224].rearrange("p (c j) -> p c j", c=2), reads=[R_ex], writes=[R_kmean])
            P.dma("sp", ksb_t[:], ex[128:256, 8192:8224].rearrange("p (c j) -> p c j", c=2), reads=[R_ex], writes=[R_ksb])
        return f

    def layer1(self, xin, x3, xout, loader, tabs=None):
        nc, P = self.nc, self.P
        for g in range(NG):
            with ExitStack() as es:
                kTa = self.alloc(es, "kTa", [128, 2, 32 * 128], BF16)
                R_kTa = [Res(f"kTa{t}") for t in range(32)]
                Va = self.alloc(es, "Va", [128, 32, 4, VW], BF16)
                R_Va = [Res(f"Va{t}") for t in range(32)]
                kmean = self.alloc(es, "kmean", [128, 2, 16], F32)
                R_kmean = Res("kmean")
                ksb_t = self.alloc(es, "ksb_t", [128, 2, 16], F32)
                R_ksb = Res("ksb")
                loader(kTa, R_kTa, Va, R_Va, kmean, R_kmean, ksb_t, R_ksb)
                P.op("pool", lambda: nc.gpsimd.memset(Va[:, :, :, 64:65], 1.0), reads=R_Va, writes=R_Va)
                P.op("dve", lambda: nc.vector.tensor_tensor(out=kmean[:], in0=kmean[:], in1=ksb_t[:], op=ALU.add), reads=[R_kmean, R_ksb], writes=[R_kmean])
                P.op("dve", lambda: nc.vector.tensor_scalar(out=kmean[:], in0=kmean[:], scalar1=1.0 / 256.0, scalar2=None, op0=ALU.mult), reads=[R_kmean], writes=[R_kmean])
                qoT = self.alloc(es, "qoT1", [128, 16, GTOK], BF16)
                R_qo = [Res(f"qo{t}") for t in range(GT)]
                gate_sb = self.alloc(es, "gate", [128, GT, NH, 16], F32)
                R_gate = Res("gate")
                self.phase_qkv(g, 1, xin, 0, "wq1", qoT, R_qo, gate_sb=gate_sb, R_gate=R_gate, kmean=kmean, R_kmean=R_kmean)
                self.phase_moba(g, qoT, R_qo, kTa, R_kTa, Va, R_Va, gate_sb, R_gate, tabs=tabs)
                self.phase_oproj(g, qoT, R_qo, "wo1", 1, xin, x3)
            P.barrier()
            self.phase_mlp(g, "wup1", "wdn1", 2, 3, x3, xout)
        P.barrier()


def build_stage1():
    nc = bass.Bass("TRN2", target_bir_lowering=False)
    B = Builder(nc)
    B.din("xmine", [NT, 128, D])
    B.din("xprev", [NT, 128, D])
    B.din("wq0", [D, D])
    B.din("wkv0", [D, 512])
    B.din("wo0", [D, D])
    B.din("wup0", [D, DFF])
    B.din("wdn0", [DFF, D])
    B.din("wkvs", [D, 512])
    B.din("gains", [5, D])
    B.din("sinks", [1, NH])
    B.din("ident", [128, 128])
    B.din("tab_swa", [128, 2, NH * 128])
    B.din("tab_swa0", [128, 2, NH * 128])
    B.dtmp("x1", [NT, 128, D])
    B.dout("x2", [NT, 128, D])
    B.dout("kts", [128, 2, NT * 128])
    B.dout("vs", [128, NT, 256])
    B.dout("ksum", [128, 2, NT])
    with ExitStack() as es:
        B.setup_consts(es)
        B.layer0("xmine", "x1", "x2")
        B.phase_kvshared("x2", lambda kc, b: B.dram["kts"][:, kc, b * 512:(b + 1) * 512], lambda i: B.dram["vs"][:, i, :],
                         B.dram["ksum"][:, :, :], Res("kvout"))
        B.P.barrier()
    return nc


def build_stage2():
    nc = bass.Bass("TRN2", target_bir_lowering=False)
    B = Builder(nc)
    B.din("x2", [NT, 128, D])
    B.din("kt_all", [128, 2, 32 * 128])
    B.din("v_all", [128, 32, 256])
    B.din("ksa", [128, 2, 16])
    B.din("ksb", [128, 2, 16])
    B.din("wq1", [D, D])
    B.din("wo1", [D, D])
    B.din("wup1", [D, DFF])
    B.din("wdn1", [DFF, D])
    B.din("gains", [4, D])
    B.din("ident", [128, 128])
    B.din("tab_full", [128, 2, NH * 128])
    B.din("tab_own", [128, 2, NH * 128])
    B.din("ftab", [1, NH * 15])
    B.dtmp("x3", [NT, 128, D])
    B.dout("out", [NT, 128, D])
    with ExitStack() as es:
        B.setup_consts(es)
        B.layer1("x2", "x3", "out", B.load_kv_host(B.dram["kt_all"][:, :, :], B.dram["v_all"][:, :, :], B.dram["ksa"][:, :, :], B.dram["ksb"][:, :, :]))
        B.P.barrier()
    return nc


def core_tiles(x, b, p):
    xb = x[b].reshape(32, 128, D)
    mine = np.ascontiguousarray(xb[p::2])
    prev = np.zeros_like(mine)
    for i in range(NT):
        gt = 2 * i + p - 1
        if gt >= 0:
            prev[i] = xb[gt]
    return mine, prev


def stage1_inputs(inputs):
    perm = q_perm()
    wqkv = inputs["w_qkv_a"][0]
    common = {
        "wq0": np.ascontiguousarray(wqkv[:, :D][:, perm]),
        "wkv0": np.ascontiguousarray(wqkv[:, D:]),
        "wo0": np.ascontiguousarray(inputs["w_o_a"][0][perm, :]),
        "wup0": np.ascontiguousarray(inputs["w_up"][0]),
        "wdn0": np.ascontiguousarray(inputs["w_down"][0]),
        "wkvs": np.ascontiguousarray(inputs["w_kv_shared"]),
        "gains": np.ascontiguousarray(np.stack([inputs["norm_attn_pre"][0], inputs["norm_attn_post"][0], inputs["norm_mlp_pre"][0],
                                               inputs["norm_mlp_post"][0], inputs["kv_norm"]]).astype(np.float32)),
        "sinks": np.ascontiguousarray(inputs["sinks_a"][0][[head_of(c, u) for c in range(16) for u in range(2)]].reshape(1, NH)),
        "ident": np.eye(128, dtype=np.float32),
    }
    tabs = [build_tables(0), build_tables(1)]
    maps = []
    for c in range(8):
        b, p = divmod(c, 2)
        mine, prev = core_tiles(inputs["x"], b, p)
        m = dict(common)
        m["xmine"] = mine
        m["xprev"] = prev
        m["tab_swa"] = tabs[p]["tab_swa"]
        m["tab_swa0"] = tabs[p]["tab_swa0"]
        maps.append(m)
    return maps


def stage2_inputs(inputs, r1):
    perm = q_perm()
    common = {
        "wq1": np.ascontiguousarray(inputs["w_q_b"][0][:, perm]),
        "wo1": np.ascontiguousarray(inputs["w_o_b"][0][perm, :]),
        "wup1": np.ascontiguousarray(inputs["w_up"][1]),
        "wdn1": np.ascontiguousarray(inputs["w_down"][1]),
        "gains": np.ascontiguousarray(np.stack([inputs["norm_attn_pre"][1], inputs["norm_attn_post"][1], inputs["norm_mlp_pre"][1],
                                               inputs["norm_mlp_post"][1]]).astype(np.float32)),
        "ident": np.eye(128, dtype=np.float32),
    }
    tabs = [build_tables(0), build_tables(1)]
    maps = []
    for c in range(8):
        b, p = divmod(c, 2)
        ra, rb = r1[2 * b], r1[2 * b + 1]
        kt_all = np.zeros((128, 2, 32, 128), dtype=np.float32)
        kt_all[:, :, 0::2, :] = ra["kts"].reshape(128, 2, NT, 128)
        kt_all[:, :, 1::2, :] = rb["kts"].reshape(128, 2, NT, 128)
        v_all = np.zeros((128, 32, 256), dtype=np.float32)
        v_all[:, 0::2, :] = ra["vs"]
        v_all[:, 1::2, :] = rb["vs"]
        m = dict(common)
        m["x2"] = np.ascontiguousarray(r1[c]["x2"])
        m["kt_all"] = kt_all.reshape(128, 2, 32 * 128)
        m["v_all"] = v_all
        m["ksa"] = np.ascontiguousarray(ra["ksum"])
        m["ksb"] = np.ascontiguousarray(rb["ksum"])
        m["tab_full"] = tabs[p]["tab_full"]
        m["tab_own"] = tabs[p]["tab_own"]
        m["ftab"] = tabs[p]["ftab"]
        maps.append(m)
    return maps


def assemble(outs):
    res = np.zeros((4, 32, 128, D), dtype=np.float32)
    for c in range(8):
        b, p = divmod(c, 2)
        res[b, p::2] = outs[c]
    return res.reshape(4, 4096, D)


NT0 = 32


def build_fused(n_cores=8):
    nc = bass.Bass("TRN2", target_bir_lowering=False)
    B = Builder(nc)
    B.din("xall", [NT0, 128, D])
    B.din("xprev", [NT0, 128, D])
    for nm, shp in (("wq0", [D, D]), ("wkv0", [D, 512]), ("wo0", [D, D]), ("wup0", [D, DFF]), ("wdn0", [DFF, D]), ("wkvs", [D, 512]),
                    ("wq1", [D, D]), ("wo1", [D, D]), ("wup1", [D, DFF]), ("wdn1", [DFF, D])):
        B.din(nm, shp)
    B.din("gains", [9, D])
    B.din("sinks", [1, NH])
    B.din("ident", [128, 128])
    for nm in ("tab_swa", "tab_swaA", "tab_swaB", "tab_full_l", "tab_own_l"):
        B.din(nm, [128, 2, NH * 128])
    B.din("ftab", [1, NH * 15])
    B.dtmp("x1", [NT0, 128, D])
    B.dtmp("x2", [NT0, 128, D])
    B.dtmp("x3", [NT, 128, D])
    kts = B.dtmp("kts", [128, 2, NT0 * 128])
    vs = B.dtmp("vs", [128, NT0, 256])
    ksum = B.dtmp("ksum", [128, 2, NT0])
    B.dout("out", [NT, 128, D])
    with ExitStack() as es:
        B.setup_consts(es)
        B.gbase = 0
        B.layer0("xall", "x1", "x2", ngroups=NT0 // GT, special={0: "tab_swaA", 16: "tab_swaB"})
        B.phase_kvshared("x2", lambda kc, b: kts[:, kc, b * 512:(b + 1) * 512], lambda i: vs[:, i, :], ksum[:, :, :], Res("kvout"), ntiles=NT0)
        B.gbase = 5
        B.layer1("x2", "x3", "out", B.load_kv_host(kts[:, :, :], vs[:, :, :], ksum[:, :, 0:16], ksum[:, :, 16:32]))
        B.P.barrier()
    return nc


def local_tiles(x, b, p):
    xb = x[b].reshape(32, 128, D)
    order = list(range(p, 32, 2)) + list(range(1 - p, 32, 2))
    xall = np.ascontiguousarray(xb[order])
    prev = np.zeros_like(xall)
    for L, gt in enumerate(order):
        if gt >= 1:
            prev[L] = xb[gt - 1]
    return xall, prev


def fused_inputs(inputs):
    perm = q_perm()
    wqkv = inputs["w_qkv_a"][0]
    common = {
        "wq0": np.ascontiguousarray(wqkv[:, :D][:, perm]),
        "wkv0": np.ascontiguousarray(wqkv[:, D:]),
        "wo0": np.ascontiguousarray(inputs["w_o_a"][0][perm, :]),
        "wup0": np.ascontiguousarray(inputs["w_up"][0]),
        "wdn0": np.ascontiguousarray(inputs["w_down"][0]),
        "wkvs": np.ascontiguousarray(inputs["w_kv_shared"]),
        "wq1": np.ascontiguousarray(inputs["w_q_b"][0][:, perm]),
        "wo1": np.ascontiguousarray(inputs["w_o_b"][0][perm, :]),
        "wup1": np.ascontiguousarray(inputs["w_up"][1]),
        "wdn1": np.ascontiguousarray(inputs["w_down"][1]),
        "gains": np.ascontiguousarray(np.stack([inputs["norm_attn_pre"][0], inputs["norm_attn_post"][0], inputs["norm_mlp_pre"][0],
                                               inputs["norm_mlp_post"][0], inputs["kv_norm"], inputs["norm_attn_pre"][1],
                                               inputs["norm_attn_post"][1], inputs["norm_mlp_pre"][1], inputs["norm_mlp_post"][1]]).astype(np.float32)),
        "sinks": np.ascontiguousarray(inputs["sinks_a"][0][[head_of(c, u) for c in range(16) for u in range(2)]].reshape(1, NH)),
        "ident": np.eye(128, dtype=np.float32),
    }
    tabs = [build_tables(0), build_tables(1)]
    maps = []
    for c in range(8):
        b, p = divmod(c, 2)
        m = dict(common)
        m["xall"], m["xprev"] = local_tiles(inputs["x"], b, p)
        for nm in ("tab_swa", "tab_swaA", "tab_swaB", "tab_full_l", "tab_own_l", "ftab"):
            m[nm] = tabs[p][nm]
        maps.append(m)
    return maps


def kernel(**inputs):
    inputs = {k: np.asarray(v) for k, v in inputs.items()}
    nc = build_fused(8)
    r = run_bass_kernel_spmd(nc, fused_inputs(inputs), core_ids=list(range(8))).results
    return assemble([x["out"] for x in r])
```

```python
import numpy as np
from contextlib import ExitStack
import concourse.bass as bass
import concourse.mybir as mybir
from concourse.bass_utils import run_bass_kernel_spmd

F32 = mybir.dt.float32
BF16 = mybir.dt.bfloat16
AF = mybir.ActivationFunctionType
ALU = mybir.AluOpType
AX = mybir.AxisListType

D = 2048
DFF = 8192
NT = 16
NG = 2
GT = 8
GTOK = GT * 128
NH = 32
EPS = 1e-6
RS = 1.0 / float(np.sqrt(D))
NEG = -1.0e30
VW = 72


class Res:
    __slots__ = ("name", "writers", "readers", "excl")

    def __init__(self, name, excl=False):
        self.name = name
        self.writers = []
        self.readers = []
        self.excl = excl


class Prog:
    def __init__(self, nc, n_dma_sems=8):
        self.nc = nc
        self.eng = {"pe": nc.tensor, "act": nc.scalar, "dve": nc.vector, "pool": nc.gpsimd, "sp": nc.sync}
        self.sem = {}
        self.cnt = {}
        for e in ("pe", "act", "dve", "pool"):
            self.sem[e] = nc.alloc_semaphore(name="s_" + e)
            self.cnt[e] = 0
        self.known = {}
        self.dma_pool = {}
        for q in ("sp", "pool"):
            self.dma_pool[q] = [[nc.alloc_semaphore(name=f"d_{q}{i}"), 0] for i in range(n_dma_sems)]
        self.dma_rr = {"sp": 0, "pool": 0}

    def _wait(self, ename, ev):
        s, v = ev
        key = (ename, s.name)
        if self.known.get(key, 0) >= v:
            return
        self.known[key] = v
        self.eng[ename].wait_ge(s, v)

    def _deps(self, ename, reads, writes):
        evs = {}

        def add(ev):
            s, v = ev
            if s.name not in evs or evs[s.name][1] < v:
                evs[s.name] = ev
        for r in reads:
            for ev in r.writers:
                add(ev)
        for w in writes:
            for ev in w.writers:
                add(ev)
            for ev in w.readers:
                add(ev)
        for ev in evs.values():
            if ename == "pe" and ev[0] is self.sem["pe"]:
                continue
            self._wait(ename, ev)

    def _commit(self, ev, reads, writes):
        for r in reads:
            r.readers.append(ev)
            if len(r.readers) > 48:
                best = {}
                for s, v in r.readers:
                    if s.name not in best or best[s.name][1] < v:
                        best[s.name] = (s, v)
                r.readers = list(best.values())
        for w in writes:
            w.writers = [ev]
            w.readers = []

    def op(self, ename, fn, reads=(), writes=(), inc=True):
        if ename != "pe":
            ex = [r for r in reads if r.excl]
            if ex:
                writes = list(writes) + ex
                reads = [r for r in reads if not r.excl]
        self._deps(ename, reads, writes)
        ins = fn()
        seq = self.cnt[ename] + 1
        if inc:
            ins.then_inc(self.sem[ename], 1)
            self.cnt[ename] = seq
        self._commit((self.sem[ename], seq), reads, writes)
        return ins

    def dma(self, q, out, in_, reads=(), writes=()):
        self._deps(q, reads, writes)
        pool = self.dma_pool[q]
        i = self.dma_rr[q]
        self.dma_rr[q] = (i + 1) % len(pool)
        ent = pool[i]
        if ent[1] > 0:
            self._wait(q, (ent[0], ent[1]))
        ent[1] += 16
        self.eng[q].dma_start(out=out, in_=in_).then_inc(ent[0], 16)
        ev = (ent[0], ent[1])
        self._commit(ev, reads, writes)
        return ev

    def collective(self, fn, reads=(), writes=()):
        q = "pool"
        self._deps(q, reads, writes)
        if not hasattr(self, "cc_sem"):
            self.cc_sem = [self.nc.alloc_semaphore(name="s_cc"), 0]
        ent = self.cc_sem
        if ent[1] > 0:
            self._wait(q, (ent[0], ent[1]))
        ent[1] += 16
        fn().then_inc(ent[0], 16)
        ev = (ent[0], ent[1])
        self._commit(ev, reads, writes)
        return ev

    def barrier(self):
        evs = []
        for e in ("pe", "act", "dve", "pool"):
            if self.cnt[e] > 0:
                evs.append((self.sem[e], self.cnt[e]))
        for q, pool in self.dma_pool.items():
            for s, v in pool:
                if v > 0:
                    evs.append((s, v))
        if hasattr(self, "cc_sem") and self.cc_sem[1] > 0:
            evs.append((self.cc_sem[0], self.cc_sem[1]))
        for e in ("pe", "act", "dve", "pool", "sp"):
            for ev in evs:
                self._wait(e, ev)


class Rot:
    def __init__(self, items):
        self.items = items
        self.i = 0

    def next(self):
        it = self.items[self.i]
        self.i = (self.i + 1) % len(self.items)
        return it


def head_of(c, u):
    return 8 * (2 * (c // 8) + u) + (c % 8)


def q_perm():
    perm = np.zeros(D, dtype=np.int64)
    for c in range(16):
        for u in range(2):
            h = head_of(c, u)
            perm[c * 128 + u * 64: c * 128 + u * 64 + 64] = np.arange(h * 64, h * 64 + 64)
    return perm


def my_slopes():
    sl = np.zeros(NH, dtype=np.float64)
    for c in range(16):
        for u in range(2):
            sl[c * 2 + u] = 2.0 ** (-8.0 * (head_of(c, u) + 1) / NH)
    return sl


def build_tables(p):
    sl = my_slopes()[None, :, None]
    s = np.arange(128, dtype=np.float64)[:, None, None]
    t = np.arange(128, dtype=np.float64)[None, None, :]
    d = t - s + 0.0 * sl
    causal = np.where(d >= 0, np.exp(-sl * d), 0.0)
    swaprev = np.where(d < 0, np.exp(-sl * (d + 128.0)), 0.0)
    full1 = np.exp(-sl * (d + 128.0))
    full0 = np.exp(-sl * (d + 256.0))
    zeros = np.zeros_like(causal)
    f = lambda a: np.ascontiguousarray(a.reshape(128, NH * 128).astype(np.float32))
    tabs = {
        "tab_swa": np.stack([f(swaprev), f(causal)], axis=1),
        "tab_swa0": np.stack([f(zeros if p == 0 else swaprev), f(causal)], axis=1),
        "tab_full": np.stack([f(full0), f(full1)], axis=1),
        "tab_own": np.stack([f(causal if p == 0 else full1), f(zeros if p == 0 else causal)], axis=1),
        "tab_swaA": np.stack([f(zeros if p == 0 else swaprev), f(causal)], axis=1),
        "tab_swaB": np.stack([f(swaprev if p == 0 else zeros), f(causal)], axis=1),
        "tab_full_l": np.stack([f(full0 if p == 0 else full1), f(full1 if p == 0 else full0)], axis=1),
        "tab_own_l": np.stack([f(causal), f(zeros if p == 0 else full1)], axis=1),
    }
    fr = np.zeros((NH, 15), dtype=np.float64)
    for k in range(15):
        delta = 15 - k
        fr[:, k] = np.exp(-my_slopes() * 128.0 * (2 * delta + p - 2))
    tabs["ftab"] = np.ascontiguousarray(fr.reshape(1, NH * 15).astype(np.float32))
    return tabs


class Builder:
    def __init__(self, nc):
        self.nc = nc
        self.P = Prog(nc)
        self.dram = {}
        self.dres = {}

    def din(self, name, shape, dt=F32):
        self.dram[name] = self.nc.dram_tensor(name, list(shape), dt, kind="ExternalInput").ap()
        return self.dram[name]

    def dout(self, name, shape, dt=F32):
        self.dram[name] = self.nc.dram_tensor(name, list(shape), dt, kind="ExternalOutput").ap()
        return self.dram[name]

    def dtmp(self, name, shape, dt=F32):
        self.dram[name] = self.nc.dram_tensor(name, list(shape), dt, kind="Internal").ap()
        return self.dram[name]

    def grow(self, row):
        r = getattr(self, "gbase", 0) + row
        return self.dram["gains"][r:r + 1, :]

    def dr(self, name, idx=0):
        key = (name, idx)
        if key not in self.dres:
            self.dres[key] = Res(f"{name}[{idx}]")
        return self.dres[key]

    def uname(self, name):
        self.uid = getattr(self, "uid", 0) + 1
        return f"{name}_{self.uid}"

    def alloc(self, es, name, shape, dt):
        return es.enter_context(self.nc.sbuf_tensor(self.uname(name), list(shape), dt))

    def palloc(self, es, name, shape, dt=F32):
        return es.enter_context(self.nc.psum_tensor(self.uname(name), list(shape), dt))

    def bufs(self, es, name, shape, dt, n, psum=False):
        items = []
        for i in range(n):
            t = (self.palloc if psum else self.alloc)(es, f"{name}{i}", shape, dt)
            items.append((t, Res(f"{name}{i}", excl=psum)))
        return Rot(items)

    def load_bc(self, es, name, src_row):
        n = src_row.shape[-1]
        t = self.alloc(es, name, [128, n], F32)
        r = Res(name)
        self.P.dma("sp", t[:], src_row.partition_broadcast(128), writes=[r])
        return t, r

    def setup_consts(self, es):
        nc, P = self.nc, self.P
        self.ident = self.alloc(es, "ident", [128, 128], BF16)
        self.R_ident = Res("ident")
        P.dma("pool", self.ident[:], self.dram["ident"][:, :], writes=[self.R_ident])
        self.epst = self.alloc(es, "epst", [128, 1], F32)
        self.R_eps = Res("eps")
        P.op("pool", lambda: nc.gpsimd.memset(self.epst[:], EPS), writes=[self.R_eps])

    def rstd_from_ms(self, ms_ap, R_ms, st, R_st):
        nc, P = self.nc, self.P
        P.op("act", lambda: nc.scalar.activation(out=st[:, 0:1], in_=ms_ap, func=AF.Sqrt, bias=self.epst[:, 0:1], scale=1.0),
             reads=[R_ms, self.R_eps], writes=[R_st])
        P.op("dve", lambda: nc.vector.reciprocal(out=st[:, 0:1], in_=st[:, 0:1]), reads=[R_st], writes=[R_st])

    def norm_tile(self, src_ap, src_res, xrot, hrot, strot, junk, R_junk, gbc, R_g):
        nc, P = self.nc, self.P
        x, R_x = xrot.next()
        h, R_h = hrot.next()
        st, R_st = strot.next()
        P.dma("sp", x[:], src_ap, reads=[src_res], writes=[R_x])
        P.op("act", lambda: nc.scalar.activation(out=junk[:], in_=x[:], func=AF.Square, scale=RS, accum_out=st[:, 1:2]),
             reads=[R_x], writes=[R_st])
        self.rstd_from_ms(st[:, 1:2], R_st, st, R_st)
        P.op("dve", lambda: nc.vector.scalar_tensor_tensor(out=h[:], in0=x[:], scalar=st[:, 0:1], in1=gbc[:], op0=ALU.mult, op1=ALU.mult),
             reads=[R_x, R_st, R_g], writes=[R_h])
        return h, R_h, x, R_x

    def transpose_tile(self, h, R_h, dst_fn, R_dst, ptrot):
        nc, P = self.nc, self.P
        for cq in range(4):
            pT, R_pT = ptrot.next()
            for j in range(4):
                c = cq * 4 + j
                P.op("pe", lambda c=c, j=j, pT=pT: nc.tensor.transpose(out=pT[:, j, :], in_=h[:, c * 128:(c + 1) * 128], identity=self.ident[:]),
                     reads=[R_h, self.R_ident], writes=[R_pT], inc=(j == 3))
            if cq % 2 == 0:
                P.op("act", lambda cq=cq, pT=pT: nc.scalar.copy(out=dst_fn(cq), in_=pT[:]), reads=[R_pT], writes=[R_dst])
            else:
                P.op("dve", lambda cq=cq, pT=pT: nc.vector.tensor_copy(out=dst_fn(cq), in_=pT[:]), reads=[R_pT], writes=[R_dst])

    def wslab_src(self, wname, c0, c1):
        return self.dram[wname].rearrange("(k p) n -> p k n", p=128)[:, :, c0:c1]

    def phase_qkv(self, g, layer, xsrc, gain_row, wq_name, qoT, R_qo, kT=None, R_kT=None, Vaug=None, R_V=None,
                  xprev=None, wkv_name=None, gate_sb=None, R_gate=None, kmean=None, R_kmean=None, after_first_loads=None):
        nc, P = self.nc, self.P
        with ExitStack() as es:
            xrot = self.bufs(es, "p1x", [128, D], F32, 2)
            hrot = self.bufs(es, "p1h", [128, D], BF16, 2)
            strot = self.bufs(es, "p1st", [128, 2], F32, 4)
            junk = self.alloc(es, "p1junk", [128, D], BF16)
            R_junk = Res("junk")
            hTrot = self.bufs(es, "p1hT", [128, 16, 512], BF16, 2)
            wqrot = self.bufs(es, "p1wq", [128, 16, 512], BF16, 2)
            gbc, R_g = self.load_bc(es, "p1g", self.grow(gain_row))
            ptrot = self.bufs(es, "p1pT", [128, 4, 128], BF16, 3, psum=True)
            pmrot = self.bufs(es, "p1pm", [128, 512], F32, 3, psum=True)
            if layer == 0:
                wkv = self.alloc(es, "p1wkv", [128, 16, 512], BF16)
                R_wkv = Res("wkv")
                P.dma("pool", wkv[:], self.wslab_src(wkv_name, 0, 512), writes=[R_wkv])
                P.op("pool", lambda: nc.gpsimd.memset(Vaug[:, :, :, 64:65], 1.0), writes=R_V)
            else:
                qfrot = self.bufs(es, "p1qf", [128, 512], F32, 2)
                pg = self.palloc(es, "p1pg", [128, 4, 2, 16], F32)
                R_pg = Res("pg", excl=True)

            blocks = []
            if layer == 0:
                blocks += [("prev", 0), ("prev", 1)]
            blocks += [("mine", 0), ("mine", 1)]
            slab_buf = {}

            def issue_slab(n):
                if n >= 4:
                    return
                w, R_w = wqrot.next()
                P.dma("pool", w[:], self.wslab_src(wq_name, n * 512, (n + 1) * 512), writes=[R_w])
                slab_buf[n] = (w, R_w)
            issue_slab(0)
            issue_slab(1)
            if after_first_loads is not None:
                after_first_loads()
            evac_i = 0
            mine_blocks = []
            for (kind, b) in blocks:
                hT, R_hT = hTrot.next()
                for t in range(4):
                    i = g * GT + b * 4 + t
                    if kind == "prev":
                        src, sres = xprev[i, :, :], self.dr("xprev", i)
                    else:
                        src, sres = self.dram[xsrc][i, :, :], self.dr(xsrc, i)
                    h, R_h, _, _ = self.norm_tile(src, sres, xrot, hrot, strot, junk, R_junk, gbc, R_g)
                    self.transpose_tile(h, R_h, lambda cq, hT=hT, t=t: hT[:, cq * 4:(cq + 1) * 4, t * 128:(t + 1) * 128], R_hT, ptrot)
                if layer == 0:
                    col0 = (0 if kind == "prev" else GTOK) + b * 512
                    vt0 = (0 if kind == "prev" else GT) + b * 4
                    for kc in range(2):
                        pm, R_pm = pmrot.next()
                        for k in range(16):
                            P.op("pe", lambda k=k, kc=kc, pm=pm, hT=hT: nc.tensor.matmul(pm[:], lhsT=wkv[:, k, kc * 128:(kc + 1) * 128], rhs=hT[:, k, :], start=(k == 0), stop=(k == 15)),
                                 reads=[R_wkv, R_hT], writes=[R_pm], inc=(k == 15))
                        P.op("act", lambda kc=kc, pm=pm, col0=col0: nc.scalar.copy(out=kT[:, kc, col0:col0 + 512], in_=pm[:]),
                             reads=[R_pm], writes=[R_kT[(col0 // 128) + j] for j in range(4)])
                    for t in range(4):
                        pm, R_pm = pmrot.next()
                        for k in range(16):
                            P.op("pe", lambda k=k, t=t, pm=pm, hT=hT: nc.tensor.matmul(pm[:, 0:256], lhsT=hT[:, k, t * 128:(t + 1) * 128], rhs=wkv[:, k, 256:512], start=(k == 0), stop=(k == 15)),
                                 reads=[R_wkv, R_hT], writes=[R_pm], inc=(k == 15))
                        P.op("dve", lambda t=t, pm=pm, vt0=vt0: nc.vector.tensor_copy(out=Vaug[:, vt0 + t, :, 0:64], in_=pm[:, 0:256].rearrange("p (a b) -> p a b", a=4)),
                             reads=[R_pm], writes=[R_V[vt0 + t]])
                if kind == "mine":
                    mine_blocks.append((hT, R_hT, b))
            for sq in range(4):
                w, R_w = slab_buf.pop(sq)
                for (hT, R_hT, b) in mine_blocks:
                    for cc in range(4):
                        c = sq * 4 + cc
                        pm, R_pm = pmrot.next()
                        for k in range(16):
                            P.op("pe", lambda k=k, cc=cc, pm=pm, w=w, hT=hT: nc.tensor.matmul(pm[:], lhsT=w[:, k, cc * 128:(cc + 1) * 128], rhs=hT[:, k, :], start=(k == 0), stop=(k == 15)),
                                 reads=[R_w, R_hT], writes=[R_pm], inc=(k == 15))
                        dst = qoT[:, c, b * 512:(b + 1) * 512]
                        wr = [R_qo[b * 4 + j] for j in range(4)]
                        if layer == 0:
                            if evac_i % 2 == 0:
                                P.op("act", lambda pm=pm, dst=dst: nc.scalar.copy(out=dst, in_=pm[:]), reads=[R_pm], writes=wr)
                            else:
                                P.op("dve", lambda pm=pm, dst=dst: nc.vector.tensor_copy(out=dst, in_=pm[:]), reads=[R_pm], writes=wr)
                            evac_i += 1
                        else:
                            P.op("act", lambda pm=pm, dst=dst: nc.scalar.copy(out=dst, in_=pm[:]), reads=[R_pm], writes=wr)
                            qf, R_qf = qfrot.next()
                            P.op("dve", lambda pm=pm, qf=qf: nc.vector.tensor_copy(out=qf[:], in_=pm[:]), reads=[R_pm], writes=[R_qf])
                            kc = c // 8
                            n = 0
                            for t in range(4):
                                for u in range(2):
                                    P.op("pe", lambda t=t, u=u, qf=qf, kc=kc: nc.tensor.matmul(pg[:, t, u, :], lhsT=qf[u * 64:(u + 1) * 64, t * 128:(t + 1) * 128],
                                                                                              rhs=kmean[u * 64:(u + 1) * 64, kc, :], start=True, stop=True),
                                         reads=[R_qf, R_kmean], writes=[R_pg], inc=(n == 7))
                                    n += 1
                            P.op("dve", lambda c=c, b=b: nc.vector.tensor_copy(out=gate_sb[:, b * 4:(b + 1) * 4, 2 * c:2 * c + 2, :], in_=pg[:]),
                                 reads=[R_pg], writes=[R_gate])
                issue_slab(sq + 2)
        P.barrier()

    def finish_tile(self, o_t, R_o, qoT, R_qo_t, tl, ptrot):
        self.transpose_tile(o_t, R_o, lambda cq: qoT[:, cq * 4:(cq + 1) * 4, tl * 128:(tl + 1) * 128], R_qo_t, ptrot)

    def score_block(self, pS, R_pS, kT, R_k0, R_k1, kcol0, kcol1, kc, u, qoT, R_q, c0, tl, tab4, R_tab, pte, R_pte, ptm, R_ptm, mul_eng="dve"):
        nc, P = self.nc, self.P
        for bb, (kcol, R_k) in enumerate(((kcol0, R_k0), (kcol1, R_k1))):
            P.op("pe", lambda bb=bb, kcol=kcol: nc.tensor.matmul(pS[:, bb, :], lhsT=kT[u * 64:(u + 1) * 64, kc, kcol:kcol + 128],
                                                           rhs=qoT[u * 64:(u + 1) * 64, c0:c0 + 4, tl * 128:(tl + 1) * 128], start=True, stop=True),
                 reads=[R_k, R_q], writes=[R_pS], inc=(bb == 1))
        P.op("act", lambda: nc.scalar.activation(out=pte[:], in_=pS[:], func=AF.Exp, scale=0.125), reads=[R_pS], writes=[R_pte])
        meng = nc.vector if mul_eng == "dve" else nc.gpsimd
        P.op(mul_eng, lambda: meng.tensor_tensor(out=ptm[:].rearrange("p b (a t) -> p b a t", a=4), in0=pte[:].rearrange("p b (a t) -> p b a t", a=4),
                                                 in1=tab4, op=ALU.mult), reads=[R_pte, R_tab], writes=[R_ptm])

    def pv_block(self, pO, R_pO, ptm, R_ptm, Vaug, vt0, vt1, R_v0, R_v1, grp):
        nc, P = self.nc, self.P
        n = 0
        for hh in range(4):
            for bb, vt in enumerate((vt0, vt1)):
                P.op("pe", lambda hh=hh, bb=bb, vt=vt: nc.tensor.matmul(pO[:, hh, 0:65], lhsT=ptm[:, bb, hh * 128:(hh + 1) * 128], rhs=Vaug[:, vt, grp, 0:65],
                                                                    start=(bb == 0), stop=(bb == 1)),
                     reads=[R_ptm, R_v0, R_v1], writes=[R_pO], inc=(n == 7))
                n += 1

    def load_tab(self, es, name, dname):
        t = self.alloc(es, name, [128, 2, 16, 2, 128], BF16)
        r = Res(name)
        self.P.dma("pool", t[:].rearrange("p b c u t -> p b (c u t)"), self.dram[dname][:, :, :], writes=[r])
        return t, r

    def run_pipelined(self, units):
        if not units:
            return
        units[0][0]()
        for n in range(len(units)):
            if n + 1 < len(units):
                units[n + 1][0]()
            units[n][1]()

    def phase_swa(self, g, qoT, R_qo, kT, R_kT, Vaug, R_V, special=None, tabs=None):
        nc, P = self.nc, self.P
        special = special or {0: "tab_swa0"}
        with ExitStack() as es:
            sp_tabs = {}
            if tabs is not None:
                tab, R_tab = tabs["tab_swa"]
            else:
                tab, R_tab = self.load_tab(es, "swtab", "tab_swa")
            for ti, nm in special.items():
                if g * GT <= ti < (g + 1) * GT:
                    sp_tabs[ti] = self.load_tab(es, "swtab0", nm)
            snk, R_snk = self.load_bc(es, "snk", self.dram["sinks"][0:1, :])
            P.op("act", lambda: nc.scalar.activation(out=snk[:], in_=snk[:], func=AF.Exp), reads=[R_snk], writes=[R_snk])
            snk_v = snk[:].rearrange("p (c u) -> p c u", u=2)
            pSrot = self.bufs(es, "swS", [128, 2, 512], F32, 2, psum=True)
            pOrot = self.bufs(es, "swO", [128, 4, VW], F32, 2, psum=True)
            ptrot = self.bufs(es, "swpT", [128, 4, 128], BF16, 2, psum=True)
            pterot = self.bufs(es, "swpte", [128, 2, 512], BF16, 3)
            ptmrot = self.bufs(es, "swptm", [128, 2, 512], BF16, 3)
            orot = self.bufs(es, "swo", [128, D], BF16, 2)
            denrot = self.bufs(es, "swden", [128, 8], F32, 4)
            units = []
            for tl in range(GT):
                i = g * GT + tl
                tb, R_tb = sp_tabs.get(i, (tab, R_tab))
                tctx = {}
                batches = [(kc, u, c0) for kc in range(2) for u in range(2) for c0 in (8 * kc, 8 * kc + 4)]
                for bi, (kc, u, c0) in enumerate(batches):
                    st = {}

                    def score(tl=tl, kc=kc, u=u, c0=c0, st=st, tctx=tctx, first=(bi == 0), tb=tb, R_tb=R_tb):
                        if first:
                            tctx["o"] = orot.next()
                        pS, R_pS = pSrot.next()
                        pte, R_pte = pterot.next()
                        st["ptm"] = ptmrot.next()
                        self.score_block(pS, R_pS, kT, R_kT[tl], R_kT[GT + tl], tl * 128, GTOK + tl * 128, kc, u, qoT, R_qo[tl], c0, tl,
                                         tb[:, :, c0:c0 + 4, u, :], R_tb, pte, R_pte, st["ptm"][0], st["ptm"][1])

                    def post(tl=tl, kc=kc, u=u, c0=c0, st=st, tctx=tctx, last=(bi == len(batches) - 1)):
                        grp = 2 * kc + u
                        ptm, R_ptm = st["ptm"]
                        o_t, R_o = tctx["o"]
                        o_v = o_t[:].rearrange("p (c u d) -> p c u d", u=2, d=64)
                        pO, R_pO = pOrot.next()
                        self.pv_block(pO, R_pO, ptm, R_ptm, Vaug, tl, GT + tl, R_V[tl], R_V[GT + tl], grp)
                        den, R_den = denrot.next()
                        P.op("dve", lambda: nc.vector.tensor_tensor(out=den[:, 0:4], in0=pO[:, :, 64], in1=snk_v[:, c0:c0 + 4, u], op=ALU.add),
                             reads=[R_pO, R_snk], writes=[R_den])
                        P.op("dve", lambda: nc.vector.reciprocal(out=den[:, 4:8], in_=den[:, 0:4]), reads=[R_den], writes=[R_den])
                        P.op("dve", lambda: nc.vector.tensor_tensor(out=o_v[:, c0:c0 + 4, u, :], in0=pO[:, :, 0:64],
                                                                    in1=den[:, 4:8].unsqueeze(2).to_broadcast([128, 4, 64]), op=ALU.mult),
                             reads=[R_pO, R_den], writes=[R_o])
                        if last:
                            self.finish_tile(o_t, R_o, qoT, R_qo[tl], tl, ptrot)
                    units.append((score, post))
            self.run_pipelined(units)
        P.barrier()

    def moba_select(self, tl, i, gate_sb, R_gate, w_t, R_w, ft_v, R_ft, gw, eq, mx, R_sel):
        nc, P = self.nc, self.P
        npast = i
        if npast < 1:
            return
        gv = gate_sb[:, tl, :, 0:npast]
        fsl = ft_v[:, :, 15 - npast:15]
        wv = w_t[:, :, 0:npast]
        if npast <= 3:
            P.op("dve", lambda: nc.vector.tensor_copy(out=wv, in_=fsl), reads=[R_ft], writes=[R_w])
            return
        gwv = gw[:, :, 0:npast]
        eqv = eq[:, :, 0:npast]
        mb = mx[:].unsqueeze(2).to_broadcast([128, NH, npast])
        rs = [R_gate, R_sel]
        P.op("dve", lambda: nc.vector.tensor_reduce(out=mx[:], in_=gv, axis=AX.X, op=ALU.max), reads=rs, writes=[R_sel])
        P.op("dve", lambda: nc.vector.tensor_tensor(out=eqv, in0=gv, in1=mb, op=ALU.is_ge), reads=rs, writes=[R_sel])
        P.op("dve", lambda: nc.vector.scalar_tensor_tensor(out=gwv, in0=eqv, scalar=NEG, in1=gv, op0=ALU.mult, op1=ALU.add), reads=rs, writes=[R_sel])
        P.op("dve", lambda: nc.vector.tensor_reduce(out=mx[:], in_=gwv, axis=AX.X, op=ALU.max), reads=rs, writes=[R_sel])
        P.op("dve", lambda: nc.vector.tensor_tensor(out=eqv, in0=gwv, in1=mb, op=ALU.is_ge), reads=rs, writes=[R_sel])
        P.op("dve", lambda: nc.vector.scalar_tensor_tensor(out=gwv, in0=eqv, scalar=NEG, in1=gwv, op0=ALU.mult, op1=ALU.add), reads=rs, writes=[R_sel])
        P.op("dve", lambda: nc.vector.tensor_reduce(out=mx[:], in_=gwv, axis=AX.X, op=ALU.max), reads=rs, writes=[R_sel])
        P.op("dve", lambda: nc.vector.tensor_tensor(out=eqv, in0=gv, in1=mb, op=ALU.is_ge), reads=rs, writes=[R_sel])
        P.op("dve", lambda: nc.vector.tensor_tensor(out=wv, in0=eqv, in1=fsl, op=ALU.mult), reads=[R_sel, R_ft], writes=[R_w])

    def phase_moba(self, g, qoT, R_qo, kTa, R_kTa, Va, R_Va, gate_sb, R_gate, tabs=None):
        nc, P = self.nc, self.P
        with ExitStack() as es:
            if tabs is not None:
                tabF, R_tabF = tabs["tab_full_l"]
                tabO, R_tabO = tabs["tab_own_l"]
            else:
                tabF, R_tabF = self.load_tab(es, "mbF", "tab_full_l")
                tabO, R_tabO = self.load_tab(es, "mbO", "tab_own_l")
            ft, R_ft = self.load_bc(es, "mbft", self.dram["ftab"][0:1, :])
            ft_v = ft[:].rearrange("p (h k) -> p h k", k=15)
            pSrot = self.bufs(es, "mbS", [128, 2, 512], F32, 2, psum=True)
            pOrot = self.bufs(es, "mbOp", [128, 4, VW], F32, 2, psum=True)
            ptrot = self.bufs(es, "mbpT", [128, 4, 128], BF16, 2, psum=True)
            pterot = self.bufs(es, "mbpte", [128, 2, 512], BF16, 3)
            ptmrot = self.bufs(es, "mbptm", [128, 2, 512], BF16, 3)
            orot = self.bufs(es, "mbo", [128, D], BF16, 2)
            accrot = self.bufs(es, "mbacc", [128, 4, VW], F32, 3)
            denrot = self.bufs(es, "mbden", [128, 4], F32, 3)
            wrot = self.bufs(es, "mbw", [128, NH, 15], F32, 2)
            gw = self.alloc(es, "mbgw", [128, NH, 15], F32)
            eq = self.alloc(es, "mbeq", [128, NH, 15], F32)
            mx = self.alloc(es, "mbmx", [128, NH], F32)
            R_sel = Res("selwork")
            units = []
            for tl in range(GT):
                i = g * GT + tl
                npast = i
                tctx = {}
                batches = [(kc, u, c0) for kc in range(2) for u in range(2) for c0 in (8 * kc, 8 * kc + 4)]
                for bi, (kc, u, c0) in enumerate(batches):
                    bctx = {}
                    blocks = [i] + list(range(npast))
                    for ji, j in enumerate(blocks):
                        st = {}

                        def score(tl=tl, i=i, kc=kc, u=u, c0=c0, j=j, st=st, tctx=tctx, bctx=bctx,
                                  first_tile=(bi == 0 and ji == 0), first_batch=(ji == 0), mul_eng="dve"):
                            if first_tile:
                                tctx["o"] = orot.next()
                                tctx["w"] = wrot.next()
                                self.moba_select(tl, i, gate_sb, R_gate, tctx["w"][0], tctx["w"][1], ft_v, R_ft, gw, eq, mx, R_sel)
                            if first_batch:
                                bctx["acc"] = accrot.next()
                            own = (j == i)
                            tb, R_tb = (tabO, R_tabO) if own else (tabF, R_tabF)
                            pS, R_pS = pSrot.next()
                            pte, R_pte = pterot.next()
                            st["ptm"] = ptmrot.next()
                            self.score_block(pS, R_pS, kTa, R_kTa[j], R_kTa[16 + j], j * 128, (16 + j) * 128, kc, u, qoT, R_qo[tl], c0, tl,
                                             tb[:, :, c0:c0 + 4, u, :], R_tb, pte, R_pte, st["ptm"][0], st["ptm"][1], mul_eng=mul_eng)

                        def post(tl=tl, i=i, kc=kc, u=u, c0=c0, j=j, st=st, tctx=tctx, bctx=bctx,
                                 last_batch=(ji == len(blocks) - 1), last_tile=(bi == len(batches) - 1 and ji == len(blocks) - 1)):
                            grp = 2 * kc + u
                            ptm, R_ptm = st["ptm"]
                            o_t, R_o = tctx["o"]
                            w_t, R_w = tctx["w"]
                            acc, R_acc = bctx["acc"]
                            o_v = o_t[:].rearrange("p (c u d) -> p c u d", u=2, d=64)
                            pO, R_pO = pOrot.next()
                            self.pv_block(pO, R_pO, ptm, R_ptm, Va, j, 16 + j, R_Va[j], R_Va[16 + j], grp)
                            if j == i:
                                P.op("act", lambda: nc.scalar.copy(out=acc[:, :, 0:65], in_=pO[:, :, 0:65]), reads=[R_pO], writes=[R_acc])
                            else:
                                for hh in range(4):
                                    hi = (c0 + hh) * 2 + u
                                    P.op("dve", lambda hh=hh, hi=hi: nc.vector.scalar_tensor_tensor(
                                        out=acc[:, hh, 0:65], in0=pO[:, hh, 0:65], scalar=w_t[:, hi, j:j + 1], in1=acc[:, hh, 0:65], op0=ALU.mult, op1=ALU.add),
                                        reads=[R_pO, R_w, R_acc], writes=[R_acc])
                            if last_batch:
                                den, R_den = denrot.next()
                                P.op("dve", lambda: nc.vector.reciprocal(out=den[:], in_=acc[:, :, 64]), reads=[R_acc], writes=[R_den])
                                P.op("dve", lambda: nc.vector.tensor_tensor(out=o_v[:, c0:c0 + 4, u, :], in0=acc[:, :, 0:64],
                                                                            in1=den[:].unsqueeze(2).to_broadcast([128, 4, 64]), op=ALU.mult),
                                     reads=[R_acc, R_den], writes=[R_o])
                            if last_tile:
                                self.finish_tile(o_t, R_o, qoT, R_qo[tl], tl, ptrot)
                        units.append((score, post))
            self.run_pipelined(units)
        P.barrier()

    def prefetch_wo(self, es, wo_name):
        wo = self.alloc(es, "p3wo", [128, 16, D], BF16)
        R_wo = [Res(f"wo{n}") for n in range(4)]
        for nb in range(4):
            self.P.dma("pool", wo[:, :, nb * 512:(nb + 1) * 512], self.wslab_src(wo_name, nb * 512, (nb + 1) * 512), writes=[R_wo[nb]])
        return wo, R_wo

    def phase_oproj(self, g, qoT, R_qo, wo_name, gain_row, xsrc, xdst, wo_pre=None):
        nc, P = self.nc, self.P
        with ExitStack() as es:
            if wo_pre is not None:
                wo, R_wo = wo_pre
            else:
                wo, R_wo = self.prefetch_wo(es, wo_name)
            gbc, R_g = self.load_bc(es, "p3g", self.grow(gain_row))
            xrot = self.bufs(es, "p3x", [128, D], F32, 2)
            tmprot = self.bufs(es, "p3tmp", [128, D], F32, 2)
            xorot = self.bufs(es, "p3xo", [128, D], F32, 2)
            strot = self.bufs(es, "p3st", [128, 8], F32, 3)
            junk = self.alloc(es, "p3junk", [128, 512], BF16)
            R_junk = Res("junk")
            pmrot = self.bufs(es, "p3pm", [128, 512], F32, 8, psum=True)
            for tl in range(GT):
                i = g * GT + tl
                x, R_x = xrot.next()
                P.dma("sp", x[:], self.dram[xsrc][i, :, :], reads=[self.dr(xsrc, i)], writes=[R_x])
                st, R_st = strot.next()
                pms = []
                for nb in range(4):
                    pm, R_pm = pmrot.next()
                    pms.append((pm, R_pm))
                    for k in range(16):
                        P.op("pe", lambda k=k, nb=nb, pm=pm: nc.tensor.matmul(pm[:], lhsT=qoT[:, k, tl * 128:(tl + 1) * 128], rhs=wo[:, k, nb * 512:(nb + 1) * 512], start=(k == 0), stop=(k == 15)),
                             reads=[R_qo[tl], R_wo[nb]], writes=[R_pm], inc=(k == 15))
                    P.op("act", lambda nb=nb, pm=pm, st=st: nc.scalar.activation(out=junk[:], in_=pm[:], func=AF.Square, scale=RS, accum_out=st[:, 2 + nb:3 + nb]),
                         reads=[R_pm], writes=[R_st])
                P.op("dve", lambda st=st: nc.vector.tensor_reduce(out=st[:, 1:2], in_=st[:, 2:6], axis=AX.X, op=ALU.add), reads=[R_st], writes=[R_st])
                self.rstd_from_ms(st[:, 1:2], R_st, st, R_st)
                tmp, R_tmp = tmprot.next()
                for nb in range(4):
                    pm, R_pm = pms[nb]
                    P.op("dve", lambda nb=nb, pm=pm, st=st, tmp=tmp: nc.vector.scalar_tensor_tensor(out=tmp[:, nb * 512:(nb + 1) * 512], in0=pm[:], scalar=st[:, 0:1],
                                                                                                in1=gbc[:, nb * 512:(nb + 1) * 512], op0=ALU.mult, op1=ALU.mult),
                         reads=[R_pm, R_st, R_g], writes=[R_tmp])
                xo, R_xo = xorot.next()
                P.op("pool", lambda tmp=tmp, x=x, xo=xo: nc.gpsimd.tensor_tensor(out=xo[:], in0=tmp[:], in1=x[:], op=ALU.add), reads=[R_tmp, R_x], writes=[R_xo])
                P.dma("pool", self.dram[xdst][i, :, :], xo[:], reads=[R_xo], writes=[self.dr(xdst, i)])
        P.barrier()

    def phase_mlp(self, g, wup_name, wdn_name, gain_pre, gain_post, xsrc, xdst):
        nc, P = self.nc, self.P
        with ExitStack() as es0:
            h2T = self.alloc(es0, "p4h2T", [128, 16, GTOK], BF16)
            R_h2T = [Res(f"h2T{t}") for t in range(GT)]
            yacc = self.alloc(es0, "p4y", [128, GT, D], F32)
            R_y = [Res(f"y{t}") for t in range(GT)]
            esW = ExitStack()
            wuprot = self.bufs(esW, "p4wu", [128, 16, 512], BF16, 2)
            wdnrot = self.bufs(esW, "p4wd", [128, 4, D], BF16, 2)
            NS = DFF // 512
            wup_b, wdn_b, aT_b = {}, {}, {}
            wdn_src = self.dram[wdn_name].rearrange("(s c p) n -> s p c n", p=128, c=4)

            def load_up(s):
                if s < NS:
                    w, R_w = wuprot.next()
                    P.dma("pool", w[:], self.wslab_src(wup_name, s * 512, (s + 1) * 512), writes=[R_w])
                    wup_b[s] = (w, R_w)

            def load_dn(s):
                if s < NS:
                    w, R_w = wdnrot.next()
                    P.dma("pool", w[:], wdn_src[s], writes=[R_w])
                    wdn_b[s] = (w, R_w)
            load_up(0)
            load_dn(0)
            load_up(1)
            load_dn(1)
            with ExitStack() as es:
                xrot = self.bufs(es, "p4x", [128, D], F32, 2)
                hrot = self.bufs(es, "p4h", [128, D], BF16, 2)
                strot = self.bufs(es, "p4st", [128, 2], F32, 4)
                junk = self.alloc(es, "p4junk", [128, D], BF16)
                R_junk = Res("junk")
                gbc, R_g = self.load_bc(es, "p4g", self.grow(gain_pre))
                ptrot = self.bufs(es, "p4pT", [128, 4, 128], BF16, 4, psum=True)
                for tl in range(GT):
                    i = g * GT + tl
                    h, R_h, _, _ = self.norm_tile(self.dram[xsrc][i, :, :], self.dr(xsrc, i), xrot, hrot, strot, junk, R_junk, gbc, R_g)
                    self.transpose_tile(h, R_h, lambda cq, tl=tl: h2T[:, cq * 4:(cq + 1) * 4, tl * 128:(tl + 1) * 128], R_h2T[tl], ptrot)
            P.barrier()
            with ExitStack() as es:
                aTrot = self.bufs(es, "p4aT", [128, 4, GTOK], BF16, 2)
                rrot = self.bufs(es, "p4r", [128, 512], F32, 3)
                purot = self.bufs(es, "p4pu", [128, 512], F32, 3, psum=True)
                pdrot = self.bufs(es, "p4pd", [128, 512], F32, 4, psum=True)
                def up(s):
                    w, R_w = wup_b.pop(s)
                    aT, R_aT = aTrot.next()
                    aT_b[s] = (aT, R_aT)
                    for cc in range(4):
                        for tb in range(2):
                            pu, R_pu = purot.next()
                            for k in range(16):
                                P.op("pe", lambda k=k, cc=cc, tb=tb, pu=pu: nc.tensor.matmul(pu[:], lhsT=w[:, k, cc * 128:(cc + 1) * 128], rhs=h2T[:, k, tb * 512:(tb + 1) * 512], start=(k == 0), stop=(k == 15)),
                                     reads=[R_w] + R_h2T[tb * 4:(tb + 1) * 4], writes=[R_pu], inc=(k == 15))
                            r, R_r = rrot.next()
                            P.op("act", lambda pu=pu, r=r: nc.scalar.activation(out=r[:], in_=pu[:], func=AF.Relu), reads=[R_pu], writes=[R_r])
                            P.op("dve", lambda r=r, cc=cc, tb=tb, aT=aT: nc.vector.tensor_tensor(out=aT[:, cc, tb * 512:(tb + 1) * 512], in0=r[:], in1=r[:], op=ALU.mult),
                                 reads=[R_r], writes=[R_aT])

                def down(s):
                    w, R_w = wdn_b.pop(s)
                    aT, R_aT = aT_b.pop(s)
                    for tl in range(GT):
                        for nb in range(4):
                            pd, R_pd = pdrot.next()
                            for cc in range(4):
                                P.op("pe", lambda cc=cc, nb=nb, tl=tl, pd=pd: nc.tensor.matmul(pd[:], lhsT=aT[:, cc, tl * 128:(tl + 1) * 128], rhs=w[:, cc, nb * 512:(nb + 1) * 512], start=(cc == 0), stop=(cc == 3)),
                                     reads=[R_aT, R_w], writes=[R_pd], inc=(cc == 3))
                            ydst = yacc[:, tl, nb * 512:(nb + 1) * 512]
                            if s == 0:
                                P.op("act", lambda pd=pd, ydst=ydst: nc.scalar.copy(out=ydst, in_=pd[:]), reads=[R_pd], writes=[R_y[tl]])
                            else:
                                P.op("dve", lambda pd=pd, ydst=ydst: nc.vector.tensor_tensor(out=ydst, in0=pd[:], in1=ydst, op=ALU.add), reads=[R_pd, R_y[tl]], writes=[R_y[tl]])

                up(0)
                for s in range(NS):
                    if s + 1 < NS:
                        up(s + 1)
                    load_up(s + 2)
                    down(s)
                    load_dn(s + 2)
            esW.close()
            P.barrier()
            with ExitStack() as es:
                xrot = self.bufs(es, "p4x2", [128, D], F32, 3)
                tmprot = self.bufs(es, "p4tmp", [128, D], F32, 3)
                xorot = self.bufs(es, "p4xo", [128, D], F32, 3)
                strot = self.bufs(es, "p4st2", [128, 2], F32, 4)
                junk = self.alloc(es, "p4junk2", [128, D], BF16)
                R_junk = Res("junk")
                gbc, R_g = self.load_bc(es, "p4g2", self.grow(gain_post))
                for tl in range(GT):
                    i = g * GT + tl
                    x, R_x = xrot.next()
                    P.dma("sp", x[:], self.dram[xsrc][i, :, :], reads=[self.dr(xsrc, i)], writes=[R_x])
                    st, R_st = strot.next()
                    P.op("act", lambda tl=tl, st=st: nc.scalar.activation(out=junk[:], in_=yacc[:, tl, :], func=AF.Square, scale=RS, accum_out=st[:, 1:2]),
                         reads=[R_y[tl]], writes=[R_st])
                    self.rstd_from_ms(st[:, 1:2], R_st, st, R_st)
                    tmp, R_tmp = tmprot.next()
                    P.op("dve", lambda tl=tl, st=st, tmp=tmp: nc.vector.scalar_tensor_tensor(out=tmp[:], in0=yacc[:, tl, :], scalar=st[:, 0:1], in1=gbc[:], op0=ALU.mult, op1=ALU.mult),
                         reads=[R_y[tl], R_st, R_g], writes=[R_tmp])
                    xo, R_xo = xorot.next()
                    P.op("pool", lambda tmp=tmp, x=x, xo=xo: nc.gpsimd.tensor_tensor(out=xo[:], in0=tmp[:], in1=x[:], op=ALU.add), reads=[R_tmp, R_x], writes=[R_xo])
                    P.dma("pool", self.dram[xdst][i, :, :], xo[:], reads=[R_xo], writes=[self.dr(xdst, i)])
        P.barrier()

    def phase_kvshared(self, xsrc, kdst, vdst, ksdst, R_dst, ntiles=NT):
        nc, P = self.nc, self.P
        with ExitStack() as es:
            xrot = self.bufs(es, "p5x", [128, D], F32, 4)
            hrot = self.bufs(es, "p5h", [128, D], BF16, 4)
            strot = self.bufs(es, "p5st", [128, 2], F32, 6)
            junk = self.alloc(es, "p5junk", [128, D], BF16)
            R_junk = Res("junk")
            hTrot = self.bufs(es, "p5hT", [128, 16, 512], BF16, 2)
            gbc, R_g = self.load_bc(es, "p5g", self.grow(4))
            ptrot = self.bufs(es, "p5pT", [128, 4, 128], BF16, 4, psum=True)
            pmrot = self.bufs(es, "p5pm", [128, 512], F32, 3, psum=True)
            wkv = self.alloc(es, "p5wkv", [128, 16, 512], BF16)
            R_wkv = Res("wkvs")
            P.dma("pool", wkv[:], self.wslab_src("wkvs", 0, 512), writes=[R_wkv])
            ksum = self.alloc(es, "p5ksum", [128, 2, ntiles], F32)
            R_ksum = Res("ksum")
            kstrot = self.bufs(es, "p5kst", [128, 512], F32, 2)
            vstrot = self.bufs(es, "p5vst", [128, 256], F32, 2)
            for b in range(ntiles // 4):
                hT, R_hT = hTrot.next()
                for t in range(4):
                    i = b * 4 + t
                    h, R_h, _, _ = self.norm_tile(self.dram[xsrc][i, :, :], self.dr(xsrc, i), xrot, hrot, strot, junk, R_junk, gbc, R_g)
                    self.transpose_tile(h, R_h, lambda cq, hT=hT, t=t: hT[:, cq * 4:(cq + 1) * 4, t * 128:(t + 1) * 128], R_hT, ptrot)
                for kc in range(2):
                    pm, R_pm = pmrot.next()
                    for k in range(16):
                        P.op("pe", lambda k=k, kc=kc, pm=pm, hT=hT: nc.tensor.matmul(pm[:], lhsT=wkv[:, k, kc * 128:(kc + 1) * 128], rhs=hT[:, k, :], start=(k == 0), stop=(k == 15)),
                             reads=[R_wkv, R_hT], writes=[R_pm], inc=(k == 15))
                    kst, R_kst = kstrot.next()
                    P.op("act", lambda pm=pm, kst=kst: nc.scalar.copy(out=kst[:], in_=pm[:]), reads=[R_pm], writes=[R_kst])
                    P.op("dve", lambda pm=pm, kc=kc, b=b: nc.vector.tensor_reduce(out=ksum[:, kc, b * 4:(b + 1) * 4], in_=pm[:].rearrange("p (a t) -> p a t", a=4), axis=AX.X, op=ALU.add),
                         reads=[R_pm, R_ksum], writes=[R_ksum])
                    P.dma("pool", kdst(kc, b), kst[:], reads=[R_kst], writes=[R_dst])
                for t in range(4):
                    i = b * 4 + t
                    pm, R_pm = pmrot.next()
                    for k in range(16):
                        P.op("pe", lambda k=k, t=t, pm=pm, hT=hT: nc.tensor.matmul(pm[:, 0:256], lhsT=hT[:, k, t * 128:(t + 1) * 128], rhs=wkv[:, k, 256:512], start=(k == 0), stop=(k == 15)),
                             reads=[R_wkv, R_hT], writes=[R_pm], inc=(k == 15))
                    vst, R_vst = vstrot.next()
                    P.op("dve", lambda pm=pm, vst=vst: nc.vector.tensor_copy(out=vst[:], in_=pm[:, 0:256]), reads=[R_pm], writes=[R_vst])
                    P.dma("pool", vdst(i), vst[:], reads=[R_vst], writes=[R_dst])
            P.dma("sp", ksdst, ksum[:], reads=[R_ksum], writes=[R_dst])
        P.barrier()

    def layer0(self, xin, x1, x2, ngroups=NG, special=None, tabs=None):
        nc, P = self.nc, self.P
        for g in range(ngroups):
            with ExitStack() as es:
                qoT = self.alloc(es, "qoT", [128, 16, GTOK], BF16)
                R_qo = [Res(f"qo{t}") for t in range(GT)]
                kT = self.alloc(es, "kT", [128, 2, 2 * GTOK], BF16)
                R_kT = [Res(f"kT{t}") for t in range(2 * GT)]
                Vaug = self.alloc(es, "Vaug", [128, 2 * GT, 4, VW], BF16)
                R_V = [Res(f"V{t}") for t in range(2 * GT)]
                tabs = {}
                tab_t = self.alloc(es, "swtab", [128, 2, 16, 2, 128], BF16)
                tabs["tab_swa"] = (tab_t, Res("swtab"))

                def load_tab_now(tab_t=tab_t, tabs=tabs):
                    P.dma("pool", tab_t[:].rearrange("p b c u t -> p b (c u t)"), self.dram["tab_swa"][:, :, :], writes=[tabs["tab_swa"][1]])
                self.phase_qkv(g, 0, xin, 0, "wq0", qoT, R_qo, kT=kT, R_kT=R_kT, Vaug=Vaug, R_V=R_V, xprev=self.dram["xprev"], wkv_name="wkv0",
                               after_first_loads=load_tab_now)
                wo_pre = self.prefetch_wo(es, "wo0")
                self.phase_swa(g, qoT, R_qo, kT, R_kT, Vaug, R_V, special=special, tabs=tabs)
                self.phase_oproj(g, qoT, R_qo, "wo0", 1, xin, x1, wo_pre=wo_pre)
            P.barrier()
            self.phase_mlp(g, "wup0", "wdn0", 2, 3, x1, x2)

    def load_kv_host(self, kt_src, v_src, ksa, ksb):
        def f(kTa, R_kTa, Va, R_Va, kmean, R_kmean, ksb_t, R_ksb):
            P = self.P
            P.dma("pool", kTa[:], kt_src, writes=R_kTa)
            v4 = v_src.rearrange("p t (g d) -> p t g d", g=4)
            for q4 in range(4):
                P.dma("pool", Va[:, q4 * 8:(q4 + 1) * 8, :, 0:64], v4[:, q4 * 8:(q4 + 1) * 8, :, :], writes=R_Va[q4 * 8:(q4 + 1) * 8])
            P.dma("sp", kmean[:], ksa, writes=[R_kmean])
            P.dma("sp", ksb_t[:], ksb, writes=[R_ksb])
        return f

    def load_kv_gathered(self, ex, R_ex):
        def f(kTa, R_kTa, Va, R_Va, kmean, R_kmean, ksb_t, R_ksb):
            P = self.P
            kv = kTa[:].rearrange("p c (i r t) -> p c i r t", r=2, t=128)
            vv = Va[:].rearrange("p (i r) g w -> p i r g w", r=2)
            for r in range(2):
                rows = ex[r * 128:(r + 1) * 128, :]
                P.dma("pool", kv[:, :, :, r, :], rows[:, 0:4096].rearrange("p (c i t) -> p c i t", c=2, i=16), reads=[R_ex], writes=R_kTa)
                vsrc = rows[:, 4096:8192].rearrange("p (i g d) -> p i g d", g=4, d=64)
                for gq in range(4):
                    P.dma("pool", vv[:, :, r, gq, 0:64], vsrc[:, :, gq, :], reads=[R_ex], writes=R_Va)
            P.dma("sp", kmean[:], ex[0:128, 8192:8224].rearrange("p (c j) -> p c j", c=2), reads=[R_ex], writes=[R_kmean])
            P.dma("sp", ksb_t[:], ex[128:256, 8192:8224].rearrange("p (c j) -> p c j", c=2), reads=[R_ex], writes=[R_ksb])
        return f

    def layer1(self, xin, x3, xout, loader, tabs=None):
        nc, P = self.nc, self.P
        for g in range(NG):
            with ExitStack() as es:
                kTa = self.alloc(es, "kTa", [128, 2, 32 * 128], BF16)
                R_kTa = [Res(f"kTa{t}") for t in range(32)]
                Va = self.alloc(es, "Va", [128, 32, 4, VW], BF16)
                R_Va = [Res(f"Va{t}") for t in range(32)]
                kmean = self.alloc(es, "kmean", [128, 2, 16], F32)
                R_kmean = Res("kmean")
                ksb_t = self.alloc(es, "ksb_t", [128, 2, 16], F32)
                R_ksb = Res("ksb")
                loader(kTa, R_kTa, Va, R_Va, kmean, R_kmean, ksb_t, R_ksb)
                P.op("pool", lambda: nc.gpsimd.memset(Va[:, :, :, 64:65], 1.0), reads=R_Va, writes=R_Va)
                P.op("dve", lambda: nc.vector.tensor_tensor(out=kmean[:], in0=kmean[:], in1=ksb_t[:], op=ALU.add), reads=[R_kmean, R_ksb], writes=[R_kmean])
                P.op("dve", lambda: nc.vector.tensor_scalar(out=kmean[:], in0=kmean[:], scalar1=1.0 / 256.0, scalar2=None, op0=ALU.mult), reads=[R_kmean], writes=[R_kmean])
                qoT = self.alloc(es, "qoT1", [128, 16, GTOK], BF16)
                R_qo = [Res(f"qo{t}") for t in range(GT)]
                gate_sb = self.alloc(es, "gate", [128, GT, NH, 16], F32)
                R_gate = Res("gate")
                self.phase_qkv(g, 1, xin, 0, "wq1", qoT, R_qo, gate_sb=gate_sb, R_gate=R_gate, kmean=kmean, R_kmean=R_kmean)
                self.phase_moba(g, qoT, R_qo, kTa, R_kTa, Va, R_Va, gate_sb, R_gate, tabs=tabs)
                self.phase_oproj(g, qoT, R_qo, "wo1", 1, xin, x3)
            P.barrier()
            self.phase_mlp(g, "wup1", "wdn1", 2, 3, x3, xout)
        P.barrier()


def build_stage1():
    nc = bass.Bass("TRN2", target_bir_lowering=False)
    B = Builder(nc)
    B.din("xmine", [NT, 128, D])
    B.din("xprev", [NT, 128, D])
    B.din("wq0", [D, D])
    B.din("wkv0", [D, 512])
    B.din("wo0", [D, D])
    B.din("wup0", [D, DFF])
    B.din("wdn0", [DFF, D])
    B.din("wkvs", [D, 512])
    B.din("gains", [5, D])
    B.din("sinks", [1, NH])
    B.din("ident", [128, 128])
    B.din("tab_swa", [128, 2, NH * 128])
    B.din("tab_swa0", [128, 2, NH * 128])
    B.dtmp("x1", [NT, 128, D])
    B.dout("x2", [NT, 128, D])
    B.dout("kts", [128, 2, NT * 128])
    B.dout("vs", [128, NT, 256])
    B.dout("ksum", [128, 2, NT])
    with ExitStack() as es:
        B.setup_consts(es)
        B.layer0("xmine", "x1", "x2")
        B.phase_kvshared("x2", lambda kc, b: B.dram["kts"][:, kc, b * 512:(b + 1) * 512], lambda i: B.dram["vs"][:, i, :],
                         B.dram["ksum"][:, :, :], Res("kvout"))
        B.P.barrier()
    return nc


def build_stage2():
    nc = bass.Bass("TRN2", target_bir_lowering=False)
    B = Builder(nc)
    B.din("x2", [NT, 128, D])
    B.din("kt_all", [128, 2, 32 * 128])
    B.din("v_all", [128, 32, 256])
    B.din("ksa", [128, 2, 16])
    B.din("ksb", [128, 2, 16])
    B.din("wq1", [D, D])
    B.din("wo1", [D, D])
    B.din("wup1", [D, DFF])
    B.din("wdn1", [DFF, D])
    B.din("gains", [4, D])
    B.din("ident", [128, 128])
    B.din("tab_full", [128, 2, NH * 128])
    B.din("tab_own", [128, 2, NH * 128])
    B.din("ftab", [1, NH * 15])
    B.dtmp("x3", [NT, 128, D])
    B.dout("out", [NT, 128, D])
    with ExitStack() as es:
        B.setup_consts(es)
        B.layer1("x2", "x3", "out", B.load_kv_host(B.dram["kt_all"][:, :, :], B.dram["v_all"][:, :, :], B.dram["ksa"][:, :, :], B.dram["ksb"][:, :, :]))
        B.P.barrier()
    return nc


def core_tiles(x, b, p):
    xb = x[b].reshape(32, 128, D)
    mine = np.ascontiguousarray(xb[p::2])
    prev = np.zeros_like(mine)
    for i in range(NT):
        gt = 2 * i + p - 1
        if gt >= 0:
            prev[i] = xb[gt]
    return mine, prev


def stage1_inputs(inputs):
    perm = q_perm()
    wqkv = inputs["w_qkv_a"][0]
    common = {
        "wq0": np.ascontiguousarray(wqkv[:, :D][:, perm]),
        "wkv0": np.ascontiguousarray(wqkv[:, D:]),
        "wo0": np.ascontiguousarray(inputs["w_o_a"][0][perm, :]),
        "wup0": np.ascontiguousarray(inputs["w_up"][0]),
        "wdn0": np.ascontiguousarray(inputs["w_down"][0]),
        "wkvs": np.ascontiguousarray(inputs["w_kv_shared"]),
        "gains": np.ascontiguousarray(np.stack([inputs["norm_attn_pre"][0], inputs["norm_attn_post"][0], inputs["norm_mlp_pre"][0],
                                               inputs["norm_mlp_post"][0], inputs["kv_norm"]]).astype(np.float32)),
        "sinks": np.ascontiguousarray(inputs["sinks_a"][0][[head_of(c, u) for c in range(16) for u in range(2)]].reshape(1, NH)),
        "ident": np.eye(128, dtype=np.float32),
    }
    tabs = [build_tables(0), build_tables(1)]
    maps = []
    for c in range(8):
        b, p = divmod(c, 2)
        mine, prev = core_tiles(inputs["x"], b, p)
        m = dict(common)
        m["xmine"] = mine
        m["xprev"] = prev
        m["tab_swa"] = tabs[p]["tab_swa"]
        m["tab_swa0"] = tabs[p]["tab_swa0"]
        maps.append(m)
    return maps


def stage2_inputs(inputs, r1):
    perm = q_perm()
    common = {
        "wq1": np.ascontiguousarray(inputs["w_q_b"][0][:, perm]),
        "wo1": np.ascontiguousarray(inputs["w_o_b"][0][perm, :]),
        "wup1": np.ascontiguousarray(inputs["w_up"][1]),
        "wdn1": np.ascontiguousarray(inputs["w_down"][1]),
        "gains": np.ascontiguousarray(np.stack([inputs["norm_attn_pre"][1], inputs["norm_attn_post"][1], inputs["norm_mlp_pre"][1],
                                               inputs["norm_mlp_post"][1]]).astype(np.float32)),
        "ident": np.eye(128, dtype=np.float32),
    }
    tabs = [build_tables(0), build_tables(1)]
    maps = []
    for c in range(8):
        b, p = divmod(c, 2)
        ra, rb = r1[2 * b], r1[2 * b + 1]
        kt_all = np.zeros((128, 2, 32, 128), dtype=np.float32)
        kt_all[:, :, 0::2, :] = ra["kts"].reshape(128, 2, NT, 128)
        kt_all[:, :, 1::2, :] = rb["kts"].reshape(128, 2, NT, 128)
        v_all = np.zeros((128, 32, 256), dtype=np.float32)
        v_all[:, 0::2, :] = ra["vs"]
        v_all[:, 1::2, :] = rb["vs"]
        m = dict(common)
        m["x2"] = np.ascontiguousarray(r1[c]["x2"])
        m["kt_all"] = kt_all.reshape(128, 2, 32 * 128)
        m["v_all"] = v_all
        m["ksa"] = np.ascontiguousarray(ra["ksum"])
        m["ksb"] = np.ascontiguousarray(rb["ksum"])
        m["tab_full"] = tabs[p]["tab_full"]
        m["tab_own"] = tabs[p]["tab_own"]
        m["ftab"] = tabs[p]["ftab"]
        maps.append(m)
    return maps


def assemble(outs):
    res = np.zeros((4, 32, 128, D), dtype=np.float32)
    for c in range(8):
        b, p = divmod(c, 2)
        res[b, p::2] = outs[c]
    return res.reshape(4, 4096, D)


NT0 = 32


def build_fused(n_cores=8):
    nc = bass.Bass("TRN2", target_bir_lowering=False)
    B = Builder(nc)
    B.din("xall", [NT0, 128, D])
    B.din("xprev", [NT0, 128, D])
    for nm, shp in (("wq0", [D, D]), ("wkv0", [D, 512]), ("wo0", [D, D]), ("wup0", [D, DFF]), ("wdn0", [DFF, D]), ("wkvs", [D, 512]),
                    ("wq1", [D, D]), ("wo1", [D, D]), ("wup1", [D, DFF]), ("wdn1", [DFF, D])):
        B.din(nm, shp)
    B.din("gains", [9, D])
    B.din("sinks", [1, NH])
    B.din("ident", [128, 128])
    for nm in ("tab_swa", "tab_swaA", "tab_swaB", "tab_full_l", "tab_own_l"):
        B.din(nm, [128, 2, NH * 128])
    B.din("ftab", [1, NH * 15])
    B.dtmp("x1", [NT0, 128, D])
    B.dtmp("x2", [NT0, 128, D])
    B.dtmp("x3", [NT, 128, D])
    kts = B.dtmp("kts", [128, 2, NT0 * 128])
    vs = B.dtmp("vs", [128, NT0, 256])
    ksum = B.dtmp("ksum", [128, 2, NT0])
    B.dout("out", [NT, 128, D])
    with ExitStack() as es:
        B.setup_consts(es)
        B.gbase = 0
        B.layer0("xall", "x1", "x2", ngroups=NT0 // GT, special={0: "tab_swaA", 16: "tab_swaB"})
        B.phase_kvshared("x2", lambda kc, b: kts[:, kc, b * 512:(b + 1) * 512], lambda i: vs[:, i, :], ksum[:, :, :], Res("kvout"), ntiles=NT0)
        B.gbase = 5
        B.layer1("x2", "x3", "out", B.load_kv_host(kts[:, :, :], vs[:, :, :], ksum[:, :, 0:16], ksum[:, :, 16:32]))
        B.P.barrier()
    return nc


def local_tiles(x, b, p):
    xb = x[b].reshape(32, 128, D)
    order = list(range(p, 32, 2)) + list(range(1 - p, 32, 2))
    xall = np.ascontiguousarray(xb[order])
    prev = np.zeros_like(xall)
    for L, gt in enumerate(order):
        if gt >= 1:
            prev[L] = xb[gt - 1]
    return xall, prev


def fused_inputs(inputs):
    perm = q_perm()
    wqkv = inputs["w_qkv_a"][0]
    common = {
        "wq0": np.ascontiguousarray(wqkv[:, :D][:, perm]),
        "wkv0": np.ascontiguousarray(wqkv[:, D:]),
        "wo0": np.ascontiguousarray(inputs["w_o_a"][0][perm, :]),
        "wup0": np.ascontiguousarray(inputs["w_up"][0]),
        "wdn0": np.ascontiguousarray(inputs["w_down"][0]),
        "wkvs": np.ascontiguousarray(inputs["w_kv_shared"]),
        "wq1": np.ascontiguousarray(inputs["w_q_b"][0][:, perm]),
        "wo1": np.ascontiguousarray(inputs["w_o_b"][0][perm, :]),
        "wup1": np.ascontiguousarray(inputs["w_up"][1]),
        "wdn1": np.ascontiguousarray(inputs["w_down"][1]),
        "gains": np.ascontiguousarray(np.stack([inputs["norm_attn_pre"][0], inputs["norm_attn_post"][0], inputs["norm_mlp_pre"][0],
                                               inputs["norm_mlp_post"][0], inputs["kv_norm"], inputs["norm_attn_pre"][1],
                                               inputs["norm_attn_post"][1], inputs["norm_mlp_pre"][1], inputs["norm_mlp_post"][1]]).astype(np.float32)),
        "sinks": np.ascontiguousarray(inputs["sinks_a"][0][[head_of(c, u) for c in range(16) for u in range(2)]].reshape(1, NH)),
        "ident": np.eye(128, dtype=np.float32),
    }
    tabs = [build_tables(0), build_tables(1)]
    maps = []
    for c in range(8):
        b, p = divmod(c, 2)
        m = dict(common)
        m["xall"], m["xprev"] = local_tiles(inputs["x"], b, p)
        for nm in ("tab_swa", "tab_swaA", "tab_swaB", "tab_full_l", "tab_own_l", "ftab"):
            m[nm] = tabs[p][nm]
        maps.append(m)
    return maps


def kernel(**inputs):
    inputs = {k: np.asarray(v) for k, v in inputs.items()}
    nc = build_fused(8)
    r = run_bass_kernel_spmd(nc, fused_inputs(inputs), core_ids=list(range(8))).results
    return assemble([x["out"] for x in r])
```
